# Optimizing a Trainium2 kernel written in Bass

```python
import math
import jax
import jax.numpy as jnp
from jax import lax
import numpy as np

D_MODEL = 1024
BATCH = 8
SEQ = 2048
DEPTH = 4

GRID_W = 64
CTX_LEN = 256
Q_BLOCK = 128
N_MIXERS = 4
ROPE_THETA = 10000.0
EPS = 1e-6
NEG_INF = -1e30

GQA_HEADS = 8
GQA_KV_HEADS = 2
GQA_HEAD_DIM = 128
MLA_HEADS = 8
MLA_Q_LORA = 384
MLA_KV_LORA = 256
MLA_NOPE = 128
MLA_ROPE = 64
MLA_V = 128
WIN_HEADS = 16
WIN_KV_HEADS = 2
WIN_HEAD_DIM = 64
WINDOW = 128
DIFF_HEADS = 8
DIFF_HEAD_DIM = 64
DIFF_V_DIM = 2 * DIFF_HEAD_DIM
DENSE_FF = 3584
N_EXPERTS = 8
TOP_K = 2
EXPERT_FF = 3584

N_GQA = (DEPTH + 3) // 4
N_MLA = (DEPTH + 2) // 4
N_WIN = (DEPTH + 1) // 4
N_DIFF = DEPTH // 4
N_DENSE = (DEPTH + 1) // 2
N_MOE = DEPTH // 2

kernel_name = "hybrid_interleaved_diffusion_backbone"


def rms_norm(x, g):
    xf = x.astype(jnp.float32)
    y = xf * lax.rsqrt(jnp.mean(xf * xf, axis=-1, keepdims=True) + EPS)
    return (y * g.astype(jnp.float32)).astype(x.dtype)


def modulate(h, shift, scale):
    return h * (1.0 + scale) + shift


def axial_rope_tables(rows, rot_dim):
    quarter = rot_dim // 4
    inv_freq = ROPE_THETA ** (-jnp.arange(quarter, dtype=jnp.float32) / quarter)
    row = jnp.repeat(jnp.arange(rows, dtype=jnp.float32), GRID_W)
    col = jnp.tile(jnp.arange(GRID_W, dtype=jnp.float32), rows)
    ang = jnp.concatenate([row[:, None] * inv_freq, col[:, None] * inv_freq], axis=-1)
    return jnp.cos(ang), jnp.sin(ang)


def apply_rope(x, cos, sin):
    half = x.shape[-1] // 2
    shape = (1, x.shape[1]) + (1,) * (x.ndim - 3) + (half,)
    cs, sn = cos.reshape(shape), sin.reshape(shape)
    xf = x.astype(jnp.float32)
    x1, x2 = xf[..., :half], xf[..., half:]
    return jnp.concatenate([x1 * cs - x2 * sn, x1 * sn + x2 * cs], axis=-1).astype(x.dtype)


def attend(q, k, v, scale, mask=None, sink=None):
    s = jnp.einsum('bqhgd,bkhd->bhgqk', q, k).astype(jnp.float32) * scale
    if mask is not None:
        s = jnp.where(mask, s, NEG_INF)
    if sink is None:
        p = jax.nn.softmax(s, axis=-1)
    else:
        sk = sink.astype(jnp.float32)[None, :, :, None, None]
        m = jnp.maximum(jnp.max(s, axis=-1, keepdims=True), sk)
        e = jnp.exp(s - m)
        p = e / (jnp.sum(e, axis=-1, keepdims=True) + jnp.exp(sk - m))
    return jnp.einsum('bhgqk,bkhd->bqhgd', p.astype(v.dtype), v)


def sweep_query_blocks(fn, *qs):
    B, S = qs[0].shape[:2]
    nb = S // Q_BLOCK
    blocks = tuple(jnp.moveaxis(q.reshape((B, nb, Q_BLOCK) + q.shape[2:]), 1, 0) for q in qs)
    out = lax.map(lambda a: fn(a[0], *a[1:]), (jnp.arange(nb),) + blocks)
    out = jnp.moveaxis(out, 0, 1)
    return out.reshape((B, S) + out.shape[3:])


def gqa_project(h, w_qkv, q_gain, k_gain, n_kv, group, hd):
    B, T, _ = h.shape
    qkv = h @ w_qkv
    nq, nk = n_kv * group * hd, n_kv * hd
    q = qkv[..., :nq].reshape(B, T, n_kv, group, hd)
    k = qkv[..., nq:nq + nk].reshape(B, T, n_kv, hd)
    v = qkv[..., nq + nk:].reshape(B, T, n_kv, hd)
    return rms_norm(q, q_gain), rms_norm(k, k_gain), v


def gqa_axial(h_lat, h_ctx, w_qkv, q_gain, k_gain, w_o, rope, need_ctx):
    B, S, _ = h_lat.shape
    G = GQA_HEADS // GQA_KV_HEADS
    scale = GQA_HEAD_DIM ** -0.5
    q_l, k_l, v_l = gqa_project(h_lat, w_qkv, q_gain, k_gain, GQA_KV_HEADS, G, GQA_HEAD_DIM)
    q_l, k_l = apply_rope(q_l, *rope), apply_rope(k_l, *rope)
    q_c, k_c, v_c = gqa_project(h_ctx, w_qkv, q_gain, k_gain, GQA_KV_HEADS, G, GQA_HEAD_DIM)
    k_all = jnp.concatenate([k_c, k_l], axis=1)
    v_all = jnp.concatenate([v_c, v_l], axis=1)
    o_l = sweep_query_blocks(lambda i, qb: attend(qb, k_all, v_all, scale), q_l)
    o_l = o_l.reshape(B, S, -1) @ w_o
    o_c = attend(q_c, k_c, v_c, scale).reshape(B, h_ctx.shape[1], -1) @ w_o if need_ctx else None
    return o_l, o_c


def mla(h_lat, h_ctx, w_down, qa_gain, kva_gain, w_uq, w_ukv, q_gain, k_gain, w_o, rope, need_ctx):
    def project(h, rope_tab):
        B, T, _ = h.shape
        dn = h @ w_down
        cq = rms_norm(dn[..., :MLA_Q_LORA], qa_gain)
        ckv = rms_norm(dn[..., MLA_Q_LORA:MLA_Q_LORA + MLA_KV_LORA], kva_gain)
        k_pe = rms_norm(dn[..., MLA_Q_LORA + MLA_KV_LORA:], k_gain[MLA_NOPE:])
        q = (cq @ w_uq).reshape(B, T, MLA_HEADS, MLA_NOPE + MLA_ROPE)
        q_nope = rms_norm(q[..., :MLA_NOPE], q_gain[:MLA_NOPE])
        q_pe = rms_norm(q[..., MLA_NOPE:], q_gain[MLA_NOPE:])
        kv = (ckv @ w_ukv).reshape(B, T, MLA_HEADS, MLA_NOPE + MLA_V)
        k_nope = rms_norm(kv[..., :MLA_NOPE], k_gain[:MLA_NOPE])
        v = kv[..., MLA_NOPE:]
        if rope_tab is not None:
            q_pe, k_pe = apply_rope(q_pe, *rope_tab), apply_rope(k_pe, *rope_tab)
        q = jnp.concatenate([q_nope, q_pe], axis=-1)[:, :, :, None, :]
        k = jnp.concatenate([k_nope, jnp.broadcast_to(k_pe[:, :, None, :], (B, T, MLA_HEADS, MLA_ROPE))], axis=-1)
        return q, k, v

    B, S, _ = h_lat.shape
    scale = (MLA_NOPE + MLA_ROPE) ** -0.5
    q_l, k_l, v_l = project(h_lat, rope)
    q_c, k_c, v_c = project(h_ctx, None)
    k_all = jnp.concatenate([k_c, k_l], axis=1)
    v_all = jnp.concatenate([v_c, v_l], axis=1)
    o_l = sweep_query_blocks(lambda i, qb: attend(qb, k_all, v_all, scale), q_l)
    o_l = o_l.reshape(B, S, -1) @ w_o
    o_c = attend(q_c, k_c, v_c, scale).reshape(B, h_ctx.shape[1], -1) @ w_o if need_ctx else None
    return o_l, o_c


def window_gqa(h_lat, h_ctx, w_qkv, q_gain, k_gain, sink, w_o, rope, need_ctx):
    B, S, _ = h_lat.shape
    G = WIN_HEADS // WIN_KV_HEADS
    scale = WIN_HEAD_DIM ** -0.5
    sink = sink.reshape(WIN_KV_HEADS, G)
    q_l, k_l, v_l = gqa_project(h_lat, w_qkv, q_gain, k_gain, WIN_KV_HEADS, G, WIN_HEAD_DIM)
    q_l, k_l = apply_rope(q_l, *rope), apply_rope(k_l, *rope)
    q_c, k_c, v_c = gqa_project(h_ctx, w_qkv, q_gain, k_gain, WIN_KV_HEADS, G, WIN_HEAD_DIM)
    pad = ((0, 0), (Q_BLOCK, Q_BLOCK), (0, 0), (0, 0))
    k_pad, v_pad = jnp.pad(k_l, pad), jnp.pad(v_l, pad)
    q_off = jnp.arange(Q_BLOCK)
    k_off = jnp.arange(3 * Q_BLOCK) - Q_BLOCK
    band = jnp.abs(q_off[:, None] - k_off[None, :]) <= WINDOW
    ctx_mask = jnp.ones((Q_BLOCK, k_c.shape[1]), dtype=bool)

    def block(i, qb):
        kpos = i * Q_BLOCK + k_off
        valid = band & ((kpos >= 0) & (kpos < S))[None, :]
        kb = lax.dynamic_slice_in_dim(k_pad, i * Q_BLOCK, 3 * Q_BLOCK, axis=1)
        vb = lax.dynamic_slice_in_dim(v_pad, i * Q_BLOCK, 3 * Q_BLOCK, axis=1)
        k_all = jnp.concatenate([k_c, kb], axis=1)
        v_all = jnp.concatenate([v_c, vb], axis=1)
        mask = jnp.concatenate([ctx_mask, valid], axis=1)
        return attend(qb, k_all, v_all, scale, mask=mask, sink=sink)

    o_l = sweep_query_blocks(block, q_l).reshape(B, S, -1) @ w_o
    o_c = attend(q_c, k_c, v_c, scale, sink=sink).reshape(B, h_ctx.shape[1], -1) @ w_o if need_ctx else None
    return o_l, o_c


def diff_attention(h_lat, h_ctx, w_qkv, q_gain, k_gain, lam_p, subln, w_o, rope, lam_init, need_ctx):
    nq = DIFF_HEADS * 2 * DIFF_HEAD_DIM

    def project(h):
        B, T, _ = h.shape
        qkv = h @ w_qkv
        q = qkv[..., :nq].reshape(B, T, DIFF_HEADS, 2, DIFF_HEAD_DIM)
        k = qkv[..., nq:2 * nq].reshape(B, T, DIFF_HEADS, 2, DIFF_HEAD_DIM)
        v = qkv[..., 2 * nq:].reshape(B, T, DIFF_HEADS, DIFF_V_DIM)
        return rms_norm(q, q_gain), rms_norm(k, k_gain), v

    lp = lam_p.astype(jnp.float32)
    lam = jnp.exp(jnp.sum(lp[0] * lp[1])) - jnp.exp(jnp.sum(lp[2] * lp[3])) + lam_init
    scale = DIFF_HEAD_DIM ** -0.5

    def diff_attend(q, k, v):
        s = jnp.einsum('bqhmd,bkhmd->bhmqk', q, k).astype(jnp.float32) * scale
        p = jax.nn.softmax(s, axis=-1)
        p = p[:, :, 0] - lam * p[:, :, 1]
        return jnp.einsum('bhqk,bkhd->bqhd', p.astype(v.dtype), v)

    def finish(o):
        B, T = o.shape[:2]
        return (rms_norm(o, subln) * (1.0 - lam_init)).reshape(B, T, -1) @ w_o

    q_l, k_l, v_l = project(h_lat)
    q_l, k_l = apply_rope(q_l, *rope), apply_rope(k_l, *rope)
    q_c, k_c, v_c = project(h_ctx)
    k_all = jnp.concatenate([k_c, k_l], axis=1)
    v_all = jnp.concatenate([v_c, v_l], axis=1)
    o_l = finish(sweep_query_blocks(lambda i, qb: diff_attend(qb, k_all, v_all), q_l))
    o_c = finish(diff_attend(q_c, k_c, v_c)) if need_ctx else None
    return o_l, o_c


def swiglu(h, w13, w2):
    a = h @ w13
    g, u = jnp.split(a, 2, axis=-1)
    return (jax.nn.silu(g) * u) @ w2


def moe_swiglu(h, router, w13, w2):
    logits = (h @ router).astype(jnp.float32)
    top_v, top_i = lax.top_k(logits, TOP_K)
    gates = jax.nn.softmax(top_v, axis=-1)
    combine = jnp.sum(jax.nn.one_hot(top_i, N_EXPERTS, dtype=jnp.float32) * gates[..., None], axis=-2)
    out = jnp.zeros_like(h)
    for e in range(N_EXPERTS):
        out = out + combine[..., e:e + 1].astype(h.dtype) * swiglu(h, w13[e], w2[e])
    return out


def channel_mixer(h, i, ffn_w13, ffn_w2, moe_router, moe_w13, moe_w2):
    j = i // 2
    if i % 2 == 0:
        return swiglu(h, ffn_w13[j], ffn_w2[j])
    return moe_swiglu(h, moe_router[j], moe_w13[j], moe_w2[j])


def setup_inputs(seed: int = 0) -> dict:
    key = jax.random.key(seed)
    ks = iter(jax.random.split(key, 40))
    D = D_MODEL

    def nrm(shape, s):
        return jax.random.normal(next(ks), shape, jnp.float32) * s

    def w(shape):
        return nrm(shape, shape[-2] ** -0.5)

    def gain(shape):
        return 1.0 + nrm(shape, 0.02)

    return {
        "x": nrm((BATCH, SEQ, D), 1.0),
        "c": nrm((BATCH, D), 1.0),
        "ctx": nrm((BATCH, CTX_LEN, D), 1.0),
        "c_ctx": nrm((D,), 1.0),
        "ada_w": nrm((DEPTH, D, 6 * D), 0.5 * D ** -0.5),
        "ada_b": nrm((DEPTH, 6 * D), 0.02),
        "norm_mix": gain((DEPTH, D)),
        "norm_ffn": gain((DEPTH, D)),
        "gqa_wqkv": w((N_GQA, D, (GQA_HEADS + 2 * GQA_KV_HEADS) * GQA_HEAD_DIM)),
        "gqa_q_gain": gain((N_GQA, GQA_HEAD_DIM)),
        "gqa_k_gain": gain((N_GQA, GQA_HEAD_DIM)),
        "gqa_wo": w((N_GQA, GQA_HEADS * GQA_HEAD_DIM, D)),
        "mla_wdown": w((N_MLA, D, MLA_Q_LORA + MLA_KV_LORA + MLA_ROPE)),
        "mla_qa_gain": gain((N_MLA, MLA_Q_LORA)),
        "mla_kva_gain": gain((N_MLA, MLA_KV_LORA)),
        "mla_wuq": w((N_MLA, MLA_Q_LORA, MLA_HEADS * (MLA_NOPE + MLA_ROPE))),
        "mla_wukv": w((N_MLA, MLA_KV_LORA, MLA_HEADS * (MLA_NOPE + MLA_V))),
        "mla_q_gain": gain((N_MLA, MLA_NOPE + MLA_ROPE)),
        "mla_k_gain": gain((N_MLA, MLA_NOPE + MLA_ROPE)),
        "mla_wo": w((N_MLA, MLA_HEADS * MLA_V, D)),
        "win_wqkv": w((N_WIN, D, (WIN_HEADS + 2 * WIN_KV_HEADS) * WIN_HEAD_DIM)),
        "win_q_gain": gain((N_WIN, WIN_HEAD_DIM)),
        "win_k_gain": gain((N_WIN, WIN_HEAD_DIM)),
        "win_sink": nrm((N_WIN, WIN_HEADS), 0.5),
        "win_wo": w((N_WIN, WIN_HEADS * WIN_HEAD_DIM, D)),
        "diff_wqkv": w((N_DIFF, D, 2 * DIFF_HEADS * 2 * DIFF_HEAD_DIM + DIFF_HEADS * DIFF_V_DIM)),
        "diff_q_gain": gain((N_DIFF, DIFF_HEAD_DIM)),
        "diff_k_gain": gain((N_DIFF, DIFF_HEAD_DIM)),
        "diff_lambda": nrm((N_DIFF, 4, DIFF_HEAD_DIM), 0.1),
        "diff_subln": gain((N_DIFF, DIFF_V_DIM)),
        "diff_wo": w((N_DIFF, DIFF_HEADS * DIFF_V_DIM, D)),
        "ffn_w13": w((N_DENSE, D, 2 * DENSE_FF)),
        "ffn_w2": w((N_DENSE, DENSE_FF, D)),
        "moe_router": w((N_MOE, D, N_EXPERTS)),
        "moe_w13": w((N_MOE, N_EXPERTS, D, 2 * EXPERT_FF)),
        "moe_w2": w((N_MOE, N_EXPERTS, EXPERT_FF, D)),
    }


def reference(x, c, ctx, c_ctx, ada_w, ada_b, norm_mix, norm_ffn,
              gqa_wqkv, gqa_q_gain, gqa_k_gain, gqa_wo,
              mla_wdown, mla_qa_gain, mla_kva_gain, mla_wuq, mla_wukv, mla_q_gain, mla_k_gain, mla_wo,
              win_wqkv, win_q_gain, win_k_gain, win_sink, win_wo,
              diff_wqkv, diff_q_gain, diff_k_gain, diff_lambda, diff_subln, diff_wo,
              ffn_w13, ffn_w2, moe_router, moe_w13, moe_w2):
    S = x.shape[1]
    rows = S // GRID_W
    rope_gqa = axial_rope_tables(rows, GQA_HEAD_DIM)
    rope_mla = axial_rope_tables(rows, MLA_ROPE)
    rope_win = axial_rope_tables(rows, WIN_HEAD_DIM)
    rope_diff = axial_rope_tables(rows, DIFF_HEAD_DIM)
    silu_c = jax.nn.silu(c)
    silu_cc = jax.nn.silu(c_ctx)

    for i in range(DEPTH):
        need_ctx = i < DEPTH - 1
        j = i // N_MIXERS
        kind = i % N_MIXERS
        mod_l = (silu_c @ ada_w[i] + ada_b[i])[:, None, :]
        mod_c = silu_cc @ ada_w[i] + ada_b[i]
        sh_a, sc_a, g_a, sh_f, sc_f, g_f = jnp.split(mod_l, 6, axis=-1)
        csh_a, csc_a, cg_a, csh_f, csc_f, cg_f = jnp.split(mod_c, 6, axis=-1)

        h_l = modulate(rms_norm(x, norm_mix[i]), sh_a, sc_a)
        h_c = modulate(rms_norm(ctx, norm_mix[i]), csh_a, csc_a)
        if kind == 0:
            o_l, o_c = gqa_axial(h_l, h_c, gqa_wqkv[j], gqa_q_gain[j], gqa_k_gain[j], gqa_wo[j],
                                 rope_gqa, need_ctx)
        elif kind == 1:
            o_l, o_c = mla(h_l, h_c, mla_wdown[j], mla_qa_gain[j], mla_kva_gain[j], mla_wuq[j],
                           mla_wukv[j], mla_q_gain[j], mla_k_gain[j], mla_wo[j], rope_mla, need_ctx)
        elif kind == 2:
            o_l, o_c = window_gqa(h_l, h_c, win_wqkv[j], win_q_gain[j], win_k_gain[j], win_sink[j],
                                  win_wo[j], rope_win, need_ctx)
        else:
            lam_init = 0.8 - 0.6 * math.exp(-0.3 * i)
            o_l, o_c = diff_attention(h_l, h_c, diff_wqkv[j], diff_q_gain[j], diff_k_gain[j],
                                      diff_lambda[j], diff_subln[j], diff_wo[j], rope_diff,
                                      lam_init, need_ctx)
        x = x + g_a * o_l
        if need_ctx:
            ctx = ctx + cg_a * o_c

        h_l = modulate(rms_norm(x, norm_ffn[i]), sh_f, sc_f)
        x = x + g_f * channel_mixer(h_l, i, ffn_w13, ffn_w2, moe_router, moe_w13, moe_w2)
        if need_ctx:
            h_c = modulate(rms_norm(ctx, norm_ffn[i]), csh_f, csc_f)
            ctx = ctx + cg_f * channel_mixer(h_c, i, ffn_w13, ffn_w2, moe_router, moe_w13, moe_w2)
    return x
```

```python
import math
from contextlib import ExitStack
import numpy as np
import ml_dtypes
import concourse.bass as bass
import concourse.mybir as mybir
from concourse.bass_utils import run_bass_kernel_spmd

F32 = mybir.dt.float32
BF16 = mybir.dt.bfloat16
AF = mybir.ActivationFunctionType
ALU = mybir.AluOpType
AX = mybir.AxisListType
PE, ACT, DVE, POOL, SP = "tensor", "scalar", "vector", "gpsimd", "sync"
ENGS = (PE, ACT, DVE, POOL, SP)

D = 1024
T = 2304
NT = 18
CTX = 256
EPS = 1e-6
TBS = [(0, 256)] + [(256 + 512 * i, 512) for i in range(4)]
N_DMA_SEMS = 72


class Buf:
    __slots__ = ("name", "writers", "readers", "dsem", "excl")

    def __init__(self, name, excl=False):
        self.name = name
        self.writers = []
        self.readers = []
        self.dsem = None
        self.excl = excl


class Op:
    __slots__ = ("eng", "fn", "deps", "is_dma", "sig", "sigval", "waits", "id", "dsem_idx")


class Prog:
    def __init__(self):
        self.ops = []
        self.n_dsem = 0
        self.free_dsems = []
        self.live = []
        self.phase_dma = []
        self.last_op = {}
        self.emitted = 0
        self.cnt = {e: 0 for e in ENGS}
        self.seen = {e: {} for e in ENGS}

    def _new(self, eng, fn, is_dma):
        o = Op()
        o.id = len(self.ops)
        o.eng = eng
        o.fn = fn
        o.is_dma = is_dma
        o.sig = False
        o.sigval = None
        o.waits = None
        o.dsem_idx = None
        o.deps = set()
        return o

    def op(self, eng, fn, reads=(), writes=(), joint=False, dma_buf=None):
        o = self._new(eng, fn, dma_buf is not None)
        deps = o.deps
        for b in reads:
            deps.update(b.writers)
            if b.excl:
                deps.update(b.readers)
        for b in writes:
            deps.update(b.readers)
            if not (joint and not b.readers):
                deps.update(b.writers)
        for b in reads:
            self._add(b.readers, o)
        for b in writes:
            if b.readers or not joint:
                b.writers = [o.id]
                b.readers = []
            else:
                self._add(b.writers, o)
        if o.is_dma:
            if dma_buf.dsem is None:
                if self.free_dsems:
                    dma_buf.dsem = self.free_dsems.pop()
                else:
                    dma_buf.dsem = [self.n_dsem, 0]
                    self.n_dsem += 1
                    assert self.n_dsem <= N_DMA_SEMS, "out of DMA semaphores"
                self.live.append(dma_buf)
            dma_buf.dsem[1] += 16
            o.sigval = dma_buf.dsem[1]
            o.dsem_idx = dma_buf.dsem[0]
            self.phase_dma.append(o.id)
        else:
            self.last_op[eng] = o.id
        self.ops.append(o)
        return o

    def _add(self, lst, o):
        if not o.is_dma:
            for i, pid in enumerate(lst):
                p = self.ops[pid]
                if (not p.is_dma) and p.eng == o.eng:
                    lst[i] = o.id
                    return
        lst.append(o.id)

    def barrier(self):
        last = dict(self.last_op)
        dmas = list(self.phase_dma)
        for e in ENGS:
            o = self._new(e, None, False)
            o.deps = set(dmas)
            for e2, oid in last.items():
                if e2 != e:
                    o.deps.add(oid)
            self.ops.append(o)
        self.phase_dma = []
        for b in self.live:
            self.free_dsems.append(b.dsem)
            b.dsem = None
        self.live = []

    def emit(self, block, sems):
        ops = self.ops
        s0 = self.emitted
        new = ops[s0:]
        for o in new:
            o.deps = {d for d in o.deps if d >= s0}
            for d in o.deps:
                p = ops[d]
                if p.is_dma:
                    continue
                if p.eng == PE and o.eng == PE and not o.is_dma:
                    continue
                p.sig = True
        for o in new:
            if o.is_dma:
                continue
            if o.sig:
                self.cnt[o.eng] += 1
                o.sigval = self.cnt[o.eng]
        for o in new:
            w = {}
            for d in o.deps:
                p = ops[d]
                if p.is_dma:
                    key = ("dma", p.dsem_idx)
                else:
                    if p.eng == PE and o.eng == PE and not o.is_dma:
                        continue
                    key = ("eng", p.eng)
                if w.get(key, 0) < p.sigval:
                    w[key] = p.sigval
            s = self.seen[o.eng]
            o.waits = []
            for key, v in w.items():
                if s.get(key, 0) >= v:
                    continue
                s[key] = v
                o.waits.append((key, v))
        per = {e: [] for e in ENGS}
        for o in new:
            per[o.eng].append(o)

        def run(name, eng):
            for o in per[name]:
                for key, v in o.waits:
                    sem = sems["dma"][key[1]] if key[0] == "dma" else sems[key[1]]
                    eng.wait_ge(sem, v)
                if o.fn is None:
                    continue
                ins = o.fn(eng)
                if ins is None:
                    continue
                if o.is_dma:
                    ins.then_inc(sems["dma"][o.dsem_idx], 16)
                elif o.sig:
                    ins.then_inc(sems[o.eng], 1)

        block.tensor(lambda e: run(PE, e))
        block.scalar(lambda e: run(ACT, e))
        block.vector(lambda e: run(DVE, e))
        block.gpsimd(lambda e: run(POOL, e))
        block.sync(lambda e: run(SP, e))
        self.emitted = len(ops)
        for o in new:
            o.fn = None


_uid = [0]


def uname(s):
    _uid[0] += 1
    return f"{s}_{_uid[0]}"


class Ring:
    def __init__(self, es, nc, name, shape, dtype, n, psum=False):
        self.items = []
        for i in range(n):
            mk = nc.psum_tensor if psum else nc.sbuf_tensor
            t = es.enter_context(mk(uname(name), list(shape), dtype))
            self.items.append((t, Buf(name + str(i), excl=psum)))
        self.i = 0

    def next(self):
        it = self.items[self.i % len(self.items)]
        self.i += 1
        return it


def _rope_tables(g):
    half = g // 2
    quarter = g // 4
    inv = (10000.0 ** (-np.arange(quarter, dtype=np.float32) / quarter)).astype(np.float32)
    s = np.arange(2048)
    row = (s // 64).astype(np.float32)
    col = (s % 64).astype(np.float32)
    ang = np.concatenate([row[:, None] * inv[None, :], col[:, None] * inv[None, :]], axis=1).astype(np.float32)
    cos = np.cos(ang).astype(np.float32)
    sin = np.sin(ang).astype(np.float32)
    cosF = np.ones((128, T), np.float32)
    sinF = np.zeros((128, T), np.float32)
    for p in range(128):
        i = p % g
        j = i % half
        cosF[p, CTX:] = cos[:, j]
        sinF[p, CTX:] = -sin[:, j] if i < half else sin[:, j]
    return cosF, sinF


def _consts():
    c = {}
    c["identF"] = np.eye(128, dtype=np.float32)
    ones_bd = np.zeros((128, 128), np.float32)
    ones_bd[:64, :64] = 1
    ones_bd[64:, 64:] = 1
    ones_lo = np.zeros((128, 128), np.float32)
    ones_lo[:, :64] = 1
    ones_hi = np.zeros((128, 128), np.float32)
    ones_hi[:, 64:] = 1
    perm128 = np.zeros((128, 128), np.float32)
    perm64 = np.zeros((128, 128), np.float32)
    for m in range(128):
        perm128[(m + 64) % 128, m] = 1
        i = m % 64
        perm64[(m - i) + (i + 32) % 64, m] = 1
    mats = np.stack([np.ones((128, 128), np.float32), ones_bd, ones_lo, ones_hi, perm128, perm64], axis=1)
    c["matsB"] = mats.astype(ml_dtypes.bfloat16)
    c128, s128 = _rope_tables(128)
    c64, s64 = _rope_tables(64)
    c["rope"] = np.stack([c128, s128, c64, s64], axis=0)
    mask = np.zeros((128, 6, 512), np.float32)
    kp = np.arange(128)[:, None]
    q = np.arange(512)[None, :]
    for rel in range(6):
        mask[:, rel, :] = (np.abs(q - (rel - 1) * 128 - kp) <= 128).astype(np.float32)
    c["wmask"] = mask.astype(ml_dtypes.bfloat16)
    return c


def build(nlayers=4, dbg=False):
    nc = bass.Bass("TRN2", target_bir_lowering=False)
    big = nlayers >= 2

    def din(name, shape, dt=F32):
        return nc.dram_tensor(name, list(shape), dt, kind="ExternalInput").ap()

    x_in = din("x", [2048, D])
    ctx_in = din("ctx", [CTX, D])
    cfm_in = din("cfm", [128, 8, 2])
    ada_w = din("ada_w", [4, D, 6 * D])
    ada_b = din("ada_b", [4, 6 * D])
    norm_mix = din("norm_mix", [4, D])
    norm_ffn = din("norm_ffn", [4, D])
    gqa_wqkv = din("gqa_wqkv", [1, D, 1536])
    gqa_q_gain = din("gqa_q_gain", [1, 128])
    gqa_k_gain = din("gqa_k_gain", [1, 128])
    gqa_wo = din("gqa_wo", [1, D, D])
    mla_wdown = din("mla_wdown", [1, D, 704])
    mla_qa_gain = din("mla_qa_gain", [1, 384])
    mla_kva_gain = din("mla_kva_gain", [1, 256])
    mla_wuq = din("mla_wuq", [1, 384, 1536])
    mla_wukv = din("mla_wukv", [1, 256, 2048])
    mla_q_gain = din("mla_q_gain", [1, 192])
    mla_k_gain = din("mla_k_gain", [1, 192])
    mla_wo = din("mla_wo", [1, D, D])
    win_wqkv = din("win_wqkv", [1, D, 1280])
    win_q_gain = din("win_q_gain", [1, 64])
    win_k_gain = din("win_k_gain", [1, 64])
    win_sink = din("win_sink", [1, 16])
    win_wo = din("win_wo", [1, D, D])
    diff_wqkv = din("diff_wqkv", [1, D, 3072])
    diff_q_gain = din("diff_q_gain", [1, 64])
    diff_k_gain = din("diff_k_gain", [1, 64])
    diff_lambda = din("diff_lambda", [1, 4, 64])
    diff_subln = din("diff_subln", [1, 128])
    diff_wo = din("diff_wo", [1, D, D])
    ffn_w13 = din("ffn_w13", [2, D, 7168])
    ffn_w2 = din("ffn_w2", [2, 3584, D])
    moe_router = din("moe_router", [2, D, 8])
    moe_w13 = din("moe_w13", [2, 8, D, 7168] if big else [1, 1, 8, 8])
    moe_w2 = din("moe_w2", [2, 8, 3584, D] if big else [1, 1, 8, 8])
    identF_in = din("identF", [128, 128])
    matsB_in = din("matsB", [128, 6, 128], BF16)
    rope_in = din("rope", [4, 128, T])
    wmask_in = din("wmask", [128, 6, 512], BF16)
    out_d = nc.dram_tensor("out", [2048, D], F32, kind="ExternalOutput").ap()
    skind = "ExternalOutput" if dbg else "Internal"
    modrows = nc.dram_tensor("modrows", [4, 2, 6 * D], F32, kind=skind).ap()
    Qs = nc.dram_tensor("Qs", [12, 128, T], BF16, kind=skind).ap()
    Ks = nc.dram_tensor("Ks", [9, 128, T], BF16, kind=skind).ap()
    Vs = nc.dram_tensor("Vs", [T, 1024], BF16, kind=skind).ap()
    if dbg:
        dbg_hT = nc.dram_tensor("dbg_hT", [128, 8, T], BF16, kind="ExternalOutput").ap()
        dbg_AO = nc.dram_tensor("dbg_AO", [128, 8, T], BF16, kind="ExternalOutput").ap()
        dbg_xa = nc.dram_tensor("dbg_xa", [128, NT, D], F32, kind="ExternalOutput").ap()
        dbg_modF = nc.dram_tensor("dbg_modF", [128, 4, 2, 48], F32, kind="ExternalOutput").ap()

    P = Prog()
    top = ExitStack()
    sems = {e: top.enter_context(nc.semaphore("s_" + e)) for e in ENGS}
    sems["dma"] = [top.enter_context(nc.semaphore(f"sd{i}")) for i in range(N_DMA_SEMS)]

    def sb(es, name, shape, dt):
        return es.enter_context(nc.sbuf_tensor(uname(name), list(shape), dt))

    x_sb = sb(top, "x_sb", [128, NT, D], F32)
    xb = [Buf(f"x{t}") for t in range(NT)]
    identF = sb(top, "identF", [128, 128], F32)
    matsB = sb(top, "matsB", [128, 6, 128], BF16)
    epsT = sb(top, "epsT", [128, 1], F32)
    modF = sb(top, "modF", [128, 4, 2, 48], F32)
    cst = Buf("consts")
    b_modF = Buf("modF")
    ones128 = matsB[:, 0, :]
    ones_bd = matsB[:, 1, :]
    ones_lo = matsB[:, 2, :]
    ones_hi = matsB[:, 3, :]
    perm128 = matsB[:, 4, :]
    perm64 = matsB[:, 5, :]

    def run_phase(fn):
        with ExitStack() as es:
            fn(es)
            P.barrier()
            with nc.Block() as block:
                P.emit(block, sems)

    def dma(eng, out, in_, reads, writes, sbuf, joint=True, **kw):
        return P.op(eng, lambda e: e.dma_start(out=out, in_=in_, **kw), reads=reads, writes=writes, joint=joint, dma_buf=sbuf)

    def mm(out, lhsT, rhs, start, stop, reads, wbuf):
        return P.op(PE, lambda e: e.matmul(out, lhsT=lhsT, rhs=rhs, start=start, stop=stop), reads=reads, writes=[wbuf], joint=not start)

    def act(out, in_, func, reads, writes, joint=True, **kw):
        return P.op(ACT, lambda e: e.activation(out=out, in_=in_, func=func, **kw), reads=reads, writes=writes, joint=joint)

    def tt(out, in0, in1, op, reads, writes, joint=True, eng=DVE):
        return P.op(eng, lambda e: e.tensor_tensor(out=out, in0=in0, in1=in1, op=op), reads=reads, writes=writes, joint=joint)

    def ts(out, in0, s1, s2, op0, op1, reads, writes, joint=True, eng=DVE):
        if s2 is None:
            return P.op(eng, lambda e: e.tensor_scalar(out=out, in0=in0, scalar1=s1, scalar2=None, op0=op0), reads=reads, writes=writes, joint=joint)
        return P.op(eng, lambda e: e.tensor_scalar(out=out, in0=in0, scalar1=s1, scalar2=s2, op0=op0, op1=op1), reads=reads, writes=writes, joint=joint)

    def stt(out, in0, scalar, in1, op0, op1, reads, writes, joint=True, eng=DVE):
        return P.op(eng, lambda e: e.scalar_tensor_tensor(out=out, in0=in0, scalar=scalar, in1=in1, op0=op0, op1=op1), reads=reads, writes=writes, joint=joint)

    def recip(out, in_, reads, writes, joint=True):
        return P.op(DVE, lambda e: e.reciprocal(out=out, in_=in_), reads=reads, writes=writes, joint=joint)

    def tiles_of(tb):
        t0, n = tb
        return list(range(t0 // 128, (t0 + n) // 128))

    def phase0(es):
        for t in range(2):
            dma(SP, x_sb[:, t, :], ctx_in[t * 128:(t + 1) * 128, :], [], [xb[t]], xb[t])
        for t in range(16):
            dma(SP, x_sb[:, 2 + t, :], x_in[t * 128:(t + 1) * 128, :], [], [xb[2 + t]], xb[2 + t])
        dma(SP, identF[:], identF_in[:, :], [], [cst], cst)
        dma(SP, matsB[:], matsB_in[:, :, :], [], [cst], cst)
        P.op(DVE, lambda e: e.memset(epsT[:], EPS), writes=[cst], joint=True)
        cfm = sb(es, "cfm", [128, 8, 2], F32)
        silu2 = sb(es, "silu2", [128, 8, 2], F32)
        b_c = Buf("cfm")
        b_s = Buf("silu2")
        dma(SP, cfm[:], cfm_in[:, :, :], [], [b_c], b_c)
        act(silu2[:], cfm[:], AF.Silu, [b_c], [b_s])
        ones2 = sb(es, "ones2", [1, 2], F32)
        b_o2 = Buf("ones2")
        P.op(DVE, lambda e: e.memset(ones2[:], 1.0), writes=[b_o2])
        wr = Ring(es, nc, "adaw", [128, 8, 512], F32, 2)
        br = Ring(es, nc, "adab", [1, 512], F32, 2)
        rows = sb(es, "rows", [2, 6 * D], F32)
        b_rows = Buf("rows")
        pr = Ring(es, nc, "p0ps", [128, 512], F32, 2, psum=True)
        pt = Ring(es, nc, "p0pt", [128, 512], F32, 1, psum=True)
        for L in range(nlayers):
            for cb in range(12):
                w, bw = wr.next()
                bt, bbt = br.next()
                dma(SP, w[:], ada_w[L].rearrange("(c p) n -> p c n", p=128)[:, :, cb * 512:(cb + 1) * 512], [], [bw], bw)
                dma(SP, bt[:], ada_b[L:L + 1, cb * 512:(cb + 1) * 512], [], [bbt], bbt)
                ps, bps = pr.next()
                for kc in range(8):
                    mm(ps[0:2, :], silu2[:, kc, :], w[:, kc, :], kc == 0, False, [b_s, bw], bps)
                mm(ps[0:2, :], ones2[:, :], bt[:, :], False, True, [b_o2, bbt], bps)
                P.op(DVE, lambda e, ps=ps, cb=cb: e.tensor_copy(out=rows[:, cb * 512:(cb + 1) * 512], in_=ps[0:2, :]),
                     reads=[bps], writes=[b_rows], joint=True)
            dma(SP, modrows[L], rows[:, :], [b_rows], [], b_rows)
            tp, btp = pt.next()
            for j in range(48):
                P.op(PE, lambda e, j=j, tp=tp: e.transpose(out=tp[:, 2 * j:2 * j + 2], in_=rows[0:2, j * 128:(j + 1) * 128], identity=identF[0:2, 0:2]),
                     reads=[b_rows, cst], writes=[btp], joint=(j > 0))
            P.op(DVE, lambda e, tp=tp, L=L: e.tensor_copy(out=modF[:, L, :, :].rearrange("p r j -> p j r"), in_=tp[:, 0:96].rearrange("p (j r) -> p j r", r=2)),
                 reads=[btp], writes=[b_modF], joint=True)

    def norm_phase(es, L, which, hT, hb, tbs, moe=None):
        gsrc = (norm_mix if which == 0 else norm_ffn)
        gF = sb(es, "gF", [128, 8], F32)
        AB = sb(es, "AB", [128, 2, 2, 8], F32)
        b_g = Buf("gF")
        b_AB = Buf("AB")
        dma(SP, gF[:], gsrc[L].rearrange("(c p) -> p c", p=128), [], [b_g], b_g, allow_slow_non_contiguous=True)
        ish, isc = (0, 1) if which == 0 else (3, 4)
        for r in range(2):
            stt(AB[:, r, 0, :], modF[:, L, r, isc * 8:(isc + 1) * 8], 1.0, gF[:], ALU.add, ALU.mult, [b_modF, b_g], [b_AB])
            P.op(DVE, lambda e, r=r: e.tensor_copy(out=AB[:, r, 1, :], in_=modF[:, L, r, ish * 8:(ish + 1) * 8]), reads=[b_modF], writes=[b_AB], joint=True)
        junk = sb(es, "junk", [128, D], BF16)
        b_junk = Buf("junk")
        ss = sb(es, "ss", [128, NT], F32)
        rstd = sb(es, "rstd", [128, NT], F32)
        b_ss = [Buf(f"ss{i}") for i in range(5)]
        b_rstd = [Buf(f"rstd{i}") for i in range(5)]
        P.op(DVE, lambda e: e.memset(ss[:], 0.0), writes=b_ss)
        xnr = Ring(es, nc, "xn", [128, 4, D], F32, 1)
        ptr = Ring(es, nc, "nps", [128, 512], F32, 2, psum=True)
        if moe is not None:
            h32r = Ring(es, nc, "h32", [128, 8, 512], F32, 1)
            R32 = sb(es, "R32", [128, 8, 8], F32)
            b_R = Buf("R32")
            dma(SP, R32[:], moe["router"].rearrange("(c p) e -> p c e", p=128), [], [b_R], b_R)
            lgr = Ring(es, nc, "lgps", [128, 8], F32, 2, psum=True)
        for bi, tb in enumerate(TBS):
            if tb not in tbs:
                continue
            t0, n = tb
            tl = tiles_of(tb)
            r = 1 if bi == 0 else 0
            for t in tl:
                act(junk[:], x_sb[:, t, :], AF.Square, [xb[t]], [b_junk, b_ss[bi]], joint=False, accum_out=ss[:, t:t + 1])
            act(rstd[:, tl[0]:tl[-1] + 1], ss[:, tl[0]:tl[-1] + 1], AF.Sqrt, [b_ss[bi], cst], [b_rstd[bi]], joint=False, scale=1.0 / D, bias=epsT[:, 0:1])
            recip(rstd[:, tl[0]:tl[-1] + 1], rstd[:, tl[0]:tl[-1] + 1], [b_rstd[bi]], [b_rstd[bi]], joint=False)
            xn, bxn = xnr.next()
            for j, t in enumerate(tl):
                ts(xn[:, j, :], x_sb[:, t, :], rstd[:, t:t + 1], None, ALU.mult, None, [xb[t], b_rstd[bi]], [bxn])
            if moe is not None:
                h32, bh32 = h32r.next()
            for c in range(8):
                ps, bps = ptr.next()
                for j, t in enumerate(tl):
                    P.op(PE, lambda e, ps=ps, j=j, c=c, xn=xn: e.transpose(out=ps[:, j * 128:(j + 1) * 128], in_=xn[:, j, c * 128:(c + 1) * 128], identity=identF[:]),
                         reads=[bxn, cst], writes=[bps], joint=(j > 0))
                act(hT[:, c, t0:t0 + n], ps[:, 0:n], AF.Identity, [bps, b_AB], [hb[bi]], scale=AB[:, r, 0, c:c + 1], bias=AB[:, r, 1, c:c + 1])
                if moe is not None:
                    ts(h32[:, c, 0:n], ps[:, 0:n], AB[:, r, 0, c:c + 1], AB[:, r, 1, c:c + 1], ALU.mult, ALU.add, [bps, b_AB], [bh32])
            if moe is not None:
                for j, t in enumerate(tl):
                    lp, blp = lgr.next()
                    for c in range(8):
                        mm(lp[:, :], h32[:, c, j * 128:(j + 1) * 128], R32[:, c, :], c == 0, c == 7, [bh32, b_R], blp)
                    P.op(DVE, lambda e, lp=lp, t=t: e.tensor_copy(out=moe["lg"][:, t, :], in_=lp[:, :]), reads=[blp], writes=[moe["blg"]], joint=True)

    def route(es, moe, comb, b_comb):
        lg = moe["lg"]
        blg = moe["blg"]
        m8 = sb(es, "m8", [128, NT, 8], F32)
        tmp = sb(es, "rtmp", [128, NT, 8], F32)
        msk = sb(es, "rmsk", [128, NT, 8], F32)
        den = sb(es, "rden", [128, NT], F32)
        b1, b2, b3, b4 = Buf("m8"), Buf("rtmp"), Buf("rmsk"), Buf("rden")
        for t in range(NT):
            P.op(DVE, lambda e, t=t: e.max(out=m8[:, t, :], in_=lg[:, t, :]), reads=[blg], writes=[b1], joint=True)
        tt(msk[:], lg[:], m8[:, :, 1:2].to_broadcast([128, NT, 8]), ALU.is_ge, [blg, b1], [b3])
        tt(tmp[:], lg[:], m8[:, :, 0:1].to_broadcast([128, NT, 8]), ALU.subtract, [blg, b1], [b2])
        act(tmp[:], tmp[:], AF.Exp, [b2], [b2], joint=False)
        tt(tmp[:], tmp[:], msk[:], ALU.mult, [b2, b3], [b2], joint=False)
        P.op(DVE, lambda e: e.reduce_sum(out=den[:], in_=tmp[:], axis=AX.X), reads=[b2], writes=[b4])
        recip(den[:], den[:], [b4], [b4], joint=False)
        tt(comb[:], tmp[:], den[:].unsqueeze(2).to_broadcast([128, NT, 8]), ALU.mult, [b2, b4], [b_comb], joint=False)

    def load_gate_rows(es, L, idx):
        g = sb(es, "grow", [128, 2, D], F32)
        bg = Buf("grow")
        for r in range(2):
            dma(SP, g[:, r, :], modrows[L, r:r + 1, idx * D:(idx + 1) * D].partition_broadcast(128), [], [bg], bg)
        return g, bg

    def ffn_phase(es, L, hT, hb, tbs, experts, comb, b_comb):
        grow, bgrow = load_gate_rows(es, L, 5)
        w13r = Ring(es, nc, "w13", [128, 8, 1024], BF16, 2)
        w2r = Ring(es, nc, "w2", [128, 4, D], BF16, 2)
        y = sb(es, "y", [128, 4, T], BF16)
        yb = [Buf(f"y{i}") for i in range(5)]
        sgr = Ring(es, nc, "sg", [128, 512], BF16, 3)
        tmr = Ring(es, nc, "ftmp", [128, 512], F32, 3)
        gpr = Ring(es, nc, "gps", [128, 512], F32, 2, psum=True)
        upr = Ring(es, nc, "ups", [128, 512], F32, 2, psum=True)
        opr = Ring(es, nc, "ops", [128, 512], F32, 3, psum=True)
        for ei, (w13, w2) in enumerate(experts):
            w13v = w13.rearrange("(c p) n -> p c n", p=128)
            w2v = w2.rearrange("(c p) n -> p c n", p=128)
            for fb in range(7):
                wa, bwa = w13r.next()
                wb_, bwb = w2r.next()
                dma(POOL, wa[:, :, 0:512], w13v[:, :, fb * 512:(fb + 1) * 512], [], [bwa], bwa)
                dma(POOL, wa[:, :, 512:1024], w13v[:, :, 3584 + fb * 512:3584 + (fb + 1) * 512], [], [bwa], bwa)
                dma(POOL, wb_[:], w2v[:, fb * 4:(fb + 1) * 4, :], [], [bwb], bwb)
                for bi, tb in enumerate(TBS):
                    if tb not in tbs:
                        continue
                    t0, n = tb
                    for fc in range(4):
                        gp, bgp = gpr.next()
                        up, bup = upr.next()
                        for kc in range(8):
                            mm(gp[:, 0:n], wa[:, kc, fc * 128:(fc + 1) * 128], hT[:, kc, t0:t0 + n], kc == 0, kc == 7, [bwa, hb[bi]], bgp)
                        for kc in range(8):
                            mm(up[:, 0:n], wa[:, kc, 512 + fc * 128:512 + (fc + 1) * 128], hT[:, kc, t0:t0 + n], kc == 0, kc == 7, [bwa, hb[bi]], bup)
                        sg, bsg = sgr.next()
                        act(sg[:, 0:n], gp[:, 0:n], AF.Silu, [bgp], [bsg], joint=False)
                        tt(y[:, fc, t0:t0 + n], sg[:, 0:n], up[:, 0:n], ALU.mult, [bsg, bup], [yb[bi]])
                for bi, tb in enumerate(TBS):
                    if tb not in tbs:
                        continue
                    r = 1 if bi == 0 else 0
                    for t in tiles_of(tb):
                        for dh in range(2):
                            op_, bop = opr.next()
                            for fc in range(4):
                                mm(op_[:, :], y[:, fc, t * 128:(t + 1) * 128], wb_[:, fc, dh * 512:(dh + 1) * 512], fc == 0, fc == 3, [yb[bi], bwb], bop)
                            tm, btm = tmr.next()
                            tt(tm[:], op_[:, :], grow[:, r, dh * 512:(dh + 1) * 512], ALU.mult, [bop, bgrow], [btm], joint=False)
                            xs = x_sb[:, t, dh * 512:(dh + 1) * 512]
                            if comb is None:
                                tt(xs, tm[:], xs, ALU.add, [btm, xb[t]], [xb[t]], joint=False)
                            else:
                                stt(xs, tm[:], comb[:, t, ei:ei + 1], xs, ALU.mult, ALU.add, [btm, xb[t], b_comb], [xb[t]], joint=False)

    def load_w_chunk(ring, wv, pieces, kc_n):
        w, bw = ring.next()
        off = 0
        for (c0, ncol) in pieces:
            dma(POOL, w[:, 0:kc_n, off:off + ncol], wv[:, :, c0:c0 + ncol], [], [bw], bw)
            off += ncol
        return w, bw

    def gain_tile(es, pieces):
        g = sb(es, "gain", [128, 1], F32)
        bg = Buf("gain")
        off = 0
        for ap in pieces:
            n = ap.shape[0]
            dma(SP, g[off:off + n, :], ap.rearrange("(p o) -> p o", o=1), [], [bg], bg)
            off += n
        return g, bg

    class ProjCtx:
        pass

    def proj_setup(es, need_rope):
        pc = ProjCtx()
        pc.wring = Ring(es, nc, "wch", [128, 8, 128], BF16, 3)
        pc.pps = Ring(es, nc, "pps", [128, 512], F32, 3, psum=True)
        pc.sps = Ring(es, nc, "sps", [128, 512], F32, 1, psum=True)
        pc.rps = Ring(es, nc, "rps", [128, 512], F32, 1, psum=True)
        pc.sq = Ring(es, nc, "sq", [128, 512], BF16, 3)
        pc.qg = Ring(es, nc, "qg", [128, 512], BF16, 3)
        pc.rstd = Ring(es, nc, "prstd", [128, 512], F32, 2)
        pc.t1 = Ring(es, nc, "pt1", [128, 512], F32, 1)
        pc.t2 = Ring(es, nc, "pt2", [128, 512], F32, 1)
        pc.ob = Ring(es, nc, "pob", [128, 512], BF16, 3)
        pc.rope = None
        if need_rope is not None:
            pc.rope = sb(es, "rope", [128, 2, T], F32)
            pc.b_rope = Buf("rope")
            i0 = 0 if need_rope == 128 else 2
            for k in range(2):
                dma(SP, pc.rope[:, k, :], rope_in[i0 + k], [], [pc.b_rope], pc.b_rope)
        return pc

    def proj_group(pc, src, src_bufs, kc_n, chunks, tbs, group, rope, dsts):
        nch = len(chunks)
        for bi, tb in enumerate(TBS):
            if tb not in tbs:
                continue
            t0, n = tb
            pss = []
            for (w, bw, g, bg) in chunks:
                ps, bps = pc.pps.next()
                for kc in range(kc_n):
                    mm(ps[:, 0:n], w[:, kc, :], src[:, kc, t0:t0 + n], kc == 0, kc == kc_n - 1, [bw, src_bufs[bi]], bps)
                pss.append((ps, bps))
            qgs = []
            sp, bsp = pc.sps.next()
            for ci, (ps, bps) in enumerate(pss):
                (w, bw, g, bg) = chunks[ci]
                sq, bsq = pc.sq.next()
                act(sq[:, 0:n], ps[:, 0:n], AF.Square, [bps], [bsq], joint=False)
                qg, bqg = pc.qg.next()
                ts(qg[:, 0:n], ps[:, 0:n], g[:, 0:1], None, ALU.mult, None, [bps, bg], [bqg], joint=False)
                qgs.append((qg, bqg))
                onesm = ones_bd if group == 64 else ones128
                if group == "all":
                    mm(sp[:, 0:n], onesm, sq[:, 0:n], ci == 0, ci == nch - 1, [bsq, cst], bsp)
                else:
                    assert nch == 1
                    mm(sp[:, 0:n], onesm, sq[:, 0:n], True, True, [bsq, cst], bsp)
            cnt = {128: 128, 64: 64, "all": 128 * nch}[group]
            rs, brs = pc.rstd.next()
            act(rs[:, 0:n], sp[:, 0:n], AF.Sqrt, [bsp, cst], [brs], joint=False, scale=1.0 / cnt, bias=epsT[:, 0:1])
            recip(rs[:, 0:n], rs[:, 0:n], [brs], [brs], joint=False)
            for ci, (qg, bqg) in enumerate(qgs):
                ob, bob = pc.ob.next()
                if rope is not None:
                    rp, brp = pc.rps.next()
                    mm(rp[:, 0:n], perm128 if rope == 128 else perm64, qg[:, 0:n], True, True, [bqg, cst], brp)
                    t1, bt1 = pc.t1.next()
                    t2, bt2 = pc.t2.next()
                    tt(t1[:, 0:n], qg[:, 0:n], pc.rope[:, 0, t0:t0 + n], ALU.mult, [bqg, pc.b_rope], [bt1], joint=False)
                    tt(t2[:, 0:n], rp[:, 0:n], pc.rope[:, 1, t0:t0 + n], ALU.mult, [brp, pc.b_rope], [bt2], joint=False)
                    tt(t1[:, 0:n], t1[:, 0:n], t2[:, 0:n], ALU.add, [bt1, bt2], [bt1], joint=False)
                    tt(ob[:, 0:n], t1[:, 0:n], rs[:, 0:n], ALU.mult, [bt1, brs], [bob], joint=False)
                else:
                    tt(ob[:, 0:n], qg[:, 0:n], rs[:, 0:n], ALU.mult, [bqg, brs], [bob], joint=False)
                d = dsts[ci]
                if d[0] == "dram":
                    dma(SP, d[1][:, t0:t0 + n], ob[:, 0:n], [bob], [], bob)
                else:
                    P.op(POOL, lambda e, d=d, ob=ob, t0=t0, n=n: e.tensor_copy(out=d[1][:, t0:t0 + n], in_=ob[:, 0:n]),
                         reads=[bob], writes=[d[2][bi]], joint=True)

    def proj_v(es, pc, src, src_bufs, kc_n, wv, pieces, tbs):
        F = sum(p[1] for p in pieces)
        wt = sb(es, "wv", [128, kc_n, F], BF16)
        bwt = Buf("wv")
        off = 0
        for (c0, ncol) in pieces:
            dma(POOL, wt[:, :, off:off + ncol], wv[:, :, c0:c0 + ncol], [], [bwt], bwt)
            off += ncol
        vps = pc.pps
        vob = Ring(es, nc, "vob", [128, 1024], BF16, 1)
        for bi, tb in enumerate(TBS):
            if tb not in tbs:
                continue
            for t in tiles_of(tb):
                vo, bvo = vob.next()
                for f0 in range(0, F, 512):
                    fn_ = min(512, F - f0)
                    ps, bps = vps.next()
                    for kc in range(kc_n):
                        mm(ps[:, 0:fn_], src[:, kc, t * 128:(t + 1) * 128], wt[:, kc, f0:f0 + fn_], kc == 0, kc == kc_n - 1, [src_bufs[bi], bwt], bps)
                    P.op(DVE, lambda e, vo=vo, ps=ps, f0=f0, fn_=fn_: e.tensor_copy(out=vo[:, f0:f0 + fn_], in_=ps[:, 0:fn_]),
                         reads=[bps], writes=[bvo], joint=True)
                dma(SP, Vs[t * 128:(t + 1) * 128, 0:F], vo[:, 0:F], [bvo], [], bvo)

    def attn_phase(es, AO, aob, units, scale, masks=None):
        kq = {}
        ldr = Ring(es, nc, "akq", [128, T], BF16, 8)
        vr = Ring(es, nc, "av", [128, NT, 128], BF16, 4)
        spr = Ring(es, nc, "asps", [128, 512], F32, 2, psum=True)
        opr = [Ring(es, nc, "aops", [128, 512], F32, 1, psum=True) for _ in range(2)]
        lpr = [Ring(es, nc, "alps", [128, 512], F32, 1, psum=True) for _ in range(2)]
        fpr = Ring(es, nc, "afps", [128, 512], F32, 1, psum=True)
        ptr = Ring(es, nc, "apt", [128, 512], BF16, 4)
        ftm = [Ring(es, nc, f"aft{i}", [128, 512], F32, 2) for i in range(3)]
        fsq = Ring(es, nc, "afsq", [128, 512], BF16, 2)
        if masks is not None:
            wm = sb(es, "wm", [128, 6, 512], BF16)
            b_wm = Buf("wm")
            dma(SP, wm[:], wmask_in[:, :, :], [], [b_wm], b_wm)
        cache = {}
        for u in units:
            tl = {}
            for (key, ap) in u["loads"]:
                if key in cache:
                    tl[key] = cache[key]
                    continue
                tle, btl = ldr.next()
                dma(SP, tle[:], ap, [], [btl], btl, joint=False)
                tl[key] = (tle, btl)
                cache = {k: v for k, v in cache.items() if v[0] is not tle}
                cache[key] = (tle, btl)
            for (key, c0, ncol, pad) in u["vloads"]:
                if key in cache:
                    tl[key] = cache[key]
                    continue
                tle, btl = vr.next()
                if ncol < 128:
                    P.op(DVE, lambda e, tle=tle: e.memset(tle[:], 0.0), writes=[btl])
                dma(SP, tle[:, :, pad:pad + ncol], Vs[:, c0:c0 + ncol].rearrange("(t p) f -> p t f", p=128), [], [btl], btl, joint=False)
                cache = {k: v for k, v in cache.items() if v[0] is not tle}
                cache[key] = (tle, btl)
                tl[key] = (tle, btl)
            for (q0, qn, ktiles_fn) in u["qblocks"]:
                accs = sorted(set(s["acc"] for s in u["subs"]))
                O = {a: opr[a].next() for a in accs}
                Lp = {a: lpr[a].next() for a in accs}
                first = {a: True for a in accs}
                work = []
                for s in u["subs"]:
                    for kt in ktiles_fn():
                        work.append((s, kt))
                lastidx = {}
                for i, (s, kt) in enumerate(work):
                    lastidx[s["acc"]] = i
                for i, (s, kt) in enumerate(work):
                    a = s["acc"]
                    sp, bsp = spr.next()
                    ktile = kt[0]
                    npairs = len(s["pairs"])
                    for pi, (kkey, qkey, r0, nr) in enumerate(s["pairs"]):
                        kt_, bk = tl[kkey]
                        qt_, bq = tl[qkey]
                        mm(sp[:, 0:qn], kt_[r0:r0 + nr, ktile * 128:(ktile + 1) * 128], qt_[r0:r0 + nr, q0:q0 + qn], pi == 0, pi == npairs - 1, [bk, bq], bsp)
                    pt_, bpt = ptr.next()
                    act(pt_[:, 0:qn], sp[:, 0:qn], AF.Exp, [bsp], [bpt], joint=False, scale=scale)
                    if kt[1] is not None:
                        tt(pt_[:, 0:qn], pt_[:, 0:qn], wm[:, kt[1], 0:qn], ALU.mult, [bpt, b_wm], [bpt], joint=False)
                    vt, bv = tl[s["v"]]
                    mm(O[a][0][:, 0:qn], vt[:, ktile, :], pt_[:, 0:qn], first[a], i == lastidx[a], [bv, bpt], O[a][1])
                    mm(Lp[a][0][:, 0:qn], s["ones"], pt_[:, 0:qn], first[a], i == lastidx[a], [cst, bpt], Lp[a][1])
                    first[a] = False
                u["fin"](u, q0, qn, O, Lp, dict(ftm=ftm, fsq=fsq, fpr=fpr))

    def fin_default(extra=None):
        def fin(u, q0, qn, O, Lp, R):
            r, br = R["ftm"][0].next()
            if extra is not None:
                es_t, bes = extra
                ts(r[:, 0:qn], Lp[0][0][:, 0:qn], es_t[:, u["c"]:u["c"] + 1], None, ALU.add, None, [Lp[0][1], bes], [br], joint=False)
                recip(r[:, 0:qn], r[:, 0:qn], [br], [br], joint=False)
            else:
                recip(r[:, 0:qn], Lp[0][0][:, 0:qn], [Lp[0][1]], [br], joint=False)
            tt(u["AO"][:, u["c"], q0:q0 + qn], O[0][0][:, 0:qn], r[:, 0:qn], ALU.mult, [O[0][1], br], [u["aob"]], joint=True)
        return fin

    def all_k():
        return [(k, None) for k in range(NT)]

    def ctx_k():
        return [(0, None), (1, None)]

    LAT_QB = [(256 + 512 * i, 512, all_k) for i in range(4)]
    CTX_QB = [(0, 256, ctx_k)]

    def std_layer(L):
        kind = L % 4
        need_ctx = L < 3
        moe = (L % 2 == 1)
        tbs_all = list(TBS)
        tbs_lat = TBS[1:]
        hT = None
        state = {}

        def phA(es):
            hT = sb(es, "hT", [128, 8, T], BF16)
            hb = [Buf(f"hT{i}") for i in range(5)]
            norm_phase(es, L, 0, hT, hb, tbs_all)
            if kind == 0:
                wv = gqa_wqkv[0].rearrange("(c p) n -> p c n", p=128)
                pc = proj_setup(es, 128)
                gq, bgq = gain_tile(es, [gqa_q_gain[0]])
                gk, bgk = gain_tile(es, [gqa_k_gain[0]])
                for h in range(8):
                    w, bw = load_w_chunk(pc.wring, wv, [(h * 128, 128)], 8)
                    proj_group(pc, hT, hb, 8, [(w, bw, gq, bgq)], tbs_all, 128, 128, [("dram", Qs[h])])
                for kvh in range(2):
                    w, bw = load_w_chunk(pc.wring, wv, [(1024 + kvh * 128, 128)], 8)
                    proj_group(pc, hT, hb, 8, [(w, bw, gk, bgk)], tbs_all, 128, 128, [("dram", Ks[kvh])])
                proj_v(es, pc, hT, hb, 8, wv, [(1280, 256)], tbs_all)
            elif kind == 2:
                wv = win_wqkv[0].rearrange("(c p) n -> p c n", p=128)
                pc = proj_setup(es, 64)
                gq, bgq = gain_tile(es, [win_q_gain[0], win_q_gain[0]])
                gk, bgk = gain_tile(es, [win_k_gain[0], win_k_gain[0]])
                for c in range(8):
                    w, bw = load_w_chunk(pc.wring, wv, [(c * 128, 128)], 8)
                    proj_group(pc, hT, hb, 8, [(w, bw, gq, bgq)], tbs_all, 64, 64, [("dram", Qs[c])])
                for kvh in range(2):
                    w, bw = load_w_chunk(pc.wring, wv, [(1024 + kvh * 64, 64), (1024 + kvh * 64, 64)], 8)
                    proj_group(pc, hT, hb, 8, [(w, bw, gk, bgk)], tbs_all, 64, 64, [("dram", Ks[kvh])])
                proj_v(es, pc, hT, hb, 8, wv, [(1152, 128)], tbs_all)
            elif kind == 3:
                wv = diff_wqkv[0].rearrange("(c p) n -> p c n", p=128)
                pc = proj_setup(es, 64)
                gq, bgq = gain_tile(es, [diff_q_gain[0], diff_q_gain[0]])
                gk, bgk = gain_tile(es, [diff_k_gain[0], diff_k_gain[0]])
                for h in range(8):
                    w, bw = load_w_chunk(pc.wring, wv, [(h * 128, 128)], 8)
                    proj_group(pc, hT, hb, 8, [(w, bw, gq, bgq)], tbs_lat, 64, 64, [("dram", Qs[h])])
                for h in range(8):
                    w, bw = load_w_chunk(pc.wring, wv, [(1024 + h * 128, 128)], 8)
                    proj_group(pc, hT, hb, 8, [(w, bw, gk, bgk)], tbs_all, 64, 64, [("dram", Ks[h])])
                proj_v(es, pc, hT, hb, 8, wv, [(2048, 1024)], tbs_all)
            else:
                wd = mla_wdown[0].rearrange("(c p) n -> p c n", p=128)
                pc = proj_setup(es, 64)
                cqn = sb(es, "cqn", [128, 3, T], BF16)
                ckvn = sb(es, "ckvn", [128, 2, T], BF16)
                bcq = [Buf(f"cqn{i}") for i in range(5)]
                bckv = [Buf(f"ckvn{i}") for i in range(5)]
                ch = []
                for c in range(3):
                    w, bw = load_w_chunk(pc.wring, wd, [(c * 128, 128)], 8)
                    g, bg = gain_tile(es, [mla_qa_gain[0, c * 128:(c + 1) * 128]])
                    ch.append((w, bw, g, bg))
                proj_group(pc, hT, hb, 8, ch, tbs_all, "all", None, [("sbuf", cqn[:, c, :], bcq) for c in range(3)])
                ch = []
                for c in range(2):
                    w, bw = load_w_chunk(pc.wring, wd, [(384 + c * 128, 128)], 8)
                    g, bg = gain_tile(es, [mla_kva_gain[0, c * 128:(c + 1) * 128]])
                    ch.append((w, bw, g, bg))
                proj_group(pc, hT, hb, 8, ch, tbs_all, "all", None, [("sbuf", ckvn[:, c, :], bckv) for c in range(2)])
                w, bw = load_w_chunk(pc.wring, wd, [(640, 64), (640, 64)], 8)
                g, bg = gain_tile(es, [mla_k_gain[0, 128:192], mla_k_gain[0, 128:192]])
                proj_group(pc, hT, hb, 8, [(w, bw, g, bg)], tbs_all, 64, 64, [("dram", Ks[8])])
                wq = mla_wuq[0].rearrange("(c p) n -> p c n", p=128)
                wkv = mla_wukv[0].rearrange("(c p) n -> p c n", p=128)
                gqn, bgqn = gain_tile(es, [mla_q_gain[0, 0:128]])
                gqp, bgqp = gain_tile(es, [mla_q_gain[0, 128:192], mla_q_gain[0, 128:192]])
                gkn, bgkn = gain_tile(es, [mla_k_gain[0, 0:128]])
                for h in range(8):
                    w, bw = load_w_chunk(pc.wring, wq, [(h * 192, 128)], 3)
                    proj_group(pc, cqn, bcq, 3, [(w, bw, gqn, bgqn)], tbs_all, 128, None, [("dram", Qs[h])])
                for c in range(4):
                    w, bw = load_w_chunk(pc.wring, wq, [(2 * c * 192 + 128, 64), ((2 * c + 1) * 192 + 128, 64)], 3)
                    proj_group(pc, cqn, bcq, 3, [(w, bw, gqp, bgqp)], tbs_all, 64, 64, [("dram", Qs[8 + c])])
                for h in range(8):
                    w, bw = load_w_chunk(pc.wring, wkv, [(h * 256, 128)], 2)
                    proj_group(pc, ckvn, bckv, 2, [(w, bw, gkn, bgkn)], tbs_all, 128, None, [("dram", Ks[h])])
                proj_v(es, pc, ckvn, bckv, 2, wkv, [(h * 256 + 128, 128) for h in range(8)], tbs_all)

            if dbg and L == nlayers - 1:
                bd = Buf("dbgh")
                dma(SP, dbg_hT[:, :, :], hT[:], hb, [], bd)
                bd2 = Buf("dbgm")
                dma(SP, dbg_modF[:, :, :, :], modF[:], [b_modF], [], bd2)

        run_phase(phA)

        def phBC(es0):
            AO = sb(es0, "AO", [128, 8, T], BF16)
            aob = Buf("AO")

            def phB(es):
                qbs = (CTX_QB if need_ctx else []) + LAT_QB
                units = []
                if kind == 0:
                    scale = 128 ** -0.5
                    for h in range(8):
                        kvh = h // 4
                        units.append(dict(c=h, loads=[(("q", h), Qs[h]), (("k", kvh), Ks[kvh])], vloads=[(("v", kvh), kvh * 128, 128, 0)],
                                          subs=[dict(pairs=[(("k", kvh), ("q", h), 0, 128)], v=("v", kvh), ones=ones128, acc=0)],
                                          qblocks=qbs, fin=fin_default(), AO=AO, aob=aob))
                    attn_phase(es, AO, aob, units, scale)
                elif kind == 1:
                    scale = 192 ** -0.5
                    for h in range(8):
                        r0 = (h % 2) * 64
                        units.append(dict(c=h, loads=[(("q", h), Qs[h]), (("qp", h // 2), Qs[8 + h // 2]), (("k", h), Ks[h]), (("kp", 0), Ks[8])],
                                          vloads=[(("v", h), h * 128, 128, 0)],
                                          subs=[dict(pairs=[(("k", h), ("q", h), 0, 128), (("kp", 0), ("qp", h // 2), r0, 64)], v=("v", h), ones=ones128, acc=0)],
                                          qblocks=qbs, fin=fin_default(), AO=AO, aob=aob))
                    attn_phase(es, AO, aob, units, scale)
                elif kind == 2:
                    scale = 64 ** -0.5
                    esk = sb(es, "esk", [128, 8], F32)
                    besk = Buf("esk")
                    sv = win_sink[0].rearrange("(c two) -> two c", two=2)
                    dma(SP, esk[0:64, :], sv[0:1, :].partition_broadcast(64), [], [besk], besk, allow_slow_non_contiguous=True)
                    dma(SP, esk[64:128, :], sv[1:2, :].partition_broadcast(64), [], [besk], besk, allow_slow_non_contiguous=True)
                    act(esk[:], esk[:], AF.Exp, [besk], [besk], joint=False)

                    def mk_kfn(qb):
                        def kfn():
                            i0 = 4 * qb
                            res = [(0, None), (1, None)]
                            for j in range(max(0, i0 - 1), min(15, i0 + 4) + 1):
                                res.append((2 + j, j - i0 + 1))
                            return res
                        return kfn
                    wqbs = (CTX_QB if need_ctx else []) + [(256 + 512 * i, 512, mk_kfn(i)) for i in range(4)]
                    for c in range(8):
                        kvh = c // 4
                        subs = []
                        for half in range(2):
                            subs.append(dict(pairs=[(("k", kvh), ("q", c), half * 64, 64)], v=("v", kvh, half), ones=(ones_lo if half == 0 else ones_hi), acc=0))
                        units.append(dict(c=c, loads=[(("q", c), Qs[c]), (("k", kvh), Ks[kvh])],
                                          vloads=[(("v", kvh, 0), kvh * 64, 64, 0), (("v", kvh, 1), kvh * 64, 64, 64)],
                                          subs=subs, qblocks=wqbs, fin=fin_default((esk, besk)), AO=AO, aob=aob))
                    attn_phase(es, AO, aob, units, scale, masks=True)
                else:
                    scale = 64 ** -0.5
                    lam_init = 0.8 - 0.6 * math.exp(-0.3 * L)
                    lp = sb(es, "lp", [128, 4, 64], F32)
                    blp = Buf("lp")
                    dma(SP, lp[:], diff_lambda[0:1].partition_broadcast(128), [], [blp], blp)
                    lpp = sb(es, "lpp", [128, 2, 64], F32)
                    lsum = sb(es, "lsum", [128, 2], F32)
                    nlam = sb(es, "nlam", [128, 1], F32)
                    bl2 = Buf("lpp")
                    bl3 = Buf("lsum")
                    bnl = Buf("nlam")
                    tt(lpp[:, 0, :], lp[:, 0, :], lp[:, 1, :], ALU.mult, [blp], [bl2])
                    tt(lpp[:, 1, :], lp[:, 2, :], lp[:, 3, :], ALU.mult, [blp], [bl2])
                    P.op(DVE, lambda e: e.reduce_sum(out=lsum[:], in_=lpp[:], axis=AX.X), reads=[bl2], writes=[bl3])
                    act(lsum[:], lsum[:], AF.Exp, [bl3], [bl3], joint=False)
                    tt(nlam[:], lsum[:, 1:2], lsum[:, 0:1], ALU.subtract, [bl3], [bnl], joint=False)
                    ts(nlam[:], nlam[:], -lam_init, None, ALU.add, None, [bnl], [bnl], joint=False)
                    sg, bsg = gain_tile(es, [diff_subln[0]])
                    ts(sg[:], sg[:], 1.0 - lam_init, None, ALU.mult, None, [bsg], [bsg], joint=False)

                    def fin_diff(u, q0, qn, O, Lp, R):
                        r0_, br0 = R["ftm"][0].next()
                        r1_, br1 = R["ftm"][1].next()
                        o_, bo = R["ftm"][2].next()
                        recip(r0_[:, 0:qn], Lp[0][0][:, 0:qn], [Lp[0][1]], [br0], joint=False)
                        recip(r1_[:, 0:qn], Lp[1][0][:, 0:qn], [Lp[1][1]], [br1], joint=False)
                        tt(r0_[:, 0:qn], O[0][0][:, 0:qn], r0_[:, 0:qn], ALU.mult, [O[0][1], br0], [br0], joint=False)
                        tt(r1_[:, 0:qn], O[1][0][:, 0:qn], r1_[:, 0:qn], ALU.mult, [O[1][1], br1], [br1], joint=False)
                        stt(o_[:, 0:qn], r1_[:, 0:qn], nlam[:, 0:1], r0_[:, 0:qn], ALU.mult, ALU.add, [br0, br1, bnl], [bo], joint=False)
                        sq, bsq = R["fsq"].next()
                        act(sq[:, 0:qn], o_[:, 0:qn], AF.Square, [bo], [bsq], joint=False)
                        fp, bfp = R["fpr"].next()
                        mm(fp[:, 0:qn], ones128, sq[:, 0:qn], True, True, [bsq, cst], bfp)
                        act(r0_[:, 0:qn], fp[:, 0:qn], AF.Sqrt, [bfp, cst], [br0], joint=False, scale=1.0 / 128, bias=epsT[:, 0:1])
                        recip(r0_[:, 0:qn], r0_[:, 0:qn], [br0], [br0], joint=False)
                        stt(u["AO"][:, u["c"], q0:q0 + qn], o_[:, 0:qn], sg[:, 0:1], r0_[:, 0:qn], ALU.mult, ALU.mult, [bo, br0, bsg], [u["aob"]], joint=True)

                    for h in range(8):
                        subs = [dict(pairs=[(("k", h), ("q", h), m * 64, 64)], v=("v", h), ones=ones128, acc=m) for m in range(2)]
                        units.append(dict(c=h, loads=[(("q", h), Qs[h]), (("k", h), Ks[h])], vloads=[(("v", h), h * 128, 128, 0)],
                                          subs=subs, qblocks=qbs, fin=fin_diff, AO=AO, aob=aob))
                    attn_phase(es, AO, aob, units, scale)

            run_phase(phB)
            if dbg and L == nlayers - 1:
                def phBd(es):
                    bd = Buf("dbga")
                    dma(SP, dbg_AO[:, :, :], AO[:], [aob], [], bd)
                run_phase(phBd)

            def phC(es):
                wo_d = [gqa_wo, mla_wo, win_wo, diff_wo][kind][0].rearrange("(c p) n -> p c n", p=128)
                wo = sb(es, "wo", [128, 8, D], BF16)
                bwo = Buf("wo")
                for hh in range(2):
                    dma(POOL, wo[:, hh * 4:(hh + 1) * 4, :], wo_d[:, hh * 4:(hh + 1) * 4, :], [], [bwo], bwo)
                grow, bgrow = load_gate_rows(es, L, 2)
                opr = Ring(es, nc, "cps", [128, 512], F32, 4, psum=True)
                tmr = Ring(es, nc, "ctmp", [128, 512], F32, 3)
                tl = list(range(NT)) if need_ctx else list(range(2, NT))
                for t in tl:
                    r = 1 if t < 2 else 0
                    for dh in range(2):
                        ps, bps = opr.next()
                        for c in range(8):
                            mm(ps[:, :], AO[:, c, t * 128:(t + 1) * 128], wo[:, c, dh * 512:(dh + 1) * 512], c == 0, c == 7, [aob, bwo], bps)
                        tm, btm = tmr.next()
                        tt(tm[:], ps[:, :], grow[:, r, dh * 512:(dh + 1) * 512], ALU.mult, [bps, bgrow], [btm], joint=False)
                        xs = x_sb[:, t, dh * 512:(dh + 1) * 512]
                        tt(xs, tm[:], xs, ALU.add, [btm, xb[t]], [xb[t]], joint=False)
            run_phase(phC)
            if dbg and L == nlayers - 1:
                def phCd(es):
                    for t in range(NT):
                        dma(SP, dbg_xa[:, t, :], x_sb[:, t, :], [xb[t]], [], xb[t])
                run_phase(phCd)

        with ExitStack() as es0:
            phBC(es0)

        tbs_f = tbs_all if need_ctx else tbs_lat
        with ExitStack() as es0:
            hT = sb(es0, "hTf", [128, 8, T], BF16)
            hb = [Buf(f"hTf{i}") for i in range(5)]
            comb = sb(es0, "comb", [128, NT, 8], F32)
            b_comb = Buf("comb")

            def phD(es):
                if moe:
                    lg = sb(es, "lg", [128, NT, 8], F32)
                    blg = Buf("lg")
                    P.op(DVE, lambda e: e.memset(lg[:], 0.0), writes=[blg])
                    m = dict(router=moe_router[L // 2], lg=lg, blg=blg)
                    norm_phase(es, L, 1, hT, hb, tbs_f, moe=m)
                    route(es, m, comb, b_comb)
                else:
                    norm_phase(es, L, 1, hT, hb, tbs_f)
            run_phase(phD)

            def phE(es):
                if moe:
                    experts = [(moe_w13[L // 2, e], moe_w2[L // 2, e]) for e in range(8)]
                    ffn_phase(es, L, hT, hb, tbs_f, experts, comb, b_comb)
                else:
                    ffn_phase(es, L, hT, hb, tbs_f, [(ffn_w13[L // 2], ffn_w2[L // 2])], None, None)
            run_phase(phE)

    run_phase(phase0)
    for L in range(nlayers):
        std_layer(L)

    def phase_out(es):
        for t in range(16):
            dma(SP, out_d[t * 128:(t + 1) * 128, :], x_sb[:, 2 + t, :], [xb[2 + t]], [], xb[2 + t])
    run_phase(phase_out)
    top.close()
    return nc


_CACHE = {}


def kernel(**inputs):
    nl = int(inputs.pop("_nlayers", 4))
    dbg = bool(inputs.pop("_dbg", False))
    ncores = int(inputs.pop("_ncores", 8))
    if (nl, dbg) not in _CACHE:
        _CACHE[(nl, dbg)] = build(nl, dbg)
    nc = _CACHE[(nl, dbg)]
    consts = _consts()
    shared = {}
    for k in ["ada_w", "ada_b", "norm_mix", "norm_ffn", "gqa_wqkv", "gqa_q_gain", "gqa_k_gain", "gqa_wo", "mla_wdown",
              "mla_qa_gain", "mla_kva_gain", "mla_wuq", "mla_wukv", "mla_q_gain", "mla_k_gain", "mla_wo", "win_wqkv",
              "win_q_gain", "win_k_gain", "win_sink", "win_wo", "diff_wqkv", "diff_q_gain", "diff_k_gain", "diff_lambda",
              "diff_subln", "diff_wo", "ffn_w13", "ffn_w2", "moe_router", "moe_w13", "moe_w2"]:
        shared[k] = np.ascontiguousarray(np.asarray(inputs[k], dtype=np.float32))
        if nl < 2 and k in ("moe_w13", "moe_w2"):
            shared[k] = np.zeros((1, 1, 8, 8), np.float32)
    shared.update(consts)
    x = np.asarray(inputs["x"], dtype=np.float32)
    c = np.asarray(inputs["c"], dtype=np.float32)
    ctx = np.asarray(inputs["ctx"], dtype=np.float32)
    c_ctx = np.asarray(inputs["c_ctx"], dtype=np.float32)
    in_maps = []
    for b in range(ncores):
        m = dict(shared)
        m["x"] = np.ascontiguousarray(x[b])
        m["ctx"] = np.ascontiguousarray(ctx[b])
        cfm = np.stack([c[b].reshape(8, 128).T, c_ctx.reshape(8, 128).T], axis=-1)
        m["cfm"] = np.ascontiguousarray(cfm.astype(np.float32))
        in_maps.append(m)
    res = run_bass_kernel_spmd(nc, in_maps, core_ids=list(range(ncores)))
    if dbg:
        return res.results
    return np.stack([r["out"] for r in res.results], axis=0).astype(np.float32)
```

```python
import math
from contextlib import ExitStack
import numpy as np
import ml_dtypes
import concourse.bass as bass
import concourse.mybir as mybir
from concourse.bass_utils import run_bass_kernel_spmd

F32 = mybir.dt.float32
BF16 = mybir.dt.bfloat16
AF = mybir.ActivationFunctionType
ALU = mybir.AluOpType
AX = mybir.AxisListType
PE, ACT, DVE, POOL, SP = "tensor", "scalar", "vector", "gpsimd", "sync"
ENGS = (PE, ACT, DVE, POOL, SP)

D = 1024
T = 2304
NT = 18
CTX = 256
EPS = 1e-6
TBS = [(0, 256)] + [(256 + 512 * i, 512) for i in range(4)]
N_DMA_SEMS = 72


class Buf:
    __slots__ = ("name", "writers", "readers", "dsem", "excl")

    def __init__(self, name, excl=False):
        self.name = name
        self.writers = []
        self.readers = []
        self.dsem = None
        self.excl = excl


class Op:
    __slots__ = ("eng", "fn", "deps", "is_dma", "sig", "sigval", "waits", "id", "dsem_idx")


class Prog:
    def __init__(self):
        self.ops = []
        self.n_dsem = 0
        self.free_dsems = []
        self.live = []
        self.phase_dma = []
        self.last_op = {}
        self.emitted = 0
        self.cnt = {e: 0 for e in ENGS}
        self.seen = {e: {} for e in ENGS}

    def _new(self, eng, fn, is_dma):
        o = Op()
        o.id = len(self.ops)
        o.eng = eng
        o.fn = fn
        o.is_dma = is_dma
        o.sig = False
        o.sigval = None
        o.waits = None
        o.dsem_idx = None
        o.deps = set()
        return o

    def op(self, eng, fn, reads=(), writes=(), joint=False, dma_buf=None):
        o = self._new(eng, fn, dma_buf is not None)
        deps = o.deps
        for b in reads:
            deps.update(b.writers)
            if b.excl:
                deps.update(b.readers)
        for b in writes:
            deps.update(b.readers)
            if not (joint and not b.readers):
                deps.update(b.writers)
        for b in reads:
            self._add(b.readers, o)
        for b in writes:
            if b.readers or not joint:
                b.writers = [o.id]
                b.readers = []
            else:
                self._add(b.writers, o)
        if o.is_dma:
            if dma_buf.dsem is None:
                if self.free_dsems:
                    dma_buf.dsem = self.free_dsems.pop()
                else:
                    dma_buf.dsem = [self.n_dsem, 0]
                    self.n_dsem += 1
                    assert self.n_dsem <= N_DMA_SEMS, "out of DMA semaphores"
                self.live.append(dma_buf)
            dma_buf.dsem[1] += 16
            o.sigval = dma_buf.dsem[1]
            o.dsem_idx = dma_buf.dsem[0]
            self.phase_dma.append(o.id)
        else:
            self.last_op[eng] = o.id
        self.ops.append(o)
        return o

    def _add(self, lst, o):
        if not o.is_dma:
            for i, pid in enumerate(lst):
                p = self.ops[pid]
                if (not p.is_dma) and p.eng == o.eng:
                    lst[i] = o.id
                    return
        lst.append(o.id)

    def barrier(self):
        last = dict(self.last_op)
        dmas = list(self.phase_dma)
        for e in ENGS:
            o = self._new(e, None, False)
            o.deps = set(dmas)
            for e2, oid in last.items():
                if e2 != e:
                    o.deps.add(oid)
            self.ops.append(o)
        self.phase_dma = []
        for b in self.live:
            self.free_dsems.append(b.dsem)
            b.dsem = None
        self.live = []

    def emit(self, block, sems):
        ops = self.ops
        s0 = self.emitted
        new = ops[s0:]
        for o in new:
            o.deps = {d for d in o.deps if d >= s0}
            for d in o.deps:
                p = ops[d]
                if p.is_dma:
                    continue
                if p.eng == PE and o.eng == PE and not o.is_dma:
                    continue
                p.sig = True
        for o in new:
            if o.is_dma:
                continue
            if o.sig:
                self.cnt[o.eng] += 1
                o.sigval = self.cnt[o.eng]
        for o in new:
            w = {}
            for d in o.deps:
                p = ops[d]
                if p.is_dma:
                    key = ("dma", p.dsem_idx)
                else:
                    if p.eng == PE and o.eng == PE and not o.is_dma:
                        continue
                    key = ("eng", p.eng)
                if w.get(key, 0) < p.sigval:
                    w[key] = p.sigval
            s = self.seen[o.eng]
            o.waits = []
            for key, v in w.items():
                if s.get(key, 0) >= v:
                    continue
                s[key] = v
                o.waits.append((key, v))
        per = {e: [] for e in ENGS}
        for o in new:
            per[o.eng].append(o)

        def run(name, eng):
            for o in per[name]:
                for key, v in o.waits:
                    sem = sems["dma"][key[1]] if key[0] == "dma" else sems[key[1]]
                    eng.wait_ge(sem, v)
                if o.fn is None:
                    continue
                ins = o.fn(eng)
                if ins is None:
                    continue
                if o.is_dma:
                    ins.then_inc(sems["dma"][o.dsem_idx], 16)
                elif o.sig:
                    ins.then_inc(sems[o.eng], 1)

        block.tensor(lambda e: run(PE, e))
        block.scalar(lambda e: run(ACT, e))
        block.vector(lambda e: run(DVE, e))
        block.gpsimd(lambda e: run(POOL, e))
        block.sync(lambda e: run(SP, e))
        self.emitted = len(ops)
        for o in new:
            o.fn = None


_uid = [0]


def uname(s):
    _uid[0] += 1
    return f"{s}_{_uid[0]}"


class Ring:
    def __init__(self, es, nc, name, shape, dtype, n, psum=False):
        self.items = []
        for i in range(n):
            mk = nc.psum_tensor if psum else nc.sbuf_tensor
            t = es.enter_context(mk(uname(name), list(shape), dtype))
            self.items.append((t, Buf(name + str(i), excl=psum)))
        self.i = 0

    def next(self):
        it = self.items[self.i % len(self.items)]
        self.i += 1
        return it


def _rope_tables(g):
    half = g // 2
    quarter = g // 4
    inv = (10000.0 ** (-np.arange(quarter, dtype=np.float32) / quarter)).astype(np.float32)
    s = np.arange(2048)
    row = (s // 64).astype(np.float32)
    col = (s % 64).astype(np.float32)
    ang = np.concatenate([row[:, None] * inv[None, :], col[:, None] * inv[None, :]], axis=1).astype(np.float32)
    cos = np.cos(ang).astype(np.float32)
    sin = np.sin(ang).astype(np.float32)
    cosF = np.ones((128, T), np.float32)
    sinF = np.zeros((128, T), np.float32)
    for p in range(128):
        i = p % g
        j = i % half
        cosF[p, CTX:] = cos[:, j]
        sinF[p, CTX:] = -sin[:, j] if i < half else sin[:, j]
    return cosF, sinF


def _consts():
    c = {}
    c["identF"] = np.eye(128, dtype=np.float32)
    ones_bd = np.zeros((128, 128), np.float32)
    ones_bd[:64, :64] = 1
    ones_bd[64:, 64:] = 1
    ones_lo = np.zeros((128, 128), np.float32)
    ones_lo[:, :64] = 1
    ones_hi = np.zeros((128, 128), np.float32)
    ones_hi[:, 64:] = 1
    perm128 = np.zeros((128, 128), np.float32)
    perm64 = np.zeros((128, 128), np.float32)
    for m in range(128):
        perm128[(m + 64) % 128, m] = 1
        i = m % 64
        perm64[(m - i) + (i + 32) % 64, m] = 1
    mats = np.stack([np.ones((128, 128), np.float32), ones_bd, ones_lo, ones_hi, perm128, perm64], axis=1)
    c["matsB"] = mats.astype(ml_dtypes.bfloat16)
    c128, s128 = _rope_tables(128)
    c64, s64 = _rope_tables(64)
    c["rope"] = np.stack([c128, s128, c64, s64], axis=0)
    mask = np.zeros((128, 6, 512), np.float32)
    kp = np.arange(128)[:, None]
    q = np.arange(512)[None, :]
    for rel in range(6):
        mask[:, rel, :] = (np.abs(q - (rel - 1) * 128 - kp) <= 128).astype(np.float32)
    c["wmask"] = mask.astype(ml_dtypes.bfloat16)
    return c


def build(nlayers=4, dbg=False):
    nc = bass.Bass("TRN2", target_bir_lowering=False)
    big = nlayers >= 2

    def din(name, shape, dt=F32):
        return nc.dram_tensor(name, list(shape), dt, kind="ExternalInput").ap()

    x_in = din("x", [2048, D])
    ctx_in = din("ctx", [CTX, D])
    cfm_in = din("cfm", [128, 8, 2])
    ada_w = din("ada_w", [4, D, 6 * D])
    ada_b = din("ada_b", [4, 6 * D])
    norm_mix = din("norm_mix", [4, D])
    norm_ffn = din("norm_ffn", [4, D])
    gqa_wqkv = din("gqa_wqkv", [1, D, 1536])
    gqa_q_gain = din("gqa_q_gain", [1, 128])
    gqa_k_gain = din("gqa_k_gain", [1, 128])
    gqa_wo = din("gqa_wo", [1, D, D])
    mla_wdown = din("mla_wdown", [1, D, 704])
    mla_qa_gain = din("mla_qa_gain", [1, 384])
    mla_kva_gain = din("mla_kva_gain", [1, 256])
    mla_wuq = din("mla_wuq", [1, 384, 1536])
    mla_wukv = din("mla_wukv", [1, 256, 2048])
    mla_q_gain = din("mla_q_gain", [1, 192])
    mla_k_gain = din("mla_k_gain", [1, 192])
    mla_wo = din("mla_wo", [1, D, D])
    win_wqkv = din("win_wqkv", [1, D, 1280])
    win_q_gain = din("win_q_gain", [1, 64])
    win_k_gain = din("win_k_gain", [1, 64])
    win_sink = din("win_sink", [1, 16])
    win_wo = din("win_wo", [1, D, D])
    diff_wqkv = din("diff_wqkv", [1, D, 3072])
    diff_q_gain = din("diff_q_gain", [1, 64])
    diff_k_gain = din("diff_k_gain", [1, 64])
    diff_lambda = din("diff_lambda", [1, 4, 64])
    diff_subln = din("diff_subln", [1, 128])
    diff_wo = din("diff_wo", [1, D, D])
    ffn_w13 = din("ffn_w13", [2, D, 7168])
    ffn_w2 = din("ffn_w2", [2, 3584, D])
    moe_router = din("moe_router", [2, D, 8])
    moe_w13 = din("moe_w13", [2, 8, D, 7168] if big else [1, 1, 8, 8])
    moe_w2 = din("moe_w2", [2, 8, 3584, D] if big else [1, 1, 8, 8])
    identF_in = din("identF", [128, 128])
    matsB_in = din("matsB", [128, 6, 128], BF16)
    rope_in = din("rope", [4, 128, T])
    wmask_in = din("wmask", [128, 6, 512], BF16)
    out_d = nc.dram_tensor("out", [2048, D], F32, kind="ExternalOutput").ap()
    skind = "ExternalOutput" if dbg else "Internal"
    modrows = nc.dram_tensor("modrows", [4, 2, 6 * D], F32, kind=skind).ap()
    Qs = nc.dram_tensor("Qs", [12, 128, T], BF16, kind=skind).ap()
    Ks = nc.dram_tensor("Ks", [9, 128, T], BF16, kind=skind).ap()
    Vs = nc.dram_tensor("Vs", [T, 1024], BF16, kind=skind).ap()
    if dbg:
        dbg_hT = nc.dram_tensor("dbg_hT", [128, 8, T], BF16, kind="ExternalOutput").ap()
        dbg_AO = nc.dram_tensor("dbg_AO", [128, 8, T], BF16, kind="ExternalOutput").ap()
        dbg_xa = nc.dram_tensor("dbg_xa", [128, NT, D], F32, kind="ExternalOutput").ap()
        dbg_modF = nc.dram_tensor("dbg_modF", [128, 4, 2, 48], F32, kind="ExternalOutput").ap()

    P = Prog()
    top = ExitStack()
    sems = {e: top.enter_context(nc.semaphore("s_" + e)) for e in ENGS}
    sems["dma"] = [top.enter_context(nc.semaphore(f"sd{i}")) for i in range(N_DMA_SEMS)]

    def sb(es, name, shape, dt):
        return es.enter_context(nc.sbuf_tensor(uname(name), list(shape), dt))

    x_sb = sb(top, "x_sb", [128, NT, D], F32)
    xb = [Buf(f"x{t}") for t in range(NT)]
    identF = sb(top, "identF", [128, 128], F32)
    matsB = sb(top, "matsB", [128, 6, 128], BF16)
    epsT = sb(top, "epsT", [128, 1], F32)
    modF = sb(top, "modF", [128, 4, 2, 48], F32)
    cst = Buf("consts")
    b_modF = Buf("modF")
    ones128 = matsB[:, 0, :]
    ones_bd = matsB[:, 1, :]
    ones_lo = matsB[:, 2, :]
    ones_hi = matsB[:, 3, :]
    perm128 = matsB[:, 4, :]
    perm64 = matsB[:, 5, :]

    def run_phase(fn, name=None):
        with ExitStack() as es:
            fn(es)
            P.barrier()
            with nc.named_scope(uname(name or getattr(fn, "__name__", "ph"))):
                with nc.Block() as block:
                    P.emit(block, sems)

    def dma(eng, out, in_, reads, writes, sbuf, joint=True, **kw):
        return P.op(eng, lambda e: e.dma_start(out=out, in_=in_, **kw), reads=reads, writes=writes, joint=joint, dma_buf=sbuf)

    def mm(out, lhsT, rhs, start, stop, reads, wbuf):
        return P.op(PE, lambda e: e.matmul(out, lhsT=lhsT, rhs=rhs, start=start, stop=stop), reads=reads, writes=[wbuf], joint=not start)

    def act(out, in_, func, reads, writes, joint=True, **kw):
        return P.op(ACT, lambda e: e.activation(out=out, in_=in_, func=func, **kw), reads=reads, writes=writes, joint=joint)

    def tt(out, in0, in1, op, reads, writes, joint=True, eng=DVE):
        return P.op(eng, lambda e: e.tensor_tensor(out=out, in0=in0, in1=in1, op=op), reads=reads, writes=writes, joint=joint)

    def ts(out, in0, s1, s2, op0, op1, reads, writes, joint=True, eng=DVE):
        if s2 is None:
            return P.op(eng, lambda e: e.tensor_scalar(out=out, in0=in0, scalar1=s1, scalar2=None, op0=op0), reads=reads, writes=writes, joint=joint)
        return P.op(eng, lambda e: e.tensor_scalar(out=out, in0=in0, scalar1=s1, scalar2=s2, op0=op0, op1=op1), reads=reads, writes=writes, joint=joint)

    def stt(out, in0, scalar, in1, op0, op1, reads, writes, joint=True, eng=DVE):
        return P.op(eng, lambda e: e.scalar_tensor_tensor(out=out, in0=in0, scalar=scalar, in1=in1, op0=op0, op1=op1), reads=reads, writes=writes, joint=joint)

    def recip(out, in_, reads, writes, joint=True):
        return P.op(DVE, lambda e: e.reciprocal(out=out, in_=in_), reads=reads, writes=writes, joint=joint)

    def tiles_of(tb):
        t0, n = tb
        return list(range(t0 // 128, (t0 + n) // 128))

    def phase0(es):
        for t in range(2):
            dma(SP, x_sb[:, t, :], ctx_in[t * 128:(t + 1) * 128, :], [], [xb[t]], xb[t])
        for t in range(16):
            dma(SP, x_sb[:, 2 + t, :], x_in[t * 128:(t + 1) * 128, :], [], [xb[2 + t]], xb[2 + t])
        dma(SP, identF[:], identF_in[:, :], [], [cst], cst)
        dma(SP, matsB[:], matsB_in[:, :, :], [], [cst], cst)
        P.op(DVE, lambda e: e.memset(epsT[:], EPS), writes=[cst], joint=True)
        cfm = sb(es, "cfm", [128, 8, 2], F32)
        silu2 = sb(es, "silu2", [128, 8, 2], F32)
        b_c = Buf("cfm")
        b_s = Buf("silu2")
        dma(SP, cfm[:], cfm_in[:, :, :], [], [b_c], b_c)
        act(silu2[:], cfm[:], AF.Silu, [b_c], [b_s])
        ones2 = sb(es, "ones2", [1, 2], F32)
        b_o2 = Buf("ones2")
        P.op(DVE, lambda e: e.memset(ones2[:], 1.0), writes=[b_o2])
        wr = Ring(es, nc, "adaw", [128, 8, 512], F32, 2)
        br = Ring(es, nc, "adab", [1, 512], F32, 2)
        rows = sb(es, "rows", [2, 6 * D], F32)
        b_rows = Buf("rows")
        pr = Ring(es, nc, "p0ps", [128, 512], F32, 2, psum=True)
        pt = Ring(es, nc, "p0pt", [128, 512], F32, 1, psum=True)
        for L in range(nlayers):
            for cb in range(12):
                w, bw = wr.next()
                bt, bbt = br.next()
                dma(SP, w[:], ada_w[L].rearrange("(c p) n -> p c n", p=128)[:, :, cb * 512:(cb + 1) * 512], [], [bw], bw)
                dma(SP, bt[:], ada_b[L:L + 1, cb * 512:(cb + 1) * 512], [], [bbt], bbt)
                ps, bps = pr.next()
                for kc in range(8):
                    mm(ps[0:2, :], silu2[:, kc, :], w[:, kc, :], kc == 0, False, [b_s, bw], bps)
                mm(ps[0:2, :], ones2[:, :], bt[:, :], False, True, [b_o2, bbt], bps)
                P.op(DVE, lambda e, ps=ps, cb=cb: e.tensor_copy(out=rows[:, cb * 512:(cb + 1) * 512], in_=ps[0:2, :]),
                     reads=[bps], writes=[b_rows], joint=True)
            dma(SP, modrows[L], rows[:, :], [b_rows], [], b_rows)
            tp, btp = pt.next()
            for j in range(48):
                P.op(PE, lambda e, j=j, tp=tp: e.transpose(out=tp[:, 2 * j:2 * j + 2], in_=rows[0:2, j * 128:(j + 1) * 128], identity=identF[0:2, 0:2]),
                     reads=[b_rows, cst], writes=[btp], joint=(j > 0))
            P.op(DVE, lambda e, tp=tp, L=L: e.tensor_copy(out=modF[:, L, :, :].rearrange("p r j -> p j r"), in_=tp[:, 0:96].rearrange("p (j r) -> p j r", r=2)),
                 reads=[btp], writes=[b_modF], joint=True)

    def norm_phase(es, L, which, hT, hb, tbs, moe=None):
        gsrc = (norm_mix if which == 0 else norm_ffn)
        gF = sb(es, "gF", [128, 8], F32)
        AB = sb(es, "AB", [128, 2, 2, 8], F32)
        b_g = Buf("gF")
        b_AB = Buf("AB")
        dma(SP, gF[:], gsrc[L].rearrange("(c p) -> p c", p=128), [], [b_g], b_g, allow_slow_non_contiguous=True)
        ish, isc = (0, 1) if which == 0 else (3, 4)
        for r in range(2):
            stt(AB[:, r, 0, :], modF[:, L, r, isc * 8:(isc + 1) * 8], 1.0, gF[:], ALU.add, ALU.mult, [b_modF, b_g], [b_AB])
            P.op(DVE, lambda e, r=r: e.tensor_copy(out=AB[:, r, 1, :], in_=modF[:, L, r, ish * 8:(ish + 1) * 8]), reads=[b_modF], writes=[b_AB], joint=True)
        junk = sb(es, "junk", [128, D], BF16)
        b_junk = Buf("junk")
        ss = sb(es, "ss", [128, NT], F32)
        rstd = sb(es, "rstd", [128, NT], F32)
        b_ss = [Buf(f"ss{i}") for i in range(5)]
        b_rstd = [Buf(f"rstd{i}") for i in range(5)]
        P.op(DVE, lambda e: e.memset(ss[:], 0.0), writes=b_ss)
        xnr = Ring(es, nc, "xn", [128, 4, D], F32, 1)
        ptr = Ring(es, nc, "nps", [128, 512], F32, 2, psum=True)
        if moe is not None:
            h32r = Ring(es, nc, "h32", [128, 8, 512], F32, 1)
            R32 = sb(es, "R32", [128, 8, 8], F32)
            b_R = Buf("R32")
            dma(SP, R32[:], moe["router"].rearrange("(c p) e -> p c e", p=128), [], [b_R], b_R)
            lgr = Ring(es, nc, "lgps", [128, 8], F32, 2, psum=True)
        for bi, tb in enumerate(TBS):
            if tb not in tbs:
                continue
            t0, n = tb
            tl = tiles_of(tb)
            r = 1 if bi == 0 else 0
            for t in tl:
                act(junk[:], x_sb[:, t, :], AF.Square, [xb[t]], [b_junk, b_ss[bi]], joint=False, accum_out=ss[:, t:t + 1])
            act(rstd[:, tl[0]:tl[-1] + 1], ss[:, tl[0]:tl[-1] + 1], AF.Sqrt, [b_ss[bi], cst], [b_rstd[bi]], joint=False, scale=1.0 / D, bias=epsT[:, 0:1])
            recip(rstd[:, tl[0]:tl[-1] + 1], rstd[:, tl[0]:tl[-1] + 1], [b_rstd[bi]], [b_rstd[bi]], joint=False)
            xn, bxn = xnr.next()
            for j, t in enumerate(tl):
                ts(xn[:, j, :], x_sb[:, t, :], rstd[:, t:t + 1], None, ALU.mult, None, [xb[t], b_rstd[bi]], [bxn])
            if moe is not None:
                h32, bh32 = h32r.next()
            for c in range(8):
                ps, bps = ptr.next()
                for j, t in enumerate(tl):
                    P.op(PE, lambda e, ps=ps, j=j, c=c, xn=xn: e.transpose(out=ps[:, j * 128:(j + 1) * 128], in_=xn[:, j, c * 128:(c + 1) * 128], identity=identF[:]),
                         reads=[bxn, cst], writes=[bps], joint=(j > 0))
                act(hT[:, c, t0:t0 + n], ps[:, 0:n], AF.Identity, [bps, b_AB], [hb[bi]], scale=AB[:, r, 0, c:c + 1], bias=AB[:, r, 1, c:c + 1])
                if moe is not None:
                    ts(h32[:, c, 0:n], ps[:, 0:n], AB[:, r, 0, c:c + 1], AB[:, r, 1, c:c + 1], ALU.mult, ALU.add, [bps, b_AB], [bh32])
            if moe is not None:
                for j, t in enumerate(tl):
                    lp, blp = lgr.next()
                    for c in range(8):
                        mm(lp[:, :], h32[:, c, j * 128:(j + 1) * 128], R32[:, c, :], c == 0, c == 7, [bh32, b_R], blp)
                    P.op(DVE, lambda e, lp=lp, t=t: e.tensor_copy(out=moe["lg"][:, t, :], in_=lp[:, :]), reads=[blp], writes=[moe["blg"]], joint=True)

    def route(es, moe, comb, b_comb):
        lg = moe["lg"]
        blg = moe["blg"]
        m8 = sb(es, "m8", [128, NT, 8], F32)
        tmp = sb(es, "rtmp", [128, NT, 8], F32)
        msk = sb(es, "rmsk", [128, NT, 8], F32)
        den = sb(es, "rden", [128, NT], F32)
        b1, b2, b3, b4 = Buf("m8"), Buf("rtmp"), Buf("rmsk"), Buf("rden")
        for t in range(NT):
            P.op(DVE, lambda e, t=t: e.max(out=m8[:, t, :], in_=lg[:, t, :]), reads=[blg], writes=[b1], joint=True)
        tt(msk[:], lg[:], m8[:, :, 1:2].to_broadcast([128, NT, 8]), ALU.is_ge, [blg, b1], [b3])
        tt(tmp[:], lg[:], m8[:, :, 0:1].to_broadcast([128, NT, 8]), ALU.subtract, [blg, b1], [b2])
        act(tmp[:], tmp[:], AF.Exp, [b2], [b2], joint=False)
        tt(tmp[:], tmp[:], msk[:], ALU.mult, [b2, b3], [b2], joint=False)
        P.op(DVE, lambda e: e.reduce_sum(out=den[:], in_=tmp[:], axis=AX.X), reads=[b2], writes=[b4])
        recip(den[:], den[:], [b4], [b4], joint=False)
        tt(comb[:], tmp[:], den[:].unsqueeze(2).to_broadcast([128, NT, 8]), ALU.mult, [b2, b4], [b_comb], joint=False)

    def load_gate_rows(es, L, idx):
        g = sb(es, "grow", [128, 2, D], F32)
        bg = Buf("grow")
        for r in range(2):
            dma(SP, g[:, r, :], modrows[L, r:r + 1, idx * D:(idx + 1) * D].partition_broadcast(128), [], [bg], bg)
        return g, bg

    def ffn_phase(es, L, hT, hb, tbs, experts, comb, b_comb):
        grow, bgrow = load_gate_rows(es, L, 5)
        w13r = Ring(es, nc, "w13", [128, 8, 1024], BF16, 2)
        w2r = Ring(es, nc, "w2", [128, 4, D], BF16, 2)
        has_ctx = TBS[0] in tbs
        if has_ctx:
            w2cr = Ring(es, nc, "w2c", [128, 4, D], BF16, 2)
        y = sb(es, "y", [128, 4, T], BF16)
        yb = [Buf(f"y{i}") for i in range(5)]
        sgr = Ring(es, nc, "sg", [128, 512], BF16, 3)
        gpr = Ring(es, nc, "gps", [128, 512], F32, 2, psum=True)
        upr = Ring(es, nc, "ups", [128, 512], F32, 2, psum=True)
        opr = Ring(es, nc, "ops", [128, 512], F32, 3, psum=True)
        for ei, (w13, w2) in enumerate(experts):
            w13v = w13.rearrange("(c p) n -> p c n", p=128)
            w2v = w2.rearrange("(c p) n -> p c n", p=128)
            for fb in range(7):
                wa, bwa = w13r.next()
                wb_, bwb = w2r.next()
                dma(POOL, wa[:, :, 0:512], w13v[:, :, fb * 512:(fb + 1) * 512], [], [bwa], bwa)
                dma(POOL, wa[:, :, 512:1024], w13v[:, :, 3584 + fb * 512:3584 + (fb + 1) * 512], [], [bwa], bwa)
                dma(POOL, wb_[:], w2v[:, fb * 4:(fb + 1) * 4, :], [], [bwb], bwb)
                if has_ctx:
                    wc_, bwc = w2cr.next()
                    tt(wc_[:], wb_[:], grow[:, 1, :].unsqueeze(1).to_broadcast([128, 4, D]), ALU.mult, [bwb, bgrow], [bwc], joint=False, eng=POOL)
                tt(wb_[:], wb_[:], grow[:, 0, :].unsqueeze(1).to_broadcast([128, 4, D]), ALU.mult, [bwb, bgrow], [bwb], joint=False, eng=POOL)
                for bi, tb in enumerate(TBS):
                    if tb not in tbs:
                        continue
                    t0, n = tb
                    for fc in range(4):
                        gp, bgp = gpr.next()
                        up, bup = upr.next()
                        for kc in range(8):
                            mm(gp[:, 0:n], wa[:, kc, fc * 128:(fc + 1) * 128], hT[:, kc, t0:t0 + n], kc == 0, kc == 7, [bwa, hb[bi]], bgp)
                        for kc in range(8):
                            mm(up[:, 0:n], wa[:, kc, 512 + fc * 128:512 + (fc + 1) * 128], hT[:, kc, t0:t0 + n], kc == 0, kc == 7, [bwa, hb[bi]], bup)
                        sg, bsg = sgr.next()
                        act(sg[:, 0:n], gp[:, 0:n], AF.Silu, [bgp], [bsg], joint=False)
                        tt(y[:, fc, t0:t0 + n], sg[:, 0:n], up[:, 0:n], ALU.mult, [bsg, bup], [yb[bi]])
                for bi, tb in enumerate(TBS):
                    if tb not in tbs:
                        continue
                    r = 1 if bi == 0 else 0
                    for t in tiles_of(tb):
                        for dh in range(2):
                            op_, bop = opr.next()
                            wsel, bwsel = (wc_, bwc) if r == 1 else (wb_, bwb)
                            for fc in range(4):
                                mm(op_[:, :], y[:, fc, t * 128:(t + 1) * 128], wsel[:, fc, dh * 512:(dh + 1) * 512], fc == 0, fc == 3, [yb[bi], bwsel], bop)
                            xs = x_sb[:, t, dh * 512:(dh + 1) * 512]
                            if comb is None:
                                tt(xs, op_[:, :], xs, ALU.add, [bop, xb[t]], [xb[t]], joint=False)
                            else:
                                stt(xs, op_[:, :], comb[:, t, ei:ei + 1], xs, ALU.mult, ALU.add, [bop, xb[t], b_comb], [xb[t]], joint=False)

    def load_w_chunk(ring, wv, pieces, kc_n):
        w, bw = ring.next()
        off = 0
        for (c0, ncol) in pieces:
            dma(POOL, w[:, 0:kc_n, off:off + ncol], wv[:, :, c0:c0 + ncol], [], [bw], bw)
            off += ncol
        return w, bw

    def gain_tile(es, pieces):
        g = sb(es, "gain", [128, 1], F32)
        bg = Buf("gain")
        off = 0
        for ap in pieces:
            n = ap.shape[0]
            dma(SP, g[off:off + n, :], ap.rearrange("(p o) -> p o", o=1), [], [bg], bg)
            off += n
        return g, bg

    class ProjCtx:
        pass

    def proj_setup(es, need_rope):
        pc = ProjCtx()
        pc.wring = Ring(es, nc, "wch", [128, 8, 128], BF16, 3)
        pc.pps = Ring(es, nc, "pps", [128, 512], F32, 3, psum=True)
        pc.sps = Ring(es, nc, "sps", [128, 512], F32, 1, psum=True)
        pc.rps = Ring(es, nc, "rps", [128, 512], F32, 1, psum=True)
        pc.sq = Ring(es, nc, "sq", [128, 512], BF16, 3)
        pc.qg = Ring(es, nc, "qg", [128, 512], BF16, 3)
        pc.rstd = Ring(es, nc, "prstd", [128, 512], F32, 2)
        pc.t1 = Ring(es, nc, "pt1", [128, 512], F32, 1)
        pc.t2 = Ring(es, nc, "pt2", [128, 512], F32, 1)
        pc.ob = Ring(es, nc, "pob", [128, 512], BF16, 3)
        pc.rope = None
        if need_rope is not None:
            pc.rope = sb(es, "rope", [128, 2, T], F32)
            pc.b_rope = Buf("rope")
            i0 = 0 if need_rope == 128 else 2
            for k in range(2):
                dma(SP, pc.rope[:, k, :], rope_in[i0 + k], [], [pc.b_rope], pc.b_rope)
        return pc

    def proj_group(pc, src, src_bufs, kc_n, chunks, tbs, group, rope, dsts):
        nch = len(chunks)
        for bi, tb in enumerate(TBS):
            if tb not in tbs:
                continue
            t0, n = tb
            pss = []
            for (w, bw, g, bg) in chunks:
                ps, bps = pc.pps.next()
                for kc in range(kc_n):
                    mm(ps[:, 0:n], w[:, kc, :], src[:, kc, t0:t0 + n], kc == 0, kc == kc_n - 1, [bw, src_bufs[bi]], bps)
                pss.append((ps, bps))
            qgs = []
            sp, bsp = pc.sps.next()
            for ci, (ps, bps) in enumerate(pss):
                (w, bw, g, bg) = chunks[ci]
                sq, bsq = pc.sq.next()
                act(sq[:, 0:n], ps[:, 0:n], AF.Square, [bps], [bsq], joint=False)
                qg, bqg = pc.qg.next()
                ts(qg[:, 0:n], ps[:, 0:n], g[:, 0:1], None, ALU.mult, None, [bps, bg], [bqg], joint=False)
                qgs.append((qg, bqg))
                onesm = ones_bd if group == 64 else ones128
                if group == "all":
                    mm(sp[:, 0:n], onesm, sq[:, 0:n], ci == 0, ci == nch - 1, [bsq, cst], bsp)
                else:
                    assert nch == 1
                    mm(sp[:, 0:n], onesm, sq[:, 0:n], True, True, [bsq, cst], bsp)
            cnt = {128: 128, 64: 64, "all": 128 * nch}[group]
            rs, brs = pc.rstd.next()
            act(rs[:, 0:n], sp[:, 0:n], AF.Sqrt, [bsp, cst], [brs], joint=False, scale=1.0 / cnt, bias=epsT[:, 0:1])
            recip(rs[:, 0:n], rs[:, 0:n], [brs], [brs], joint=False)
            for ci, (qg, bqg) in enumerate(qgs):
                ob, bob = pc.ob.next()
                if rope is not None:
                    rp, brp = pc.rps.next()
                    mm(rp[:, 0:n], perm128 if rope == 128 else perm64, qg[:, 0:n], True, True, [bqg, cst], brp)
                    t1, bt1 = pc.t1.next()
                    t2, bt2 = pc.t2.next()
                    tt(t1[:, 0:n], qg[:, 0:n], pc.rope[:, 0, t0:t0 + n], ALU.mult, [bqg, pc.b_rope], [bt1], joint=False)
                    tt(t2[:, 0:n], rp[:, 0:n], pc.rope[:, 1, t0:t0 + n], ALU.mult, [brp, pc.b_rope], [bt2], joint=False)
                    tt(t1[:, 0:n], t1[:, 0:n], t2[:, 0:n], ALU.add, [bt1, bt2], [bt1], joint=False)
                    tt(ob[:, 0:n], t1[:, 0:n], rs[:, 0:n], ALU.mult, [bt1, brs], [bob], joint=False)
                else:
                    tt(ob[:, 0:n], qg[:, 0:n], rs[:, 0:n], ALU.mult, [bqg, brs], [bob], joint=False)
                d = dsts[ci]
                if d[0] == "dram":
                    dma(SP, d[1][:, t0:t0 + n], ob[:, 0:n], [bob], [], bob)
                else:
                    P.op(POOL, lambda e, d=d, ob=ob, t0=t0, n=n: e.tensor_copy(out=d[1][:, t0:t0 + n], in_=ob[:, 0:n]),
                         reads=[bob], writes=[d[2][bi]], joint=True)

    def proj_v(es, pc, src, src_bufs, kc_n, wv, pieces, tbs):
        F = sum(p[1] for p in pieces)
        wt = sb(es, "wv", [128, kc_n, F], BF16)
        bwt = Buf("wv")
        off = 0
        for (c0, ncol) in pieces:
            dma(POOL, wt[:, :, off:off + ncol], wv[:, :, c0:c0 + ncol], [], [bwt], bwt)
            off += ncol
        vps = pc.pps
        vob = Ring(es, nc, "vob", [128, 1024], BF16, 1)
        for bi, tb in enumerate(TBS):
            if tb not in tbs:
                continue
            for t in tiles_of(tb):
                vo, bvo = vob.next()
                for f0 in range(0, F, 512):
                    fn_ = min(512, F - f0)
                    ps, bps = vps.next()
                    for kc in range(kc_n):
                        mm(ps[:, 0:fn_], src[:, kc, t * 128:(t + 1) * 128], wt[:, kc, f0:f0 + fn_], kc == 0, kc == kc_n - 1, [src_bufs[bi], bwt], bps)
                    P.op(DVE, lambda e, vo=vo, ps=ps, f0=f0, fn_=fn_: e.tensor_copy(out=vo[:, f0:f0 + fn_], in_=ps[:, 0:fn_]),
                         reads=[bps], writes=[bvo], joint=True)
                dma(SP, Vs[t * 128:(t + 1) * 128, 0:F], vo[:, 0:F], [bvo], [], bvo)

    def attn_phase(es, AO, aob, units, scale, masks=None):
        kq = {}
        ldr = Ring(es, nc, "akq", [128, T], BF16, 8)
        vr = Ring(es, nc, "av", [128, NT, 128], BF16, 4)
        spr = Ring(es, nc, "asps", [128, 512], F32, 3, psum=True)
        nacc = len(set(s_["acc"] for u_ in units for s_ in u_["subs"]))
        opr = [Ring(es, nc, "aops", [128, 512], F32, 2 if nacc == 1 else 1, psum=True) for _ in range(nacc)]
        lpr = [Ring(es, nc, "alps", [128, 512], F32, 2 if nacc == 1 else 1, psum=True) for _ in range(nacc)]
        fpr = Ring(es, nc, "afps", [128, 512], F32, 1, psum=True)
        ptr = Ring(es, nc, "apt", [128, 512], BF16, 5)
        ftm = [Ring(es, nc, f"aft{i}", [128, 512], F32, 2) for i in range(3)]
        fsq = Ring(es, nc, "afsq", [128, 512], BF16, 2)
        if masks is not None:
            wm = sb(es, "wm", [128, 6, 512], BF16)
            b_wm = Buf("wm")
            dma(SP, wm[:], wmask_in[:, :, :], [], [b_wm], b_wm)
        cache = {}
        R = dict(ftm=ftm, fsq=fsq, fpr=fpr)
        items = []
        for u in units:
            for qi, (q0, qn, ktiles_fn) in enumerate(u["qblocks"]):
                work = []
                for s_ in u["subs"]:
                    for kt in ktiles_fn():
                        work.append((s_, kt))
                lastidx = {}
                firstidx = {}
                for i, (s_, kt) in enumerate(work):
                    lastidx[s_["acc"]] = i
                    firstidx.setdefault(s_["acc"], i)
                for i, (s_, kt) in enumerate(work):
                    a_ = s_["acc"]
                    items.append(dict(u=u, q0=q0, qn=qn, s=s_, kt=kt, first=(i == firstidx[a_]), last=(i == lastidx[a_]),
                                      end=(i == len(work) - 1), start_qb=(i == 0), start_unit=(i == 0 and qi == 0)))

        def do_loads(u):
            nonlocal cache
            tl = {}
            for (key, ap) in u["loads"]:
                if key in cache:
                    tl[key] = cache[key]
                    continue
                tle, btl = ldr.next()
                dma(SP, tle[:], ap, [], [btl], btl, joint=False)
                cache = {k: v for k, v in cache.items() if v[0] is not tle}
                cache[key] = (tle, btl)
                tl[key] = (tle, btl)
            for (key, c0, ncol, pad) in u["vloads"]:
                if key in cache:
                    tl[key] = cache[key]
                    continue
                tle, btl = vr.next()
                if ncol < 128:
                    P.op(DVE, lambda e, tle=tle: e.memset(tle[:], 0.0), writes=[btl])
                dma(SP, tle[:, :, pad:pad + ncol], Vs[:, c0:c0 + ncol].rearrange("(t p) f -> p t f", p=128), [], [btl], btl, joint=False)
                cache = {k: v for k, v in cache.items() if v[0] is not tle}
                cache[key] = (tle, btl)
                tl[key] = (tle, btl)
            return tl

        def do_OL(it):
            s_ = it["s"]
            a_ = s_["acc"]
            qn = it["qn"]
            O, Lp = it["O"], it["Lp"]
            vt, bv = it["tl"][s_["v"]]
            pt_, bpt = it["pt"]
            ktile = it["kt"][0]
            mm(O[a_][0][:, 0:qn], vt[:, ktile, :], pt_[:, 0:qn], it["first"], it["last"], [bv, bpt], O[a_][1])
            mm(Lp[a_][0][:, 0:qn], s_["ones"], pt_[:, 0:qn], it["first"], it["last"], [cst, bpt], Lp[a_][1])
            if it["end"]:
                it["u"]["fin"](it["u"], it["q0"], qn, O, Lp, R)

        pend = []
        cur_tl = None
        cur_O = cur_L = None
        for it in items:
            u = it["u"]
            if it["start_unit"]:
                cur_tl = do_loads(u)
            if it["start_qb"]:
                accs = sorted(set(s_["acc"] for s_ in u["subs"]))
                cur_O = {a_: opr[a_].next() for a_ in accs}
                cur_L = {a_: lpr[a_].next() for a_ in accs}
            it["tl"], it["O"], it["Lp"] = cur_tl, cur_O, cur_L
            s_ = it["s"]
            q0, qn = it["q0"], it["qn"]
            ktile = it["kt"][0]
            sp, bsp = spr.next()
            npairs = len(s_["pairs"])
            for pi, (kkey, qkey, r0, nr) in enumerate(s_["pairs"]):
                kt_, bk = cur_tl[kkey]
                qt_, bq = cur_tl[qkey]
                mm(sp[:, 0:qn], kt_[r0:r0 + nr, ktile * 128:(ktile + 1) * 128], qt_[r0:r0 + nr, q0:q0 + qn], pi == 0, pi == npairs - 1, [bk, bq], bsp)
            pt_, bpt = ptr.next()
            act(pt_[:, 0:qn], sp[:, 0:qn], AF.Exp, [bsp], [bpt], joint=False, scale=scale)
            if it["kt"][1] is not None:
                tt(pt_[:, 0:qn], pt_[:, 0:qn], wm[:, it["kt"][1], 0:qn], ALU.mult, [bpt, b_wm], [bpt], joint=False)
            it["pt"] = (pt_, bpt)
            pend.append(it)
            if len(pend) > 2:
                do_OL(pend.pop(0))
        while pend:
            do_OL(pend.pop(0))

    def fin_default(extra=None):
        def fin(u, q0, qn, O, Lp, R):
            r, br = R["ftm"][0].next()
            if extra is not None:
                es_t, bes = extra
                ts(r[:, 0:qn], Lp[0][0][:, 0:qn], es_t[:, u["c"]:u["c"] + 1], None, ALU.add, None, [Lp[0][1], bes], [br], joint=False)
                recip(r[:, 0:qn], r[:, 0:qn], [br], [br], joint=False)
            else:
                recip(r[:, 0:qn], Lp[0][0][:, 0:qn], [Lp[0][1]], [br], joint=False)
            tt(u["AO"][:, u["c"], q0:q0 + qn], O[0][0][:, 0:qn], r[:, 0:qn], ALU.mult, [O[0][1], br], [u["aob"]], joint=True)
        return fin

    def all_k():
        return [(k, None) for k in range(NT)]

    def ctx_k():
        return [(0, None), (1, None)]

    LAT_QB = [(256 + 512 * i, 512, all_k) for i in range(4)]
    CTX_QB = [(0, 256, ctx_k)]

    def std_layer(L):
        kind = L % 4
        need_ctx = L < 3
        moe = (L % 2 == 1)
        tbs_all = list(TBS)
        tbs_lat = TBS[1:]
        hT = None
        state = {}

        def phA(es):
            hT = sb(es, "hT", [128, 8, T], BF16)
            hb = [Buf(f"hT{i}") for i in range(5)]
            norm_phase(es, L, 0, hT, hb, tbs_all)
            if kind == 0:
                wv = gqa_wqkv[0].rearrange("(c p) n -> p c n", p=128)
                pc = proj_setup(es, 128)
                gq, bgq = gain_tile(es, [gqa_q_gain[0]])
                gk, bgk = gain_tile(es, [gqa_k_gain[0]])
                for h in range(8):
                    w, bw = load_w_chunk(pc.wring, wv, [(h * 128, 128)], 8)
                    proj_group(pc, hT, hb, 8, [(w, bw, gq, bgq)], tbs_all, 128, 128, [("dram", Qs[h])])
                for kvh in range(2):
                    w, bw = load_w_chunk(pc.wring, wv, [(1024 + kvh * 128, 128)], 8)
                    proj_group(pc, hT, hb, 8, [(w, bw, gk, bgk)], tbs_all, 128, 128, [("dram", Ks[kvh])])
                proj_v(es, pc, hT, hb, 8, wv, [(1280, 256)], tbs_all)
            elif kind == 2:
                wv = win_wqkv[0].rearrange("(c p) n -> p c n", p=128)
                pc = proj_setup(es, 64)
                gq, bgq = gain_tile(es, [win_q_gain[0], win_q_gain[0]])
                gk, bgk = gain_tile(es, [win_k_gain[0], win_k_gain[0]])
                for c in range(8):
                    w, bw = load_w_chunk(pc.wring, wv, [(c * 128, 128)], 8)
                    proj_group(pc, hT, hb, 8, [(w, bw, gq, bgq)], tbs_all, 64, 64, [("dram", Qs[c])])
                for kvh in range(2):
                    w, bw = load_w_chunk(pc.wring, wv, [(1024 + kvh * 64, 64), (1024 + kvh * 64, 64)], 8)
                    proj_group(pc, hT, hb, 8, [(w, bw, gk, bgk)], tbs_all, 64, 64, [("dram", Ks[kvh])])
                proj_v(es, pc, hT, hb, 8, wv, [(1152, 128)], tbs_all)
            elif kind == 3:
                wv = diff_wqkv[0].rearrange("(c p) n -> p c n", p=128)
                pc = proj_setup(es, 64)
                gq, bgq = gain_tile(es, [diff_q_gain[0], diff_q_gain[0]])
                gk, bgk = gain_tile(es, [diff_k_gain[0], diff_k_gain[0]])
                for h in range(8):
                    w, bw = load_w_chunk(pc.wring, wv, [(h * 128, 128)], 8)
                    proj_group(pc, hT, hb, 8, [(w, bw, gq, bgq)], tbs_lat, 64, 64, [("dram", Qs[h])])
                for h in range(8):
                    w, bw = load_w_chunk(pc.wring, wv, [(1024 + h * 128, 128)], 8)
                    proj_group(pc, hT, hb, 8, [(w, bw, gk, bgk)], tbs_all, 64, 64, [("dram", Ks[h])])
                proj_v(es, pc, hT, hb, 8, wv, [(2048, 1024)], tbs_all)
            else:
                wd = mla_wdown[0].rearrange("(c p) n -> p c n", p=128)
                pc = proj_setup(es, 64)
                cqn = sb(es, "cqn", [128, 3, T], BF16)
                ckvn = sb(es, "ckvn", [128, 2, T], BF16)
                bcq = [Buf(f"cqn{i}") for i in range(5)]
                bckv = [Buf(f"ckvn{i}") for i in range(5)]
                ch = []
                for c in range(3):
                    w, bw = load_w_chunk(pc.wring, wd, [(c * 128, 128)], 8)
                    g, bg = gain_tile(es, [mla_qa_gain[0, c * 128:(c + 1) * 128]])
                    ch.append((w, bw, g, bg))
                proj_group(pc, hT, hb, 8, ch, tbs_all, "all", None, [("sbuf", cqn[:, c, :], bcq) for c in range(3)])
                ch = []
                for c in range(2):
                    w, bw = load_w_chunk(pc.wring, wd, [(384 + c * 128, 128)], 8)
                    g, bg = gain_tile(es, [mla_kva_gain[0, c * 128:(c + 1) * 128]])
                    ch.append((w, bw, g, bg))
                proj_group(pc, hT, hb, 8, ch, tbs_all, "all", None, [("sbuf", ckvn[:, c, :], bckv) for c in range(2)])
                w, bw = load_w_chunk(pc.wring, wd, [(640, 64), (640, 64)], 8)
                g, bg = gain_tile(es, [mla_k_gain[0, 128:192], mla_k_gain[0, 128:192]])
                proj_group(pc, hT, hb, 8, [(w, bw, g, bg)], tbs_all, 64, 64, [("dram", Ks[8])])
                wq = mla_wuq[0].rearrange("(c p) n -> p c n", p=128)
                wkv = mla_wukv[0].rearrange("(c p) n -> p c n", p=128)
                gqn, bgqn = gain_tile(es, [mla_q_gain[0, 0:128]])
                gqp, bgqp = gain_tile(es, [mla_q_gain[0, 128:192], mla_q_gain[0, 128:192]])
                gkn, bgkn = gain_tile(es, [mla_k_gain[0, 0:128]])
                for h in range(8):
                    w, bw = load_w_chunk(pc.wring, wq, [(h * 192, 128)], 3)
                    proj_group(pc, cqn, bcq, 3, [(w, bw, gqn, bgqn)], tbs_all, 128, None, [("dram", Qs[h])])
                for c in range(4):
                    w, bw = load_w_chunk(pc.wring, wq, [(2 * c * 192 + 128, 64), ((2 * c + 1) * 192 + 128, 64)], 3)
                    proj_group(pc, cqn, bcq, 3, [(w, bw, gqp, bgqp)], tbs_all, 64, 64, [("dram", Qs[8 + c])])
                for h in range(8):
                    w, bw = load_w_chunk(pc.wring, wkv, [(h * 256, 128)], 2)
                    proj_group(pc, ckvn, bckv, 2, [(w, bw, gkn, bgkn)], tbs_all, 128, None, [("dram", Ks[h])])
                proj_v(es, pc, ckvn, bckv, 2, wkv, [(h * 256 + 128, 128) for h in range(8)], tbs_all)

            if dbg and L == nlayers - 1:
                bd = Buf("dbgh")
                dma(SP, dbg_hT[:, :, :], hT[:], hb, [], bd)
                bd2 = Buf("dbgm")
                dma(SP, dbg_modF[:, :, :, :], modF[:], [b_modF], [], bd2)

        run_phase(phA)

        def phBC(es0):
            AO = sb(es0, "AO", [128, 8, T], BF16)
            aob = Buf("AO")

            def phB(es):
                qbs = (CTX_QB if need_ctx else []) + LAT_QB
                units = []
                if kind == 0:
                    scale = 128 ** -0.5
                    for h in range(8):
                        kvh = h // 4
                        units.append(dict(c=h, loads=[(("q", h), Qs[h]), (("k", kvh), Ks[kvh])], vloads=[(("v", kvh), kvh * 128, 128, 0)],
                                          subs=[dict(pairs=[(("k", kvh), ("q", h), 0, 128)], v=("v", kvh), ones=ones128, acc=0)],
                                          qblocks=qbs, fin=fin_default(), AO=AO, aob=aob))
                    attn_phase(es, AO, aob, units, scale)
                elif kind == 1:
                    scale = 192 ** -0.5
                    for h in range(8):
                        r0 = (h % 2) * 64
                        units.append(dict(c=h, loads=[(("q", h), Qs[h]), (("qp", h // 2), Qs[8 + h // 2]), (("k", h), Ks[h]), (("kp", 0), Ks[8])],
                                          vloads=[(("v", h), h * 128, 128, 0)],
                                          subs=[dict(pairs=[(("k", h), ("q", h), 0, 128), (("kp", 0), ("qp", h // 2), r0, 64)], v=("v", h), ones=ones128, acc=0)],
                                          qblocks=qbs, fin=fin_default(), AO=AO, aob=aob))
                    attn_phase(es, AO, aob, units, scale)
                elif kind == 2:
                    scale = 64 ** -0.5
                    esk = sb(es, "esk", [128, 8], F32)
                    besk = Buf("esk")
                    sv = win_sink[0].rearrange("(c two) -> two c", two=2)
                    dma(SP, esk[0:64, :], sv[0:1, :].partition_broadcast(64), [], [besk], besk, allow_slow_non_contiguous=True)
                    dma(SP, esk[64:128, :], sv[1:2, :].partition_broadcast(64), [], [besk], besk, allow_slow_non_contiguous=True)
                    act(esk[:], esk[:], AF.Exp, [besk], [besk], joint=False)

                    def mk_kfn(qb):
                        def kfn():
                            i0 = 4 * qb
                            res = [(0, None), (1, None)]
                            for j in range(max(0, i0 - 1), min(15, i0 + 4) + 1):
                                res.append((2 + j, j - i0 + 1))
                            return res
                        return kfn
                    wqbs = (CTX_QB if need_ctx else []) + [(256 + 512 * i, 512, mk_kfn(i)) for i in range(4)]
                    for c in range(8):
                        kvh = c // 4
                        subs = []
                        for half in range(2):
                            subs.append(dict(pairs=[(("k", kvh), ("q", c), half * 64, 64)], v=("v", kvh, half), ones=(ones_lo if half == 0 else ones_hi), acc=0))
                        units.append(dict(c=c, loads=[(("q", c), Qs[c]), (("k", kvh), Ks[kvh])],
                                          vloads=[(("v", kvh, 0), kvh * 64, 64, 0), (("v", kvh, 1), kvh * 64, 64, 64)],
                                          subs=subs, qblocks=wqbs, fin=fin_default((esk, besk)), AO=AO, aob=aob))
                    attn_phase(es, AO, aob, units, scale, masks=True)
                else:
                    scale = 64 ** -0.5
                    lam_init = 0.8 - 0.6 * math.exp(-0.3 * L)
                    lp = sb(es, "lp", [128, 4, 64], F32)
                    blp = Buf("lp")
                    dma(SP, lp[:], diff_lambda[0:1].partition_broadcast(128), [], [blp], blp)
                    lpp = sb(es, "lpp", [128, 2, 64], F32)
                    lsum = sb(es, "lsum", [128, 2], F32)
                    nlam = sb(es, "nlam", [128, 1], F32)
                    bl2 = Buf("lpp")
                    bl3 = Buf("lsum")
                    bnl = Buf("nlam")
                    tt(lpp[:, 0, :], lp[:, 0, :], lp[:, 1, :], ALU.mult, [blp], [bl2])
                    tt(lpp[:, 1, :], lp[:, 2, :], lp[:, 3, :], ALU.mult, [blp], [bl2])
                    P.op(DVE, lambda e: e.reduce_sum(out=lsum[:], in_=lpp[:], axis=AX.X), reads=[bl2], writes=[bl3])
                    act(lsum[:], lsum[:], AF.Exp, [bl3], [bl3], joint=False)
                    tt(nlam[:], lsum[:, 1:2], lsum[:, 0:1], ALU.subtract, [bl3], [bnl], joint=False)
                    ts(nlam[:], nlam[:], -lam_init, None, ALU.add, None, [bnl], [bnl], joint=False)
                    sg, bsg = gain_tile(es, [diff_subln[0]])
                    ts(sg[:], sg[:], 1.0 - lam_init, None, ALU.mult, None, [bsg], [bsg], joint=False)

                    def fin_diff(u, q0, qn, O, Lp, R):
                        r0_, br0 = R["ftm"][0].next()
                        r1_, br1 = R["ftm"][1].next()
                        o_, bo = R["ftm"][2].next()
                        recip(r0_[:, 0:qn], Lp[0][0][:, 0:qn], [Lp[0][1]], [br0], joint=False)
                        recip(r1_[:, 0:qn], Lp[1][0][:, 0:qn], [Lp[1][1]], [br1], joint=False)
                        tt(r0_[:, 0:qn], O[0][0][:, 0:qn], r0_[:, 0:qn], ALU.mult, [O[0][1], br0], [br0], joint=False)
                        tt(r1_[:, 0:qn], O[1][0][:, 0:qn], r1_[:, 0:qn], ALU.mult, [O[1][1], br1], [br1], joint=False)
                        stt(o_[:, 0:qn], r1_[:, 0:qn], nlam[:, 0:1], r0_[:, 0:qn], ALU.mult, ALU.add, [br0, br1, bnl], [bo], joint=False)
                        sq, bsq = R["fsq"].next()
                        act(sq[:, 0:qn], o_[:, 0:qn], AF.Square, [bo], [bsq], joint=False)
                        fp, bfp = R["fpr"].next()
                        mm(fp[:, 0:qn], ones128, sq[:, 0:qn], True, True, [bsq, cst], bfp)
                        act(r0_[:, 0:qn], fp[:, 0:qn], AF.Sqrt, [bfp, cst], [br0], joint=False, scale=1.0 / 128, bias=epsT[:, 0:1])
                        recip(r0_[:, 0:qn], r0_[:, 0:qn], [br0], [br0], joint=False)
                        stt(u["AO"][:, u["c"], q0:q0 + qn], o_[:, 0:qn], sg[:, 0:1], r0_[:, 0:qn], ALU.mult, ALU.mult, [bo, br0, bsg], [u["aob"]], joint=True)

                    for h in range(8):
                        subs = [dict(pairs=[(("k", h), ("q", h), m * 64, 64)], v=("v", h), ones=ones128, acc=m) for m in range(2)]
                        units.append(dict(c=h, loads=[(("q", h), Qs[h]), (("k", h), Ks[h])], vloads=[(("v", h), h * 128, 128, 0)],
                                          subs=subs, qblocks=qbs, fin=fin_diff, AO=AO, aob=aob))
                    attn_phase(es, AO, aob, units, scale)

            run_phase(phB)
            if dbg and L == nlayers - 1:
                def phBd(es):
                    bd = Buf("dbga")
                    dma(SP, dbg_AO[:, :, :], AO[:], [aob], [], bd)
                run_phase(phBd)

            def phC(es):
                wo_d = [gqa_wo, mla_wo, win_wo, diff_wo][kind][0].rearrange("(c p) n -> p c n", p=128)
                wo = sb(es, "wo", [128, 8, D], BF16)
                bwo = Buf("wo")
                for hh in range(2):
                    dma(POOL, wo[:, hh * 4:(hh + 1) * 4, :], wo_d[:, hh * 4:(hh + 1) * 4, :], [], [bwo], bwo)
                grow, bgrow = load_gate_rows(es, L, 2)
                opr = Ring(es, nc, "cps", [128, 512], F32, 4, psum=True)
                tmr = Ring(es, nc, "ctmp", [128, 512], F32, 3)
                tl = list(range(NT)) if need_ctx else list(range(2, NT))
                for t in tl:
                    r = 1 if t < 2 else 0
                    for dh in range(2):
                        ps, bps = opr.next()
                        for c in range(8):
                            mm(ps[:, :], AO[:, c, t * 128:(t + 1) * 128], wo[:, c, dh * 512:(dh + 1) * 512], c == 0, c == 7, [aob, bwo], bps)
                        tm, btm = tmr.next()
                        tt(tm[:], ps[:, :], grow[:, r, dh * 512:(dh + 1) * 512], ALU.mult, [bps, bgrow], [btm], joint=False)
                        xs = x_sb[:, t, dh * 512:(dh + 1) * 512]
                        tt(xs, tm[:], xs, ALU.add, [btm, xb[t]], [xb[t]], joint=False)
            run_phase(phC)
            if dbg and L == nlayers - 1:
                def phCd(es):
                    for t in range(NT):
                        dma(SP, dbg_xa[:, t, :], x_sb[:, t, :], [xb[t]], [], xb[t])
                run_phase(phCd)

        with ExitStack() as es0:
            phBC(es0)

        tbs_f = tbs_all if need_ctx else tbs_lat
        with ExitStack() as es0:
            hT = sb(es0, "hTf", [128, 8, T], BF16)
            hb = [Buf(f"hTf{i}") for i in range(5)]
            comb = sb(es0, "comb", [128, NT, 8], F32)
            b_comb = Buf("comb")

            def phD(es):
                if moe:
                    lg = sb(es, "lg", [128, NT, 8], F32)
                    blg = Buf("lg")
                    P.op(DVE, lambda e: e.memset(lg[:], 0.0), writes=[blg])
                    m = dict(router=moe_router[L // 2], lg=lg, blg=blg)
                    norm_phase(es, L, 1, hT, hb, tbs_f, moe=m)
                    route(es, m, comb, b_comb)
                else:
                    norm_phase(es, L, 1, hT, hb, tbs_f)
            run_phase(phD)

            def phE(es):
                if moe:
                    experts = [(moe_w13[L // 2, e], moe_w2[L // 2, e]) for e in range(8)]
                    ffn_phase(es, L, hT, hb, tbs_f, experts, comb, b_comb)
                else:
                    ffn_phase(es, L, hT, hb, tbs_f, [(ffn_w13[L // 2], ffn_w2[L // 2])], None, None)
            run_phase(phE)

    run_phase(phase0)
    for L in range(nlayers):
        std_layer(L)

    def phase_out(es):
        for t in range(16):
            dma(SP, out_d[t * 128:(t + 1) * 128, :], x_sb[:, 2 + t, :], [xb[2 + t]], [], xb[2 + t])
    run_phase(phase_out)
    top.close()
    return nc


_CACHE = {}


def kernel(**inputs):
    nl = int(inputs.pop("_nlayers", 4))
    dbg = bool(inputs.pop("_dbg", False))
    ncores = int(inputs.pop("_ncores", 8))
    trace = bool(inputs.pop("_trace", False))
    if (nl, dbg) not in _CACHE:
        _CACHE[(nl, dbg)] = build(nl, dbg)
    nc = _CACHE[(nl, dbg)]
    consts = _consts()
    shared = {}
    for k in ["ada_w", "ada_b", "norm_mix", "norm_ffn", "gqa_wqkv", "gqa_q_gain", "gqa_k_gain", "gqa_wo", "mla_wdown",
              "mla_qa_gain", "mla_kva_gain", "mla_wuq", "mla_wukv", "mla_q_gain", "mla_k_gain", "mla_wo", "win_wqkv",
              "win_q_gain", "win_k_gain", "win_sink", "win_wo", "diff_wqkv", "diff_q_gain", "diff_k_gain", "diff_lambda",
              "diff_subln", "diff_wo", "ffn_w13", "ffn_w2", "moe_router", "moe_w13", "moe_w2"]:
        shared[k] = np.ascontiguousarray(np.asarray(inputs[k], dtype=np.float32))
        if nl < 2 and k in ("moe_w13", "moe_w2"):
            shared[k] = np.zeros((1, 1, 8, 8), np.float32)
    shared.update(consts)
    x = np.asarray(inputs["x"], dtype=np.float32)
    c = np.asarray(inputs["c"], dtype=np.float32)
    ctx = np.asarray(inputs["ctx"], dtype=np.float32)
    c_ctx = np.asarray(inputs["c_ctx"], dtype=np.float32)
    in_maps = []
    for b in range(ncores):
        m = dict(shared)
        m["x"] = np.ascontiguousarray(x[b])
        m["ctx"] = np.ascontiguousarray(ctx[b])
        cfm = np.stack([c[b].reshape(8, 128).T, c_ctx.reshape(8, 128).T], axis=-1)
        m["cfm"] = np.ascontiguousarray(cfm.astype(np.float32))
        in_maps.append(m)
    if trace:
        res = run_bass_kernel_spmd(nc, in_maps, core_ids=list(range(ncores)), trace=True)
        print("EXEC_NS", res.exec_time_ns, flush=True)
        try:
            sc = res.per_core_scope_times or {}
            for k_, v_ in sc.items():
                print("SCOPE", k_, v_, flush=True)
        except Exception as e_:
            print("scope err", e_)
    else:
        res = run_bass_kernel_spmd(nc, in_maps, core_ids=list(range(ncores)))
    if dbg:
        return res.results
    return np.stack([r["out"] for r in res.results], axis=0).astype(np.float32)
```

```python
import math
from contextlib import ExitStack
import numpy as np
import ml_dtypes
import concourse.bass as bass
import concourse.mybir as mybir
from concourse.bass_utils import run_bass_kernel_spmd

F32 = mybir.dt.float32
BF16 = mybir.dt.bfloat16
AF = mybir.ActivationFunctionType
ALU = mybir.AluOpType
AX = mybir.AxisListType
PE, ACT, DVE, POOL, SP = "tensor", "scalar", "vector", "gpsimd", "sync"
ENGS = (PE, ACT, DVE, POOL, SP)

D = 1024
T = 2304
NT = 18
CTX = 256
EPS = 1e-6
TBS = [(0, 256)] + [(256 + 512 * i, 512) for i in range(4)]
N_DMA_SEMS = 72


class Buf:
    __slots__ = ("name", "writers", "readers", "dsem", "excl")

    def __init__(self, name, excl=False):
        self.name = name
        self.writers = []
        self.readers = []
        self.dsem = None
        self.excl = excl


class Op:
    __slots__ = ("eng", "fn", "deps", "is_dma", "sig", "sigval", "waits", "id", "dsem_idx")


class Prog:
    def __init__(self):
        self.ops = []
        self.n_dsem = 0
        self.free_dsems = []
        self.live = []
        self.phase_dma = []
        self.last_op = {}
        self.emitted = 0
        self.cnt = {e: 0 for e in ENGS}
        self.seen = {e: {} for e in ENGS}

    def _new(self, eng, fn, is_dma):
        o = Op()
        o.id = len(self.ops)
        o.eng = eng
        o.fn = fn
        o.is_dma = is_dma
        o.sig = False
        o.sigval = None
        o.waits = None
        o.dsem_idx = None
        o.deps = set()
        return o

    def op(self, eng, fn, reads=(), writes=(), joint=False, dma_buf=None):
        o = self._new(eng, fn, dma_buf is not None)
        deps = o.deps
        for b in reads:
            deps.update(b.writers)
            if b.excl:
                deps.update(b.readers)
        for b in writes:
            deps.update(b.readers)
            if not (joint and not b.readers):
                deps.update(b.writers)
        for b in reads:
            self._add(b.readers, o)
        for b in writes:
            if b.readers or not joint:
                b.writers = [o.id]
                b.readers = []
            else:
                self._add(b.writers, o)
        if o.is_dma:
            if dma_buf.dsem is None:
                if self.free_dsems:
                    dma_buf.dsem = self.free_dsems.pop()
                else:
                    dma_buf.dsem = [self.n_dsem, 0]
                    self.n_dsem += 1
                    assert self.n_dsem <= N_DMA_SEMS, "out of DMA semaphores"
                self.live.append(dma_buf)
            dma_buf.dsem[1] += 16
            o.sigval = dma_buf.dsem[1]
            o.dsem_idx = dma_buf.dsem[0]
            self.phase_dma.append(o.id)
        else:
            self.last_op[eng] = o.id
        self.ops.append(o)
        return o

    def _add(self, lst, o):
        if not o.is_dma:
            for i, pid in enumerate(lst):
                p = self.ops[pid]
                if (not p.is_dma) and p.eng == o.eng:
                    lst[i] = o.id
                    return
        lst.append(o.id)

    def barrier(self):
        last = dict(self.last_op)
        dmas = list(self.phase_dma)
        for e in ENGS:
            o = self._new(e, None, False)
            o.deps = set(dmas)
            for e2, oid in last.items():
                if e2 != e:
                    o.deps.add(oid)
            self.ops.append(o)
        self.phase_dma = []
        for b in self.live:
            self.free_dsems.append(b.dsem)
            b.dsem = None
        self.live = []

    def emit(self, block, sems):
        ops = self.ops
        s0 = self.emitted
        new = ops[s0:]
        for o in new:
            o.deps = {d for d in o.deps if d >= s0}
            for d in o.deps:
                p = ops[d]
                if p.is_dma:
                    continue
                if p.eng == PE and o.eng == PE and not o.is_dma:
                    continue
                p.sig = True
        for o in new:
            if o.is_dma:
                continue
            if o.sig:
                self.cnt[o.eng] += 1
                o.sigval = self.cnt[o.eng]
        for o in new:
            w = {}
            for d in o.deps:
                p = ops[d]
                if p.is_dma:
                    key = ("dma", p.dsem_idx)
                else:
                    if p.eng == PE and o.eng == PE and not o.is_dma:
                        continue
                    key = ("eng", p.eng)
                if w.get(key, 0) < p.sigval:
                    w[key] = p.sigval
            s = self.seen[o.eng]
            o.waits = []
            for key, v in w.items():
                if s.get(key, 0) >= v:
                    continue
                s[key] = v
                o.waits.append((key, v))
        per = {e: [] for e in ENGS}
        for o in new:
            per[o.eng].append(o)

        def run(name, eng):
            for o in per[name]:
                for key, v in o.waits:
                    sem = sems["dma"][key[1]] if key[0] == "dma" else sems[key[1]]
                    eng.wait_ge(sem, v)
                if o.fn is None:
                    continue
                ins = o.fn(eng)
                if ins is None:
                    continue
                if o.is_dma:
                    ins.then_inc(sems["dma"][o.dsem_idx], 16)
                elif o.sig:
                    ins.then_inc(sems[o.eng], 1)

        block.tensor(lambda e: run(PE, e))
        block.scalar(lambda e: run(ACT, e))
        block.vector(lambda e: run(DVE, e))
        block.gpsimd(lambda e: run(POOL, e))
        block.sync(lambda e: run(SP, e))
        self.emitted = len(ops)
        for o in new:
            o.fn = None


_uid = [0]


def uname(s):
    _uid[0] += 1
    return f"{s}_{_uid[0]}"


class Ring:
    def __init__(self, es, nc, name, shape, dtype, n, psum=False):
        self.items = []
        for i in range(n):
            mk = nc.psum_tensor if psum else nc.sbuf_tensor
            t = es.enter_context(mk(uname(name), list(shape), dtype))
            self.items.append((t, Buf(name + str(i), excl=psum)))
        self.i = 0

    def next(self):
        it = self.items[self.i % len(self.items)]
        self.i += 1
        return it


def _rope_tables(g):
    half = g // 2
    quarter = g // 4
    inv = (10000.0 ** (-np.arange(quarter, dtype=np.float32) / quarter)).astype(np.float32)
    s = np.arange(2048)
    row = (s // 64).astype(np.float32)
    col = (s % 64).astype(np.float32)
    ang = np.concatenate([row[:, None] * inv[None, :], col[:, None] * inv[None, :]], axis=1).astype(np.float32)
    cos = np.cos(ang).astype(np.float32)
    sin = np.sin(ang).astype(np.float32)
    cosF = np.ones((128, T), np.float32)
    sinF = np.zeros((128, T), np.float32)
    for p in range(128):
        i = p % g
        j = i % half
        cosF[p, CTX:] = cos[:, j]
        sinF[p, CTX:] = -sin[:, j] if i < half else sin[:, j]
    return cosF, sinF


def _consts():
    c = {}
    c["identF"] = np.eye(128, dtype=np.float32)
    ones_bd = np.zeros((128, 128), np.float32)
    ones_bd[:64, :64] = 1
    ones_bd[64:, 64:] = 1
    ones_lo = np.zeros((128, 128), np.float32)
    ones_lo[:, :64] = 1
    ones_hi = np.zeros((128, 128), np.float32)
    ones_hi[:, 64:] = 1
    perm128 = np.zeros((128, 128), np.float32)
    perm64 = np.zeros((128, 128), np.float32)
    for m in range(128):
        perm128[(m + 64) % 128, m] = 1
        i = m % 64
        perm64[(m - i) + (i + 32) % 64, m] = 1
    mats = np.stack([np.ones((128, 128), np.float32), ones_bd, ones_lo, ones_hi, perm128, perm64], axis=1)
    c["matsB"] = mats.astype(ml_dtypes.bfloat16)
    c128, s128 = _rope_tables(128)
    c64, s64 = _rope_tables(64)
    c["rope"] = np.stack([c128, s128, c64, s64], axis=0)
    mask = np.zeros((128, 6, 512), np.float32)
    kp = np.arange(128)[:, None]
    q = np.arange(512)[None, :]
    for rel in range(6):
        mask[:, rel, :] = (np.abs(q - (rel - 1) * 128 - kp) <= 128).astype(np.float32)
    c["wmask"] = mask.astype(ml_dtypes.bfloat16)
    return c


def build(nlayers=4, dbg=False):
    nc = bass.Bass("TRN2", target_bir_lowering=False)
    big = nlayers >= 2

    def din(name, shape, dt=F32):
        return nc.dram_tensor(name, list(shape), dt, kind="ExternalInput").ap()

    x_in = din("x", [2048, D])
    ctx_in = din("ctx", [CTX, D])
    cfm_in = din("cfm", [128, 8, 2])
    ada_w = din("ada_w", [4, D, 6 * D])
    ada_b = din("ada_b", [4, 6 * D])
    norm_mix = din("norm_mix", [4, D])
    norm_ffn = din("norm_ffn", [4, D])
    gqa_wqkv = din("gqa_wqkv", [1, D, 1536])
    gqa_q_gain = din("gqa_q_gain", [1, 128])
    gqa_k_gain = din("gqa_k_gain", [1, 128])
    gqa_wo = din("gqa_wo", [1, D, D])
    mla_wdown = din("mla_wdown", [1, D, 704])
    mla_qa_gain = din("mla_qa_gain", [1, 384])
    mla_kva_gain = din("mla_kva_gain", [1, 256])
    mla_wuq = din("mla_wuq", [1, 384, 1536])
    mla_wukv = din("mla_wukv", [1, 256, 2048])
    mla_q_gain = din("mla_q_gain", [1, 192])
    mla_k_gain = din("mla_k_gain", [1, 192])
    mla_wo = din("mla_wo", [1, D, D])
    win_wqkv = din("win_wqkv", [1, D, 1280])
    win_q_gain = din("win_q_gain", [1, 64])
    win_k_gain = din("win_k_gain", [1, 64])
    win_sink = din("win_sink", [1, 16])
    win_wo = din("win_wo", [1, D, D])
    diff_wqkv = din("diff_wqkv", [1, D, 3072])
    diff_q_gain = din("diff_q_gain", [1, 64])
    diff_k_gain = din("diff_k_gain", [1, 64])
    diff_lambda = din("diff_lambda", [1, 4, 64])
    diff_subln = din("diff_subln", [1, 128])
    diff_wo = din("diff_wo", [1, D, D])
    ffn_w13 = din("ffn_w13", [2, D, 7168])
    ffn_w2 = din("ffn_w2", [2, 3584, D])
    moe_router = din("moe_router", [2, D, 8])
    moe_w13 = din("moe_w13", [2, 8, D, 7168] if big else [1, 1, 8, 8])
    moe_w2 = din("moe_w2", [2, 8, 3584, D] if big else [1, 1, 8, 8])
    identF_in = din("identF", [128, 128])
    matsB_in = din("matsB", [128, 6, 128], BF16)
    rope_in = din("rope", [4, 128, T])
    wmask_in = din("wmask", [128, 6, 512], BF16)
    out_d = nc.dram_tensor("out", [2048, D], F32, kind="ExternalOutput").ap()
    skind = "ExternalOutput" if dbg else "Internal"
    modrows = nc.dram_tensor("modrows", [4, 2, 6 * D], F32, kind=skind).ap()
    Qs = nc.dram_tensor("Qs", [12, 128, T], BF16, kind=skind).ap()
    Ks = nc.dram_tensor("Ks", [9, 128, T], BF16, kind=skind).ap()
    Vs = nc.dram_tensor("Vs", [T, 1024], BF16, kind=skind).ap()
    if dbg:
        dbg_hT = nc.dram_tensor("dbg_hT", [128, 8, T], BF16, kind="ExternalOutput").ap()
        dbg_AO = nc.dram_tensor("dbg_AO", [128, 8, T], BF16, kind="ExternalOutput").ap()
        dbg_xa = nc.dram_tensor("dbg_xa", [128, NT, D], F32, kind="ExternalOutput").ap()
        dbg_modF = nc.dram_tensor("dbg_modF", [128, 4, 2, 48], F32, kind="ExternalOutput").ap()

    P = Prog()
    top = ExitStack()
    sems = {e: top.enter_context(nc.semaphore("s_" + e)) for e in ENGS}
    sems["dma"] = [top.enter_context(nc.semaphore(f"sd{i}")) for i in range(N_DMA_SEMS)]

    def sb(es, name, shape, dt):
        return es.enter_context(nc.sbuf_tensor(uname(name), list(shape), dt))

    x_sb = sb(top, "x_sb", [128, NT, D], F32)
    xb = [Buf(f"x{t}") for t in range(NT)]
    identF = sb(top, "identF", [128, 128], F32)
    matsB = sb(top, "matsB", [128, 6, 128], BF16)
    epsT = sb(top, "epsT", [128, 1], F32)
    modF = sb(top, "modF", [128, 4, 2, 48], F32)
    cst = Buf("consts")
    b_modF = Buf("modF")
    ones128 = matsB[:, 0, :]
    ones_bd = matsB[:, 1, :]
    ones_lo = matsB[:, 2, :]
    ones_hi = matsB[:, 3, :]
    perm128 = matsB[:, 4, :]
    perm64 = matsB[:, 5, :]

    def run_phase(fn, name=None):
        with ExitStack() as es:
            fn(es)
            P.barrier()
            with nc.named_scope(uname(name or getattr(fn, "__name__", "ph"))):
                with nc.Block() as block:
                    P.emit(block, sems)

    def dma(eng, out, in_, reads, writes, sbuf, joint=True, **kw):
        return P.op(eng, lambda e: e.dma_start(out=out, in_=in_, **kw), reads=reads, writes=writes, joint=joint, dma_buf=sbuf)

    def mm(out, lhsT, rhs, start, stop, reads, wbuf):
        return P.op(PE, lambda e: e.matmul(out, lhsT=lhsT, rhs=rhs, start=start, stop=stop), reads=reads, writes=[wbuf], joint=not start)

    def act(out, in_, func, reads, writes, joint=True, **kw):
        return P.op(ACT, lambda e: e.activation(out=out, in_=in_, func=func, **kw), reads=reads, writes=writes, joint=joint)

    def tt(out, in0, in1, op, reads, writes, joint=True, eng=DVE):
        return P.op(eng, lambda e: e.tensor_tensor(out=out, in0=in0, in1=in1, op=op), reads=reads, writes=writes, joint=joint)

    def ts(out, in0, s1, s2, op0, op1, reads, writes, joint=True, eng=DVE):
        if s2 is None:
            return P.op(eng, lambda e: e.tensor_scalar(out=out, in0=in0, scalar1=s1, scalar2=None, op0=op0), reads=reads, writes=writes, joint=joint)
        return P.op(eng, lambda e: e.tensor_scalar(out=out, in0=in0, scalar1=s1, scalar2=s2, op0=op0, op1=op1), reads=reads, writes=writes, joint=joint)

    def stt(out, in0, scalar, in1, op0, op1, reads, writes, joint=True, eng=DVE):
        return P.op(eng, lambda e: e.scalar_tensor_tensor(out=out, in0=in0, scalar=scalar, in1=in1, op0=op0, op1=op1), reads=reads, writes=writes, joint=joint)

    def recip(out, in_, reads, writes, joint=True):
        return P.op(DVE, lambda e: e.reciprocal(out=out, in_=in_), reads=reads, writes=writes, joint=joint)

    def frecip(out, in_, reads, writes, joint=False):
        return P.op(DVE, lambda e: e.reciprocal(out=out, in_=in_), reads=reads, writes=writes, joint=joint)

    def tiles_of(tb):
        t0, n = tb
        return list(range(t0 // 128, (t0 + n) // 128))

    def phase0(es):
        for t in range(2):
            dma(SP, x_sb[:, t, :], ctx_in[t * 128:(t + 1) * 128, :], [], [xb[t]], xb[t])
        for t in range(16):
            dma(SP, x_sb[:, 2 + t, :], x_in[t * 128:(t + 1) * 128, :], [], [xb[2 + t]], xb[2 + t])
        dma(SP, identF[:], identF_in[:, :], [], [cst], cst)
        dma(SP, matsB[:], matsB_in[:, :, :], [], [cst], cst)
        P.op(DVE, lambda e: e.memset(epsT[:], EPS), writes=[cst], joint=True)
        cfm = sb(es, "cfm", [128, 8, 2], F32)
        silu2 = sb(es, "silu2", [128, 8, 2], F32)
        b_c = Buf("cfm")
        b_s = Buf("silu2")
        dma(SP, cfm[:], cfm_in[:, :, :], [], [b_c], b_c)
        act(silu2[:], cfm[:], AF.Silu, [b_c], [b_s])
        ones2 = sb(es, "ones2", [1, 2], F32)
        b_o2 = Buf("ones2")
        P.op(DVE, lambda e: e.memset(ones2[:], 1.0), writes=[b_o2])
        wr = Ring(es, nc, "adaw", [128, 8, 512], F32, 2)
        br = Ring(es, nc, "adab", [1, 512], F32, 2)
        rows = sb(es, "rows", [2, 6 * D], F32)
        b_rows = Buf("rows")
        pr = Ring(es, nc, "p0ps", [128, 512], F32, 2, psum=True)
        pt = Ring(es, nc, "p0pt", [128, 512], F32, 1, psum=True)
        for L in range(nlayers):
            for cb in range(12):
                w, bw = wr.next()
                bt, bbt = br.next()
                dma(SP, w[:], ada_w[L].rearrange("(c p) n -> p c n", p=128)[:, :, cb * 512:(cb + 1) * 512], [], [bw], bw)
                dma(SP, bt[:], ada_b[L:L + 1, cb * 512:(cb + 1) * 512], [], [bbt], bbt)
                ps, bps = pr.next()
                for kc in range(8):
                    mm(ps[0:2, :], silu2[:, kc, :], w[:, kc, :], kc == 0, False, [b_s, bw], bps)
                mm(ps[0:2, :], ones2[:, :], bt[:, :], False, True, [b_o2, bbt], bps)
                P.op(DVE, lambda e, ps=ps, cb=cb: e.tensor_copy(out=rows[:, cb * 512:(cb + 1) * 512], in_=ps[0:2, :]),
                     reads=[bps], writes=[b_rows], joint=True)
            dma(SP, modrows[L], rows[:, :], [b_rows], [], b_rows)
            tp, btp = pt.next()
            for j in range(48):
                P.op(PE, lambda e, j=j, tp=tp: e.transpose(out=tp[:, 2 * j:2 * j + 2], in_=rows[0:2, j * 128:(j + 1) * 128], identity=identF[0:2, 0:2]),
                     reads=[b_rows, cst], writes=[btp], joint=(j > 0))
            P.op(DVE, lambda e, tp=tp, L=L: e.tensor_copy(out=modF[:, L, :, :].rearrange("p r j -> p j r"), in_=tp[:, 0:96].rearrange("p (j r) -> p j r", r=2)),
                 reads=[btp], writes=[b_modF], joint=True)

    def norm_phase(es, L, which, hT, hb, tbs, moe=None):
        gsrc = (norm_mix if which == 0 else norm_ffn)
        gF = sb(es, "gF", [128, 8], F32)
        AB = sb(es, "AB", [128, 2, 2, 8], F32)
        b_g = Buf("gF")
        b_AB = Buf("AB")
        dma(SP, gF[:], gsrc[L].rearrange("(c p) -> p c", p=128), [], [b_g], b_g, allow_slow_non_contiguous=True)
        ish, isc = (0, 1) if which == 0 else (3, 4)
        for r in range(2):
            stt(AB[:, r, 0, :], modF[:, L, r, isc * 8:(isc + 1) * 8], 1.0, gF[:], ALU.add, ALU.mult, [b_modF, b_g], [b_AB])
            P.op(DVE, lambda e, r=r: e.tensor_copy(out=AB[:, r, 1, :], in_=modF[:, L, r, ish * 8:(ish + 1) * 8]), reads=[b_modF], writes=[b_AB], joint=True)
        junk = sb(es, "junk", [128, D], BF16)
        b_junk = Buf("junk")
        ss = sb(es, "ss", [128, NT], F32)
        rstd = sb(es, "rstd", [128, NT], F32)
        b_ss = [Buf(f"ss{i}") for i in range(5)]
        b_rstd = [Buf(f"rstd{i}") for i in range(5)]
        P.op(DVE, lambda e: e.memset(ss[:], 0.0), writes=b_ss)
        xnr = Ring(es, nc, "xn", [128, 4, D], F32, 1)
        ptr = Ring(es, nc, "nps", [128, 512], F32, 2, psum=True)
        if moe is not None:
            h32r = Ring(es, nc, "h32", [128, 8, 512], F32, 1)
            R32 = sb(es, "R32", [128, 8, 8], F32)
            b_R = Buf("R32")
            dma(SP, R32[:], moe["router"].rearrange("(c p) e -> p c e", p=128), [], [b_R], b_R)
            lgr = Ring(es, nc, "lgps", [128, 8], F32, 2, psum=True)
        for bi, tb in enumerate(TBS):
            if tb not in tbs:
                continue
            t0, n = tb
            tl = tiles_of(tb)
            r = 1 if bi == 0 else 0
            for t in tl:
                act(junk[:], x_sb[:, t, :], AF.Square, [xb[t]], [b_junk, b_ss[bi]], joint=False, accum_out=ss[:, t:t + 1])
            act(rstd[:, tl[0]:tl[-1] + 1], ss[:, tl[0]:tl[-1] + 1], AF.Sqrt, [b_ss[bi], cst], [b_rstd[bi]], joint=False, scale=1.0 / D, bias=epsT[:, 0:1])
            recip(rstd[:, tl[0]:tl[-1] + 1], rstd[:, tl[0]:tl[-1] + 1], [b_rstd[bi]], [b_rstd[bi]], joint=False)
            xn, bxn = xnr.next()
            for j, t in enumerate(tl):
                ts(xn[:, j, :], x_sb[:, t, :], rstd[:, t:t + 1], None, ALU.mult, None, [xb[t], b_rstd[bi]], [bxn])
            if moe is not None:
                h32, bh32 = h32r.next()
            for c in range(8):
                ps, bps = ptr.next()
                for j, t in enumerate(tl):
                    P.op(PE, lambda e, ps=ps, j=j, c=c, xn=xn: e.transpose(out=ps[:, j * 128:(j + 1) * 128], in_=xn[:, j, c * 128:(c + 1) * 128], identity=identF[:]),
                         reads=[bxn, cst], writes=[bps], joint=(j > 0))
                act(hT[:, c, t0:t0 + n], ps[:, 0:n], AF.Identity, [bps, b_AB], [hb[bi]], scale=AB[:, r, 0, c:c + 1], bias=AB[:, r, 1, c:c + 1])
                if moe is not None:
                    ts(h32[:, c, 0:n], ps[:, 0:n], AB[:, r, 0, c:c + 1], AB[:, r, 1, c:c + 1], ALU.mult, ALU.add, [bps, b_AB], [bh32])
            if moe is not None:
                for j, t in enumerate(tl):
                    lp, blp = lgr.next()
                    for c in range(8):
                        mm(lp[:, :], h32[:, c, j * 128:(j + 1) * 128], R32[:, c, :], c == 0, c == 7, [bh32, b_R], blp)
                    P.op(DVE, lambda e, lp=lp, t=t: e.tensor_copy(out=moe["lg"][:, t, :], in_=lp[:, :]), reads=[blp], writes=[moe["blg"]], joint=True)

    def route(es, moe, comb, b_comb):
        lg = moe["lg"]
        blg = moe["blg"]
        m8 = sb(es, "m8", [128, NT, 8], F32)
        tmp = sb(es, "rtmp", [128, NT, 8], F32)
        msk = sb(es, "rmsk", [128, NT, 8], F32)
        den = sb(es, "rden", [128, NT], F32)
        b1, b2, b3, b4 = Buf("m8"), Buf("rtmp"), Buf("rmsk"), Buf("rden")
        for t in range(NT):
            P.op(DVE, lambda e, t=t: e.max(out=m8[:, t, :], in_=lg[:, t, :]), reads=[blg], writes=[b1], joint=True)
        tt(msk[:], lg[:], m8[:, :, 1:2].to_broadcast([128, NT, 8]), ALU.is_ge, [blg, b1], [b3])
        tt(tmp[:], lg[:], m8[:, :, 0:1].to_broadcast([128, NT, 8]), ALU.subtract, [blg, b1], [b2])
        act(tmp[:], tmp[:], AF.Exp, [b2], [b2], joint=False)
        tt(tmp[:], tmp[:], msk[:], ALU.mult, [b2, b3], [b2], joint=False)
        P.op(DVE, lambda e: e.reduce_sum(out=den[:], in_=tmp[:], axis=AX.X), reads=[b2], writes=[b4])
        recip(den[:], den[:], [b4], [b4], joint=False)
        tt(comb[:], tmp[:], den[:].unsqueeze(2).to_broadcast([128, NT, 8]), ALU.mult, [b2, b4], [b_comb], joint=False)

    def load_gate_rows(es, L, idx):
        g = sb(es, "grow", [128, 2, D], F32)
        bg = Buf("grow")
        for r in range(2):
            dma(SP, g[:, r, :], modrows[L, r:r + 1, idx * D:(idx + 1) * D].partition_broadcast(128), [], [bg], bg)
        return g, bg

    def ffn_phase(es, L, hT, hb, tbs, experts, comb, b_comb):
        grow, bgrow = load_gate_rows(es, L, 5)
        w13r = Ring(es, nc, "w13", [128, 8, 1024], BF16, 2)
        w2r = Ring(es, nc, "w2", [128, 4, D], BF16, 2)
        has_ctx = TBS[0] in tbs
        if has_ctx:
            w2cr = Ring(es, nc, "w2c", [128, 4, D], BF16, 2)
        y = sb(es, "y", [128, 4, T], BF16)
        yb = [Buf(f"y{i}") for i in range(5)]
        sgr = Ring(es, nc, "sg", [128, 512], BF16, 3)
        gpr = Ring(es, nc, "gps", [128, 512], F32, 2, psum=True)
        upr = Ring(es, nc, "ups", [128, 512], F32, 2, psum=True)
        opr = Ring(es, nc, "ops", [128, 512], F32, 3, psum=True)
        for ei, (w13, w2) in enumerate(experts):
            w13v = w13.rearrange("(c p) n -> p c n", p=128)
            w2v = w2.rearrange("(c p) n -> p c n", p=128)
            for fb in range(7):
                wa, bwa = w13r.next()
                wb_, bwb = w2r.next()
                dma(POOL, wa[:, :, 0:512], w13v[:, :, fb * 512:(fb + 1) * 512], [], [bwa], bwa)
                dma(POOL, wa[:, :, 512:1024], w13v[:, :, 3584 + fb * 512:3584 + (fb + 1) * 512], [], [bwa], bwa)
                dma(POOL, wb_[:], w2v[:, fb * 4:(fb + 1) * 4, :], [], [bwb], bwb)
                if has_ctx:
                    wc_, bwc = w2cr.next()
                    tt(wc_[:], wb_[:], grow[:, 1, :].unsqueeze(1).to_broadcast([128, 4, D]), ALU.mult, [bwb, bgrow], [bwc], joint=False, eng=POOL)
                tt(wb_[:], wb_[:], grow[:, 0, :].unsqueeze(1).to_broadcast([128, 4, D]), ALU.mult, [bwb, bgrow], [bwb], joint=False, eng=POOL)
                for bi, tb in enumerate(TBS):
                    if tb not in tbs:
                        continue
                    t0, n = tb
                    for fc in range(4):
                        gp, bgp = gpr.next()
                        up, bup = upr.next()
                        for kc in range(8):
                            mm(gp[:, 0:n], wa[:, kc, fc * 128:(fc + 1) * 128], hT[:, kc, t0:t0 + n], kc == 0, kc == 7, [bwa, hb[bi]], bgp)
                        for kc in range(8):
                            mm(up[:, 0:n], wa[:, kc, 512 + fc * 128:512 + (fc + 1) * 128], hT[:, kc, t0:t0 + n], kc == 0, kc == 7, [bwa, hb[bi]], bup)
                        sg, bsg = sgr.next()
                        act(sg[:, 0:n], gp[:, 0:n], AF.Silu, [bgp], [bsg], joint=False)
                        tt(y[:, fc, t0:t0 + n], sg[:, 0:n], up[:, 0:n], ALU.mult, [bsg, bup], [yb[bi]])
                for bi, tb in enumerate(TBS):
                    if tb not in tbs:
                        continue
                    r = 1 if bi == 0 else 0
                    for t in tiles_of(tb):
                        for dh in range(2):
                            op_, bop = opr.next()
                            wsel, bwsel = (wc_, bwc) if r == 1 else (wb_, bwb)
                            for fc in range(4):
                                mm(op_[:, :], y[:, fc, t * 128:(t + 1) * 128], wsel[:, fc, dh * 512:(dh + 1) * 512], fc == 0, fc == 3, [yb[bi], bwsel], bop)
                            xs = x_sb[:, t, dh * 512:(dh + 1) * 512]
                            if comb is None:
                                tt(xs, op_[:, :], xs, ALU.add, [bop, xb[t]], [xb[t]], joint=False)
                            else:
                                stt(xs, op_[:, :], comb[:, t, ei:ei + 1], xs, ALU.mult, ALU.add, [bop, xb[t], b_comb], [xb[t]], joint=False)

    def load_w_chunk(ring, wv, pieces, kc_n):
        w, bw = ring.next()
        off = 0
        for (c0, ncol) in pieces:
            dma(POOL, w[:, 0:kc_n, off:off + ncol], wv[:, :, c0:c0 + ncol], [], [bw], bw)
            off += ncol
        return w, bw

    def gain_tile(es, pieces):
        g = sb(es, "gain", [128, 1], F32)
        bg = Buf("gain")
        off = 0
        for ap in pieces:
            n = ap.shape[0]
            dma(SP, g[off:off + n, :], ap.rearrange("(p o) -> p o", o=1), [], [bg], bg)
            off += n
        return g, bg

    class ProjCtx:
        pass

    def proj_setup(es, need_rope):
        pc = ProjCtx()
        pc.wring = Ring(es, nc, "wch", [128, 8, 128], BF16, 3)
        pc.pps = Ring(es, nc, "pps", [128, 512], F32, 3, psum=True)
        pc.sps = Ring(es, nc, "sps", [128, 512], F32, 1, psum=True)
        pc.rps = Ring(es, nc, "rps", [128, 512], F32, 1, psum=True)
        pc.sq = Ring(es, nc, "sq", [128, 512], BF16, 3)
        pc.qg = Ring(es, nc, "qg", [128, 512], BF16, 3)
        pc.rstd = Ring(es, nc, "prstd", [128, 512], F32, 3)
        pc.t1 = Ring(es, nc, "pt1", [128, 512], F32, 1)
        pc.t2 = Ring(es, nc, "pt2", [128, 512], F32, 1)
        pc.ob = Ring(es, nc, "pob", [128, 512], BF16, 3)
        pc.rope = None
        if need_rope is not None:
            pc.rope = sb(es, "rope", [128, 2, T], F32)
            pc.b_rope = Buf("rope")
            i0 = 0 if need_rope == 128 else 2
            for k in range(2):
                dma(SP, pc.rope[:, k, :], rope_in[i0 + k], [], [pc.b_rope], pc.b_rope)
        return pc

    def proj_group(pc, src, src_bufs, kc_n, chunks, tbs, group, rope, dsts):
        nch = len(chunks)
        for bi, tb in enumerate(TBS):
            if tb not in tbs:
                continue
            t0, n = tb
            pss = []
            for (w, bw, g, bg) in chunks:
                ps, bps = pc.pps.next()
                for kc in range(kc_n):
                    mm(ps[:, 0:n], w[:, kc, :], src[:, kc, t0:t0 + n], kc == 0, kc == kc_n - 1, [bw, src_bufs[bi]], bps)
                pss.append((ps, bps))
            qgs = []
            sp, bsp = pc.sps.next()
            for ci, (ps, bps) in enumerate(pss):
                (w, bw, g, bg) = chunks[ci]
                sq, bsq = pc.sq.next()
                act(sq[:, 0:n], ps[:, 0:n], AF.Square, [bps], [bsq], joint=False)
                qg, bqg = pc.qg.next()
                ts(qg[:, 0:n], ps[:, 0:n], g[:, 0:1], None, ALU.mult, None, [bps, bg], [bqg], joint=False)
                qgs.append((qg, bqg))
                onesm = ones_bd if group == 64 else ones128
                if group == "all":
                    mm(sp[:, 0:n], onesm, sq[:, 0:n], ci == 0, ci == nch - 1, [bsq, cst], bsp)
                else:
                    assert nch == 1
                    mm(sp[:, 0:n], onesm, sq[:, 0:n], True, True, [bsq, cst], bsp)
            cnt = {128: 128, 64: 64, "all": 128 * nch}[group]
            rs0, brs0 = pc.rstd.next()
            act(rs0[:, 0:n], sp[:, 0:n], AF.Sqrt, [bsp, cst], [brs0], joint=False, scale=1.0 / cnt, bias=epsT[:, 0:1])
            rs, brs = pc.rstd.next()
            frecip(rs[:, 0:n], rs0[:, 0:n], [brs0], [brs])
            for ci, (qg, bqg) in enumerate(qgs):
                ob, bob = pc.ob.next()
                if rope is not None:
                    rp, brp = pc.rps.next()
                    mm(rp[:, 0:n], perm128 if rope == 128 else perm64, qg[:, 0:n], True, True, [bqg, cst], brp)
                    t1, bt1 = pc.t1.next()
                    t2, bt2 = pc.t2.next()
                    tt(t1[:, 0:n], qg[:, 0:n], pc.rope[:, 0, t0:t0 + n], ALU.mult, [bqg, pc.b_rope], [bt1], joint=False)
                    tt(t2[:, 0:n], rp[:, 0:n], pc.rope[:, 1, t0:t0 + n], ALU.mult, [brp, pc.b_rope], [bt2], joint=False)
                    tt(t1[:, 0:n], t1[:, 0:n], t2[:, 0:n], ALU.add, [bt1, bt2], [bt1], joint=False)
                    tt(ob[:, 0:n], t1[:, 0:n], rs[:, 0:n], ALU.mult, [bt1, brs], [bob], joint=False)
                else:
                    tt(ob[:, 0:n], qg[:, 0:n], rs[:, 0:n], ALU.mult, [bqg, brs], [bob], joint=False)
                d = dsts[ci]
                if d[0] == "dram":
                    dma(SP, d[1][:, t0:t0 + n], ob[:, 0:n], [bob], [], bob)
                else:
                    P.op(POOL, lambda e, d=d, ob=ob, t0=t0, n=n: e.tensor_copy(out=d[1][:, t0:t0 + n], in_=ob[:, 0:n]),
                         reads=[bob], writes=[d[2][bi]], joint=True)

    def proj_v(es, pc, src, src_bufs, kc_n, wv, pieces, tbs):
        F = sum(p[1] for p in pieces)
        wt = sb(es, "wv", [128, kc_n, F], BF16)
        bwt = Buf("wv")
        off = 0
        for (c0, ncol) in pieces:
            dma(POOL, wt[:, :, off:off + ncol], wv[:, :, c0:c0 + ncol], [], [bwt], bwt)
            off += ncol
        vps = pc.pps
        vob = Ring(es, nc, "vob", [128, 1024], BF16, 1)
        for bi, tb in enumerate(TBS):
            if tb not in tbs:
                continue
            for t in tiles_of(tb):
                vo, bvo = vob.next()
                for f0 in range(0, F, 512):
                    fn_ = min(512, F - f0)
                    ps, bps = vps.next()
                    for kc in range(kc_n):
                        mm(ps[:, 0:fn_], src[:, kc, t * 128:(t + 1) * 128], wt[:, kc, f0:f0 + fn_], kc == 0, kc == kc_n - 1, [src_bufs[bi], bwt], bps)
                    P.op(DVE, lambda e, vo=vo, ps=ps, f0=f0, fn_=fn_: e.tensor_copy(out=vo[:, f0:f0 + fn_], in_=ps[:, 0:fn_]),
                         reads=[bps], writes=[bvo], joint=True)
                dma(SP, Vs[t * 128:(t + 1) * 128, 0:F], vo[:, 0:F], [bvo], [], bvo)

    def attn_phase(es, AO, aob, units, scale, masks=None):
        kq = {}
        ldr = Ring(es, nc, "akq", [128, T], BF16, 8)
        vr = Ring(es, nc, "av", [128, NT, 128], BF16, 4)
        spr = Ring(es, nc, "asps", [128, 512], F32, 3, psum=True)
        nacc = len(set(s_["acc"] for u_ in units for s_ in u_["subs"]))
        ftm = [Ring(es, nc, f"aft{i}", [128, 512], F32, 2) for i in range(5 if nacc == 2 else 3)]
        opr = [Ring(es, nc, "aops", [128, 512], F32, 2 if nacc == 1 else 1, psum=True) for _ in range(nacc)]
        lpr = [Ring(es, nc, "alps", [128, 512], F32, 2 if nacc == 1 else 1, psum=True) for _ in range(nacc)]
        fpr = Ring(es, nc, "afps", [128, 512], F32, 1, psum=True)
        ptr = Ring(es, nc, "apt", [128, 512], BF16, 5)
        fsq = Ring(es, nc, "afsq", [128, 512], BF16, 2)
        if masks is not None:
            wm = sb(es, "wm", [128, 6, 512], BF16)
            b_wm = Buf("wm")
            dma(SP, wm[:], wmask_in[:, :, :], [], [b_wm], b_wm)
        cache = {}
        R = dict(ftm=ftm, fsq=fsq, fpr=fpr)
        items = []
        for u in units:
            for qi, (q0, qn, ktiles_fn) in enumerate(u["qblocks"]):
                work = []
                for s_ in u["subs"]:
                    for kt in ktiles_fn():
                        work.append((s_, kt))
                lastidx = {}
                firstidx = {}
                for i, (s_, kt) in enumerate(work):
                    lastidx[s_["acc"]] = i
                    firstidx.setdefault(s_["acc"], i)
                for i, (s_, kt) in enumerate(work):
                    a_ = s_["acc"]
                    items.append(dict(u=u, q0=q0, qn=qn, s=s_, kt=kt, first=(i == firstidx[a_]), last=(i == lastidx[a_]),
                                      end=(i == len(work) - 1), start_qb=(i == 0), start_unit=(i == 0 and qi == 0)))

        def do_loads(u):
            nonlocal cache
            tl = {}
            for ld in u["loads"]:
                key, ap = ld[0], ld[1]
                if key in cache:
                    tl[key] = cache[key]
                    continue
                tle, btl = ldr.next()
                if len(ld) > 2:
                    r0_, nr_ = ld[2], ld[3]
                    P.op(DVE, lambda e, tle=tle: e.memset(tle[:], 0.0), writes=[btl])
                    dma(SP, tle[r0_:r0_ + nr_, :], ap[r0_:r0_ + nr_, :], [], [btl], btl, joint=False)
                else:
                    dma(SP, tle[:], ap, [], [btl], btl, joint=False)
                cache = {k: v for k, v in cache.items() if v[0] is not tle}
                cache[key] = (tle, btl)
                tl[key] = (tle, btl)
            for (key, c0, ncol, pad) in u["vloads"]:
                if key in cache:
                    tl[key] = cache[key]
                    continue
                tle, btl = vr.next()
                if ncol < 128:
                    P.op(DVE, lambda e, tle=tle: e.memset(tle[:], 0.0), writes=[btl])
                dma(SP, tle[:, :, pad:pad + ncol], Vs[:, c0:c0 + ncol].rearrange("(t p) f -> p t f", p=128), [], [btl], btl, joint=False)
                cache = {k: v for k, v in cache.items() if v[0] is not tle}
                cache[key] = (tle, btl)
                tl[key] = (tle, btl)
            return tl

        def do_OL(it):
            s_ = it["s"]
            a_ = s_["acc"]
            qn = it["qn"]
            O, Lp = it["O"], it["Lp"]
            vt, bv = it["tl"][s_["v"]]
            pt_, bpt = it["pt"]
            ktile = it["kt"][0]
            mm(O[a_][0][:, 0:qn], vt[:, ktile, :], pt_[:, 0:qn], it["first"], it["last"], [bv, bpt], O[a_][1])
            mm(Lp[a_][0][:, 0:qn], s_["ones"], pt_[:, 0:qn], it["first"], it["last"], [cst, bpt], Lp[a_][1])
            if it["end"]:
                cont = it["u"]["fin"](it["u"], it["q0"], qn, O, Lp, R)
                if cont is not None:
                    deferred.append([12, cont])

        pend = []
        deferred = []
        cur_tl = None
        cur_O = cur_L = None
        for it in items:
            u = it["u"]
            if it["start_unit"]:
                cur_tl = do_loads(u)
            if it["start_qb"]:
                accs = sorted(set(s_["acc"] for s_ in u["subs"]))
                cur_O = {a_: opr[a_].next() for a_ in accs}
                cur_L = {a_: lpr[a_].next() for a_ in accs}
            it["tl"], it["O"], it["Lp"] = cur_tl, cur_O, cur_L
            s_ = it["s"]
            q0, qn = it["q0"], it["qn"]
            ktile = it["kt"][0]
            sp, bsp = spr.next()
            npairs = len(s_["pairs"])
            for pi, (kkey, qkey, r0, nr) in enumerate(s_["pairs"]):
                kt_, bk = cur_tl[kkey]
                qt_, bq = cur_tl[qkey]
                mm(sp[:, 0:qn], kt_[r0:r0 + nr, ktile * 128:(ktile + 1) * 128], qt_[r0:r0 + nr, q0:q0 + qn], pi == 0, pi == npairs - 1, [bk, bq], bsp)
            pt_, bpt = ptr.next()
            act(pt_[:, 0:qn], sp[:, 0:qn], AF.Exp, [bsp], [bpt], joint=False, scale=scale)
            if it["kt"][1] is not None:
                tt(pt_[:, 0:qn], pt_[:, 0:qn], wm[:, it["kt"][1], 0:qn], ALU.mult, [bpt, b_wm], [bpt], joint=False)
            it["pt"] = (pt_, bpt)
            pend.append(it)
            if len(pend) > 2:
                do_OL(pend.pop(0))
            for d_ in list(deferred):
                d_[0] -= 1
                if d_[0] <= 0:
                    deferred.remove(d_)
                    d_[1]()
        while pend:
            do_OL(pend.pop(0))
        for d_ in deferred:
            d_[1]()

    def fin_default(extra=None):
        def fin(u, q0, qn, O, Lp, R):
            r, br = R["ftm"][0].next()
            if extra is not None:
                es_t, bes = extra
                r2, br2 = R["ftm"][1].next()
                ts(r2[:, 0:qn], Lp[0][0][:, 0:qn], es_t[:, u["c"]:u["c"] + 1], None, ALU.add, None, [Lp[0][1], bes], [br2], joint=False)
                frecip(r[:, 0:qn], r2[:, 0:qn], [br2], [br])
            else:
                frecip(r[:, 0:qn], Lp[0][0][:, 0:qn], [Lp[0][1]], [br])
            tt(u["AO"][:, u["c"], q0:q0 + qn], O[0][0][:, 0:qn], r[:, 0:qn], ALU.mult, [O[0][1], br], [u["aob"]], joint=True)
        return fin

    def all_k():
        return [(k, None) for k in range(NT)]

    def ctx_k():
        return [(0, None), (1, None)]

    LAT_QB = [(256 + 512 * i, 512, all_k) for i in range(4)]
    CTX_QB = [(0, 256, ctx_k)]

    def std_layer(L):
        kind = L % 4
        need_ctx = L < 3
        moe = (L % 2 == 1)
        tbs_all = list(TBS)
        tbs_lat = TBS[1:]
        hT = None
        state = {}

        def phA(es):
            hT = sb(es, "hT", [128, 8, T], BF16)
            hb = [Buf(f"hT{i}") for i in range(5)]
            norm_phase(es, L, 0, hT, hb, tbs_all)
            if kind == 0:
                wv = gqa_wqkv[0].rearrange("(c p) n -> p c n", p=128)
                pc = proj_setup(es, 128)
                gq, bgq = gain_tile(es, [gqa_q_gain[0]])
                gk, bgk = gain_tile(es, [gqa_k_gain[0]])
                for h in range(8):
                    w, bw = load_w_chunk(pc.wring, wv, [(h * 128, 128)], 8)
                    proj_group(pc, hT, hb, 8, [(w, bw, gq, bgq)], tbs_all, 128, 128, [("dram", Qs[h])])
                for kvh in range(2):
                    w, bw = load_w_chunk(pc.wring, wv, [(1024 + kvh * 128, 128)], 8)
                    proj_group(pc, hT, hb, 8, [(w, bw, gk, bgk)], tbs_all, 128, 128, [("dram", Ks[kvh])])
                proj_v(es, pc, hT, hb, 8, wv, [(1280, 256)], tbs_all)
            elif kind == 2:
                wv = win_wqkv[0].rearrange("(c p) n -> p c n", p=128)
                pc = proj_setup(es, 64)
                gq, bgq = gain_tile(es, [win_q_gain[0], win_q_gain[0]])
                gk, bgk = gain_tile(es, [win_k_gain[0], win_k_gain[0]])
                for c in range(8):
                    w, bw = load_w_chunk(pc.wring, wv, [(c * 128, 128)], 8)
                    proj_group(pc, hT, hb, 8, [(w, bw, gq, bgq)], tbs_all, 64, 64, [("dram", Qs[c])])
                for kvh in range(2):
                    w, bw = load_w_chunk(pc.wring, wv, [(1024 + kvh * 64, 64), (1024 + kvh * 64, 64)], 8)
                    proj_group(pc, hT, hb, 8, [(w, bw, gk, bgk)], tbs_all, 64, 64, [("dram", Ks[kvh])])
                proj_v(es, pc, hT, hb, 8, wv, [(1152, 128)], tbs_all)
            elif kind == 3:
                wv = diff_wqkv[0].rearrange("(c p) n -> p c n", p=128)
                pc = proj_setup(es, 64)
                gq, bgq = gain_tile(es, [diff_q_gain[0], diff_q_gain[0]])
                gk, bgk = gain_tile(es, [diff_k_gain[0], diff_k_gain[0]])
                for h in range(8):
                    w, bw = load_w_chunk(pc.wring, wv, [(h * 128, 128)], 8)
                    proj_group(pc, hT, hb, 8, [(w, bw, gq, bgq)], tbs_lat, 64, 64, [("dram", Qs[h])])
                for h in range(8):
                    w, bw = load_w_chunk(pc.wring, wv, [(1024 + h * 128, 128)], 8)
                    proj_group(pc, hT, hb, 8, [(w, bw, gk, bgk)], tbs_all, 64, 64, [("dram", Ks[h])])
                proj_v(es, pc, hT, hb, 8, wv, [(2048, 1024)], tbs_all)
            else:
                wd = mla_wdown[0].rearrange("(c p) n -> p c n", p=128)
                pc = proj_setup(es, 64)
                cqn = sb(es, "cqn", [128, 3, T], BF16)
                ckvn = sb(es, "ckvn", [128, 2, T], BF16)
                bcq = [Buf(f"cqn{i}") for i in range(5)]
                bckv = [Buf(f"ckvn{i}") for i in range(5)]
                ch = []
                for c in range(3):
                    w, bw = load_w_chunk(pc.wring, wd, [(c * 128, 128)], 8)
                    g, bg = gain_tile(es, [mla_qa_gain[0, c * 128:(c + 1) * 128]])
                    ch.append((w, bw, g, bg))
                proj_group(pc, hT, hb, 8, ch, tbs_all, "all", None, [("sbuf", cqn[:, c, :], bcq) for c in range(3)])
                ch = []
                for c in range(2):
                    w, bw = load_w_chunk(pc.wring, wd, [(384 + c * 128, 128)], 8)
                    g, bg = gain_tile(es, [mla_kva_gain[0, c * 128:(c + 1) * 128]])
                    ch.append((w, bw, g, bg))
                proj_group(pc, hT, hb, 8, ch, tbs_all, "all", None, [("sbuf", ckvn[:, c, :], bckv) for c in range(2)])
                w, bw = load_w_chunk(pc.wring, wd, [(640, 64), (640, 64)], 8)
                g, bg = gain_tile(es, [mla_k_gain[0, 128:192], mla_k_gain[0, 128:192]])
                proj_group(pc, hT, hb, 8, [(w, bw, g, bg)], tbs_all, 64, 64, [("dram", Ks[8])])
                wq = mla_wuq[0].rearrange("(c p) n -> p c n", p=128)
                wkv = mla_wukv[0].rearrange("(c p) n -> p c n", p=128)
                gqn, bgqn = gain_tile(es, [mla_q_gain[0, 0:128]])
                gqp, bgqp = gain_tile(es, [mla_q_gain[0, 128:192], mla_q_gain[0, 128:192]])
                gkn, bgkn = gain_tile(es, [mla_k_gain[0, 0:128]])
                for h in range(8):
                    w, bw = load_w_chunk(pc.wring, wq, [(h * 192, 128)], 3)
                    proj_group(pc, cqn, bcq, 3, [(w, bw, gqn, bgqn)], tbs_all, 128, None, [("dram", Qs[h])])
                for c in range(4):
                    w, bw = load_w_chunk(pc.wring, wq, [(2 * c * 192 + 128, 64), ((2 * c + 1) * 192 + 128, 64)], 3)
                    proj_group(pc, cqn, bcq, 3, [(w, bw, gqp, bgqp)], tbs_all, 64, 64, [("dram", Qs[8 + c])])
                for h in range(8):
                    w, bw = load_w_chunk(pc.wring, wkv, [(h * 256, 128)], 2)
                    proj_group(pc, ckvn, bckv, 2, [(w, bw, gkn, bgkn)], tbs_all, 128, None, [("dram", Ks[h])])
                proj_v(es, pc, ckvn, bckv, 2, wkv, [(h * 256 + 128, 128) for h in range(8)], tbs_all)

            if dbg and L == nlayers - 1:
                bd = Buf("dbgh")
                dma(SP, dbg_hT[:, :, :], hT[:], hb, [], bd)
                bd2 = Buf("dbgm")
                dma(SP, dbg_modF[:, :, :, :], modF[:], [b_modF], [], bd2)

        run_phase(phA)

        def phBC(es0):
            AO = sb(es0, "AO", [128, 8, T], BF16)
            aob = Buf("AO")

            def phB(es):
                qbs = (CTX_QB if need_ctx else []) + LAT_QB
                units = []
                if kind == 0:
                    scale = 128 ** -0.5
                    for h in range(8):
                        kvh = h // 4
                        units.append(dict(c=h, loads=[(("q", h), Qs[h]), (("k", kvh), Ks[kvh])], vloads=[(("v", kvh), kvh * 128, 128, 0)],
                                          subs=[dict(pairs=[(("k", kvh), ("q", h), 0, 128)], v=("v", kvh), ones=ones128, acc=0)],
                                          qblocks=qbs, fin=fin_default(), AO=AO, aob=aob))
                    attn_phase(es, AO, aob, units, scale)
                elif kind == 1:
                    scale = 192 ** -0.5
                    for h in range(8):
                        r0 = (h % 2) * 64
                        units.append(dict(c=h, loads=[(("q", h), Qs[h]), (("qp", h), Qs[8 + h // 2], r0, 64), (("k", h), Ks[h]), (("kp", 0), Ks[8])],
                                          vloads=[(("v", h), h * 128, 128, 0)],
                                          subs=[dict(pairs=[(("k", h), ("q", h), 0, 128), (("kp", 0), ("qp", h), 0, 128)], v=("v", h), ones=ones128, acc=0)],
                                          qblocks=qbs, fin=fin_default(), AO=AO, aob=aob))
                    attn_phase(es, AO, aob, units, scale)
                elif kind == 2:
                    scale = 64 ** -0.5
                    esk = sb(es, "esk", [128, 8], F32)
                    besk = Buf("esk")
                    sv = win_sink[0].rearrange("(c two) -> two c", two=2)
                    dma(SP, esk[0:64, :], sv[0:1, :].partition_broadcast(64), [], [besk], besk, allow_slow_non_contiguous=True)
                    dma(SP, esk[64:128, :], sv[1:2, :].partition_broadcast(64), [], [besk], besk, allow_slow_non_contiguous=True)
                    act(esk[:], esk[:], AF.Exp, [besk], [besk], joint=False)

                    def mk_kfn(qb):
                        def kfn():
                            i0 = 4 * qb
                            res = [(0, None), (1, None)]
                            for j in range(max(0, i0 - 1), min(15, i0 + 4) + 1):
                                res.append((2 + j, j - i0 + 1))
                            return res
                        return kfn
                    wqbs = (CTX_QB if need_ctx else []) + [(256 + 512 * i, 512, mk_kfn(i)) for i in range(4)]
                    for c in range(8):
                        kvh = c // 4
                        subs = []
                        for half in range(2):
                            subs.append(dict(pairs=[(("k", kvh), ("q", c, half), 0, 128)], v=("v", kvh, half), ones=(ones_lo if half == 0 else ones_hi), acc=0))
                        units.append(dict(c=c, loads=[(("q", c, 0), Qs[c], 0, 64), (("q", c, 1), Qs[c], 64, 64), (("k", kvh), Ks[kvh])],
                                          vloads=[(("v", kvh, 0), kvh * 64, 64, 0), (("v", kvh, 1), kvh * 64, 64, 64)],
                                          subs=subs, qblocks=wqbs, fin=fin_default((esk, besk)), AO=AO, aob=aob))
                    attn_phase(es, AO, aob, units, scale, masks=True)
                else:
                    scale = 64 ** -0.5
                    lam_init = 0.8 - 0.6 * math.exp(-0.3 * L)
                    lp = sb(es, "lp", [128, 4, 64], F32)
                    blp = Buf("lp")
                    dma(SP, lp[:], diff_lambda[0:1].partition_broadcast(128), [], [blp], blp)
                    lpp = sb(es, "lpp", [128, 2, 64], F32)
                    lsum = sb(es, "lsum", [128, 2], F32)
                    nlam = sb(es, "nlam", [128, 1], F32)
                    bl2 = Buf("lpp")
                    bl3 = Buf("lsum")
                    bnl = Buf("nlam")
                    tt(lpp[:, 0, :], lp[:, 0, :], lp[:, 1, :], ALU.mult, [blp], [bl2])
                    tt(lpp[:, 1, :], lp[:, 2, :], lp[:, 3, :], ALU.mult, [blp], [bl2])
                    P.op(DVE, lambda e: e.reduce_sum(out=lsum[:], in_=lpp[:], axis=AX.X), reads=[bl2], writes=[bl3])
                    act(lsum[:], lsum[:], AF.Exp, [bl3], [bl3], joint=False)
                    tt(nlam[:], lsum[:, 1:2], lsum[:, 0:1], ALU.subtract, [bl3], [bnl], joint=False)
                    ts(nlam[:], nlam[:], -lam_init, None, ALU.add, None, [bnl], [bnl], joint=False)
                    sg, bsg = gain_tile(es, [diff_subln[0]])
                    ts(sg[:], sg[:], 1.0 - lam_init, None, ALU.mult, None, [bsg], [bsg], joint=False)

                    def fin_diff(u, q0, qn, O, Lp, R):
                        r0_, br0 = R["ftm"][0].next()
                        r1_, br1 = R["ftm"][1].next()
                        o_, bo = R["ftm"][2].next()
                        o0_, bo0 = R["ftm"][3].next()
                        o1_, bo1 = R["ftm"][4].next()
                        P.op(DVE, lambda e: e.tensor_copy(out=r0_[:, 0:qn], in_=Lp[0][0][:, 0:qn]), reads=[Lp[0][1]], writes=[br0])
                        P.op(DVE, lambda e: e.tensor_copy(out=r1_[:, 0:qn], in_=Lp[1][0][:, 0:qn]), reads=[Lp[1][1]], writes=[br1])
                        P.op(DVE, lambda e: e.tensor_copy(out=o0_[:, 0:qn], in_=O[0][0][:, 0:qn]), reads=[O[0][1]], writes=[bo0])
                        P.op(DVE, lambda e: e.tensor_copy(out=o1_[:, 0:qn], in_=O[1][0][:, 0:qn]), reads=[O[1][1]], writes=[bo1])
                        recip(o_[:, 0:qn], r0_[:, 0:qn], [br0], [bo], joint=False)
                        tt(o0_[:, 0:qn], o0_[:, 0:qn], o_[:, 0:qn], ALU.mult, [bo0, bo], [bo0], joint=False)
                        recip(o_[:, 0:qn], r1_[:, 0:qn], [br1], [bo], joint=False)
                        tt(o1_[:, 0:qn], o1_[:, 0:qn], o_[:, 0:qn], ALU.mult, [bo1, bo], [bo1], joint=False)
                        stt(o_[:, 0:qn], o1_[:, 0:qn], nlam[:, 0:1], o0_[:, 0:qn], ALU.mult, ALU.add, [bo0, bo1, bnl], [bo], joint=False)
                        sq, bsq = R["fsq"].next()
                        tt(sq[:, 0:qn], o_[:, 0:qn], o_[:, 0:qn], ALU.mult, [bo], [bsq], joint=False)

                        def stage_b():
                            fp, bfp = R["fpr"].next()
                            mm(fp[:, 0:qn], ones128, sq[:, 0:qn], True, True, [bsq, cst], bfp)
                            act(r0_[:, 0:qn], fp[:, 0:qn], AF.Ln, [bfp, cst], [br0], joint=False, scale=1.0 / 128, bias=epsT[:, 0:1])
                            act(r1_[:, 0:qn], r0_[:, 0:qn], AF.Exp, [br0], [br1], joint=False, scale=-0.5)
                            stt(u["AO"][:, u["c"], q0:q0 + qn], o_[:, 0:qn], sg[:, 0:1], r1_[:, 0:qn], ALU.mult, ALU.mult, [bo, br1, bsg], [u["aob"]], joint=True)
                        return stage_b

                    for h in range(8):
                        subs = [dict(pairs=[(("k", h), ("q", h, m), 0, 128)], v=("v", h), ones=ones128, acc=m) for m in range(2)]
                        units.append(dict(c=h, loads=[(("q", h, 0), Qs[h], 0, 64), (("q", h, 1), Qs[h], 64, 64), (("k", h), Ks[h])], vloads=[(("v", h), h * 128, 128, 0)],
                                          subs=subs, qblocks=qbs, fin=fin_diff, AO=AO, aob=aob))
                    attn_phase(es, AO, aob, units, scale)

            run_phase(phB)
            if dbg and L == nlayers - 1:
                def phBd(es):
                    bd = Buf("dbga")
                    dma(SP, dbg_AO[:, :, :], AO[:], [aob], [], bd)
                run_phase(phBd)

            def phC(es):
                wo_d = [gqa_wo, mla_wo, win_wo, diff_wo][kind][0].rearrange("(c p) n -> p c n", p=128)
                wo = sb(es, "wo", [128, 8, D], BF16)
                bwo = Buf("wo")
                for hh in range(2):
                    dma(POOL, wo[:, hh * 4:(hh + 1) * 4, :], wo_d[:, hh * 4:(hh + 1) * 4, :], [], [bwo], bwo)
                grow, bgrow = load_gate_rows(es, L, 2)
                opr = Ring(es, nc, "cps", [128, 512], F32, 4, psum=True)
                tmr = Ring(es, nc, "ctmp", [128, 512], F32, 3)
                tl = list(range(NT)) if need_ctx else list(range(2, NT))
                for t in tl:
                    r = 1 if t < 2 else 0
                    for dh in range(2):
                        ps, bps = opr.next()
                        for c in range(8):
                            mm(ps[:, :], AO[:, c, t * 128:(t + 1) * 128], wo[:, c, dh * 512:(dh + 1) * 512], c == 0, c == 7, [aob, bwo], bps)
                        tm, btm = tmr.next()
                        tt(tm[:], ps[:, :], grow[:, r, dh * 512:(dh + 1) * 512], ALU.mult, [bps, bgrow], [btm], joint=False)
                        xs = x_sb[:, t, dh * 512:(dh + 1) * 512]
                        tt(xs, tm[:], xs, ALU.add, [btm, xb[t]], [xb[t]], joint=False)
            run_phase(phC)
            if dbg and L == nlayers - 1:
                def phCd(es):
                    for t in range(NT):
                        dma(SP, dbg_xa[:, t, :], x_sb[:, t, :], [xb[t]], [], xb[t])
                run_phase(phCd)

        with ExitStack() as es0:
            phBC(es0)

        tbs_f = tbs_all if need_ctx else tbs_lat
        with ExitStack() as es0:
            hT = sb(es0, "hTf", [128, 8, T], BF16)
            hb = [Buf(f"hTf{i}") for i in range(5)]
            comb = sb(es0, "comb", [128, NT, 8], F32)
            b_comb = Buf("comb")

            def phD(es):
                if moe:
                    lg = sb(es, "lg", [128, NT, 8], F32)
                    blg = Buf("lg")
                    P.op(DVE, lambda e: e.memset(lg[:], 0.0), writes=[blg])
                    m = dict(router=moe_router[L // 2], lg=lg, blg=blg)
                    norm_phase(es, L, 1, hT, hb, tbs_f, moe=m)
                    route(es, m, comb, b_comb)
                else:
                    norm_phase(es, L, 1, hT, hb, tbs_f)
            run_phase(phD)

            def phE(es):
                if moe:
                    experts = [(moe_w13[L // 2, e], moe_w2[L // 2, e]) for e in range(8)]
                    ffn_phase(es, L, hT, hb, tbs_f, experts, comb, b_comb)
                else:
                    ffn_phase(es, L, hT, hb, tbs_f, [(ffn_w13[L // 2], ffn_w2[L // 2])], None, None)
            run_phase(phE)

    run_phase(phase0)
    for L in range(nlayers):
        std_layer(L)

    def phase_out(es):
        for t in range(16):
            dma(SP, out_d[t * 128:(t + 1) * 128, :], x_sb[:, 2 + t, :], [xb[2 + t]], [], xb[2 + t])
    run_phase(phase_out)
    top.close()
    return nc


_CACHE = {}


def kernel(**inputs):
    nl = int(inputs.pop("_nlayers", 4))
    dbg = bool(inputs.pop("_dbg", False))
    ncores = int(inputs.pop("_ncores", 8))
    trace = bool(inputs.pop("_trace", False))
    if (nl, dbg) not in _CACHE:
        _CACHE[(nl, dbg)] = build(nl, dbg)
    nc = _CACHE[(nl, dbg)]
    consts = _consts()
    shared = {}
    for k in ["ada_w", "ada_b", "norm_mix", "norm_ffn", "gqa_wqkv", "gqa_q_gain", "gqa_k_gain", "gqa_wo", "mla_wdown",
              "mla_qa_gain", "mla_kva_gain", "mla_wuq", "mla_wukv", "mla_q_gain", "mla_k_gain", "mla_wo", "win_wqkv",
              "win_q_gain", "win_k_gain", "win_sink", "win_wo", "diff_wqkv", "diff_q_gain", "diff_k_gain", "diff_lambda",
              "diff_subln", "diff_wo", "ffn_w13", "ffn_w2", "moe_router", "moe_w13", "moe_w2"]:
        shared[k] = np.ascontiguousarray(np.asarray(inputs[k], dtype=np.float32))
        if nl < 2 and k in ("moe_w13", "moe_w2"):
            shared[k] = np.zeros((1, 1, 8, 8), np.float32)
    shared.update(consts)
    x = np.asarray(inputs["x"], dtype=np.float32)
    c = np.asarray(inputs["c"], dtype=np.float32)
    ctx = np.asarray(inputs["ctx"], dtype=np.float32)
    c_ctx = np.asarray(inputs["c_ctx"], dtype=np.float32)
    in_maps = []
    for b in range(ncores):
        m = dict(shared)
        m["x"] = np.ascontiguousarray(x[b])
        m["ctx"] = np.ascontiguousarray(ctx[b])
        cfm = np.stack([c[b].reshape(8, 128).T, c_ctx.reshape(8, 128).T], axis=-1)
        m["cfm"] = np.ascontiguousarray(cfm.astype(np.float32))
        in_maps.append(m)
    if trace:
        res = run_bass_kernel_spmd(nc, in_maps, core_ids=list(range(ncores)), trace=True)
        print("EXEC_NS", res.exec_time_ns, flush=True)
        try:
            sc = res.per_core_scope_times or {}
            for k_, v_ in sc.items():
                print("SCOPE", k_, v_, flush=True)
        except Exception as e_:
            print("scope err", e_)
    else:
        res = run_bass_kernel_spmd(nc, in_maps, core_ids=list(range(ncores)))
    if dbg:
        return res.results
    return np.stack([r["out"] for r in res.results], axis=0).astype(np.float32)
```

```python
import math
from contextlib import ExitStack
import numpy as np
import ml_dtypes
import concourse.bass as bass
import concourse.mybir as mybir
from concourse.bass_utils import run_bass_kernel_spmd

F32 = mybir.dt.float32
BF16 = mybir.dt.bfloat16
AF = mybir.ActivationFunctionType
ALU = mybir.AluOpType
AX = mybir.AxisListType
PE, ACT, DVE, POOL, SP = "tensor", "scalar", "vector", "gpsimd", "sync"
ENGS = (PE, ACT, DVE, POOL, SP)

D = 1024
T = 2304
NT = 18
CTX = 256
EPS = 1e-6
TBS = [(0, 256)] + [(256 + 512 * i, 512) for i in range(4)]
N_DMA_SEMS = 72


class Buf:
    __slots__ = ("name", "writers", "readers", "dsem", "excl")

    def __init__(self, name, excl=False):
        self.name = name
        self.writers = []
        self.readers = []
        self.dsem = None
        self.excl = excl


class Op:
    __slots__ = ("eng", "fn", "deps", "is_dma", "sig", "sigval", "waits", "id", "dsem_idx")


class Prog:
    def __init__(self):
        self.ops = []
        self.n_dsem = 0
        self.free_dsems = []
        self.live = []
        self.phase_dma = []
        self.last_op = {}
        self.emitted = 0
        self.cnt = {e: 0 for e in ENGS}
        self.seen = {e: {} for e in ENGS}

    def _new(self, eng, fn, is_dma):
        o = Op()
        o.id = len(self.ops)
        o.eng = eng
        o.fn = fn
        o.is_dma = is_dma
        o.sig = False
        o.sigval = None
        o.waits = None
        o.dsem_idx = None
        o.deps = set()
        return o

    def op(self, eng, fn, reads=(), writes=(), joint=False, dma_buf=None):
        o = self._new(eng, fn, dma_buf is not None)
        deps = o.deps
        for b in reads:
            deps.update(b.writers)
            if b.excl:
                deps.update(b.readers)
        for b in writes:
            deps.update(b.readers)
            if not (joint and not b.readers):
                deps.update(b.writers)
        for b in reads:
            self._add(b.readers, o)
        for b in writes:
            if b.readers or not joint:
                b.writers = [o.id]
                b.readers = []
            else:
                self._add(b.writers, o)
        if o.is_dma:
            if dma_buf.dsem is None:
                if self.free_dsems:
                    dma_buf.dsem = self.free_dsems.pop()
                else:
                    dma_buf.dsem = [self.n_dsem, 0]
                    self.n_dsem += 1
                    assert self.n_dsem <= N_DMA_SEMS, "out of DMA semaphores"
                self.live.append(dma_buf)
            dma_buf.dsem[1] += 16
            o.sigval = dma_buf.dsem[1]
            o.dsem_idx = dma_buf.dsem[0]
            self.phase_dma.append(o.id)
        else:
            self.last_op[eng] = o.id
        self.ops.append(o)
        return o

    def _add(self, lst, o):
        if not o.is_dma:
            for i, pid in enumerate(lst):
                p = self.ops[pid]
                if (not p.is_dma) and p.eng == o.eng:
                    lst[i] = o.id
                    return
        lst.append(o.id)

    def barrier(self):
        last = dict(self.last_op)
        dmas = list(self.phase_dma)
        for e in ENGS:
            o = self._new(e, None, False)
            o.deps = set(dmas)
            for e2, oid in last.items():
                if e2 != e:
                    o.deps.add(oid)
            self.ops.append(o)
        self.phase_dma = []
        for b in self.live:
            self.free_dsems.append(b.dsem)
            b.dsem = None
        self.live = []

    def emit(self, block, sems):
        ops = self.ops
        s0 = self.emitted
        new = ops[s0:]
        for o in new:
            o.deps = {d for d in o.deps if d >= s0}
            for d in o.deps:
                p = ops[d]
                if p.is_dma:
                    continue
                if p.eng == PE and o.eng == PE and not o.is_dma:
                    continue
                p.sig = True
        for o in new:
            if o.is_dma:
                continue
            if o.sig:
                self.cnt[o.eng] += 1
                o.sigval = self.cnt[o.eng]
        for o in new:
            w = {}
            for d in o.deps:
                p = ops[d]
                if p.is_dma:
                    key = ("dma", p.dsem_idx)
                else:
                    if p.eng == PE and o.eng == PE and not o.is_dma:
                        continue
                    key = ("eng", p.eng)
                if w.get(key, 0) < p.sigval:
                    w[key] = p.sigval
            s = self.seen[o.eng]
            o.waits = []
            for key, v in w.items():
                if s.get(key, 0) >= v:
                    continue
                s[key] = v
                o.waits.append((key, v))
        per = {e: [] for e in ENGS}
        for o in new:
            per[o.eng].append(o)

        def run(name, eng):
            for o in per[name]:
                for key, v in o.waits:
                    sem = sems["dma"][key[1]] if key[0] == "dma" else sems[key[1]]
                    eng.wait_ge(sem, v)
                if o.fn is None:
                    continue
                ins = o.fn(eng)
                if ins is None:
                    continue
                if o.is_dma:
                    ins.then_inc(sems["dma"][o.dsem_idx], 16)
                elif o.sig:
                    ins.then_inc(sems[o.eng], 1)

        block.tensor(lambda e: run(PE, e))
        block.scalar(lambda e: run(ACT, e))
        block.vector(lambda e: run(DVE, e))
        block.gpsimd(lambda e: run(POOL, e))
        block.sync(lambda e: run(SP, e))
        self.emitted = len(ops)
        for o in new:
            o.fn = None


_uid = [0]


def uname(s):
    _uid[0] += 1
    return f"{s}_{_uid[0]}"


class Ring:
    def __init__(self, es, nc, name, shape, dtype, n, psum=False):
        self.items = []
        for i in range(n):
            mk = nc.psum_tensor if psum else nc.sbuf_tensor
            t = es.enter_context(mk(uname(name), list(shape), dtype))
            self.items.append((t, Buf(name + str(i), excl=psum)))
        self.i = 0

    def next(self):
        it = self.items[self.i % len(self.items)]
        self.i += 1
        return it


def _rope_tables(g):
    half = g // 2
    quarter = g // 4
    inv = (10000.0 ** (-np.arange(quarter, dtype=np.float32) / quarter)).astype(np.float32)
    s = np.arange(2048)
    row = (s // 64).astype(np.float32)
    col = (s % 64).astype(np.float32)
    ang = np.concatenate([row[:, None] * inv[None, :], col[:, None] * inv[None, :]], axis=1).astype(np.float32)
    cos = np.cos(ang).astype(np.float32)
    sin = np.sin(ang).astype(np.float32)
    cosF = np.ones((128, T), np.float32)
    sinF = np.zeros((128, T), np.float32)
    for p in range(128):
        i = p % g
        j = i % half
        cosF[p, CTX:] = cos[:, j]
        sinF[p, CTX:] = -sin[:, j] if i < half else sin[:, j]
    return cosF, sinF


def _consts():
    c = {}
    c["identF"] = np.eye(128, dtype=np.float32)
    ones_bd = np.zeros((128, 128), np.float32)
    ones_bd[:64, :64] = 1
    ones_bd[64:, 64:] = 1
    ones_lo = np.zeros((128, 128), np.float32)
    ones_lo[:, :64] = 1
    ones_hi = np.zeros((128, 128), np.float32)
    ones_hi[:, 64:] = 1
    perm128 = np.zeros((128, 128), np.float32)
    perm64 = np.zeros((128, 128), np.float32)
    for m in range(128):
        perm128[(m + 64) % 128, m] = 1
        i = m % 64
        perm64[(m - i) + (i + 32) % 64, m] = 1
    mats = np.stack([np.ones((128, 128), np.float32), ones_bd, ones_lo, ones_hi, perm128, perm64], axis=1)
    c["matsB"] = mats.astype(ml_dtypes.bfloat16)
    c128, s128 = _rope_tables(128)
    c64, s64 = _rope_tables(64)
    c["rope"] = np.stack([c128, s128, c64, s64], axis=0)
    mask = np.zeros((128, 6, 512), np.float32)
    kp = np.arange(128)[:, None]
    q = np.arange(512)[None, :]
    for rel in range(6):
        mask[:, rel, :] = (np.abs(q - (rel - 1) * 128 - kp) <= 128).astype(np.float32)
    c["wmask"] = mask.astype(ml_dtypes.bfloat16)
    return c


def build(nlayers=4, dbg=False):
    nc = bass.Bass("TRN2", target_bir_lowering=False)
    big = nlayers >= 2

    def din(name, shape, dt=F32):
        return nc.dram_tensor(name, list(shape), dt, kind="ExternalInput").ap()

    x_in = din("x", [2048, D])
    ctx_in = din("ctx", [CTX, D])
    cfm_in = din("cfm", [128, 8, 2])
    ada_w = din("ada_w", [4, D, 6 * D])
    ada_b = din("ada_b", [4, 6 * D])
    norm_mix = din("norm_mix", [4, D])
    norm_ffn = din("norm_ffn", [4, D])
    gqa_wqkv = din("gqa_wqkv", [1, D, 1536])
    gqa_q_gain = din("gqa_q_gain", [1, 128])
    gqa_k_gain = din("gqa_k_gain", [1, 128])
    gqa_wo = din("gqa_wo", [1, D, D])
    mla_wdown = din("mla_wdown", [1, D, 704])
    mla_qa_gain = din("mla_qa_gain", [1, 384])
    mla_kva_gain = din("mla_kva_gain", [1, 256])
    mla_wuq = din("mla_wuq", [1, 384, 1536])
    mla_wukv = din("mla_wukv", [1, 256, 2048])
    mla_q_gain = din("mla_q_gain", [1, 192])
    mla_k_gain = din("mla_k_gain", [1, 192])
    mla_wo = din("mla_wo", [1, D, D])
    win_wqkv = din("win_wqkv", [1, D, 1280])
    win_q_gain = din("win_q_gain", [1, 64])
    win_k_gain = din("win_k_gain", [1, 64])
    win_sink = din("win_sink", [1, 16])
    win_wo = din("win_wo", [1, D, D])
    diff_wqkv = din("diff_wqkv", [1, D, 3072])
    diff_q_gain = din("diff_q_gain", [1, 64])
    diff_k_gain = din("diff_k_gain", [1, 64])
    diff_lambda = din("diff_lambda", [1, 4, 64])
    diff_subln = din("diff_subln", [1, 128])
    diff_wo = din("diff_wo", [1, D, D])
    ffn_w13 = din("ffn_w13", [2, D, 7168])
    ffn_w2 = din("ffn_w2", [2, 3584, D])
    moe_router = din("moe_router", [2, D, 8])
    moe_w13 = din("moe_w13", [2, 8, D, 7168] if big else [1, 1, 8, 8])
    moe_w2 = din("moe_w2", [2, 8, 3584, D] if big else [1, 1, 8, 8])
    identF_in = din("identF", [128, 128])
    matsB_in = din("matsB", [128, 6, 128], BF16)
    rope_in = din("rope", [4, 128, T])
    wmask_in = din("wmask", [128, 6, 512], BF16)
    out_d = nc.dram_tensor("out", [2048, D], F32, kind="ExternalOutput").ap()
    skind = "ExternalOutput" if dbg else "Internal"
    modrows = nc.dram_tensor("modrows", [4, 2, 6 * D], F32, kind=skind).ap()
    Qs = nc.dram_tensor("Qs", [12, 128, T], BF16, kind=skind).ap()
    Ks = nc.dram_tensor("Ks", [9, 128, T], BF16, kind=skind).ap()
    Vs = nc.dram_tensor("Vs", [T, 1024], BF16, kind=skind).ap()
    if dbg:
        dbg_hT = nc.dram_tensor("dbg_hT", [128, 8, T], BF16, kind="ExternalOutput").ap()
        dbg_AO = nc.dram_tensor("dbg_AO", [128, 8, T], BF16, kind="ExternalOutput").ap()
        dbg_xa = nc.dram_tensor("dbg_xa", [128, NT, D], F32, kind="ExternalOutput").ap()
        dbg_modF = nc.dram_tensor("dbg_modF", [128, 4, 2, 48], F32, kind="ExternalOutput").ap()

    P = Prog()
    top = ExitStack()
    sems = {e: top.enter_context(nc.semaphore("s_" + e)) for e in ENGS}
    sems["dma"] = [top.enter_context(nc.semaphore(f"sd{i}")) for i in range(N_DMA_SEMS)]

    def sb(es, name, shape, dt):
        return es.enter_context(nc.sbuf_tensor(uname(name), list(shape), dt))

    x_sb = sb(top, "x_sb", [128, NT, D], F32)
    xb = [Buf(f"x{t}") for t in range(NT)]
    identF = sb(top, "identF", [128, 128], F32)
    matsB = sb(top, "matsB", [128, 6, 128], BF16)
    epsT = sb(top, "epsT", [128, 1], F32)
    modF = sb(top, "modF", [128, 4, 2, 48], F32)
    cst = Buf("consts")
    b_modF = Buf("modF")
    ones128 = matsB[:, 0, :]
    ones_bd = matsB[:, 1, :]
    ones_lo = matsB[:, 2, :]
    ones_hi = matsB[:, 3, :]
    perm128 = matsB[:, 4, :]
    perm64 = matsB[:, 5, :]

    def run_phase(fn, name=None):
        with ExitStack() as es:
            fn(es)
            P.barrier()
            with nc.named_scope(uname(name or getattr(fn, "__name__", "ph"))):
                with nc.Block() as block:
                    P.emit(block, sems)

    def dma(eng, out, in_, reads, writes, sbuf, joint=True, **kw):
        return P.op(eng, lambda e: e.dma_start(out=out, in_=in_, **kw), reads=reads, writes=writes, joint=joint, dma_buf=sbuf)

    def mm(out, lhsT, rhs, start, stop, reads, wbuf):
        return P.op(PE, lambda e: e.matmul(out, lhsT=lhsT, rhs=rhs, start=start, stop=stop), reads=reads, writes=[wbuf], joint=not start)

    def act(out, in_, func, reads, writes, joint=True, **kw):
        return P.op(ACT, lambda e: e.activation(out=out, in_=in_, func=func, **kw), reads=reads, writes=writes, joint=joint)

    def tt(out, in0, in1, op, reads, writes, joint=True, eng=DVE):
        return P.op(eng, lambda e: e.tensor_tensor(out=out, in0=in0, in1=in1, op=op), reads=reads, writes=writes, joint=joint)

    def ts(out, in0, s1, s2, op0, op1, reads, writes, joint=True, eng=DVE):
        if s2 is None:
            return P.op(eng, lambda e: e.tensor_scalar(out=out, in0=in0, scalar1=s1, scalar2=None, op0=op0), reads=reads, writes=writes, joint=joint)
        return P.op(eng, lambda e: e.tensor_scalar(out=out, in0=in0, scalar1=s1, scalar2=s2, op0=op0, op1=op1), reads=reads, writes=writes, joint=joint)

    def stt(out, in0, scalar, in1, op0, op1, reads, writes, joint=True, eng=DVE):
        return P.op(eng, lambda e: e.scalar_tensor_tensor(out=out, in0=in0, scalar=scalar, in1=in1, op0=op0, op1=op1), reads=reads, writes=writes, joint=joint)

    def recip(out, in_, reads, writes, joint=True):
        return P.op(DVE, lambda e: e.reciprocal(out=out, in_=in_), reads=reads, writes=writes, joint=joint)

    def frecip(out, in_, reads, writes, joint=False):
        return P.op(DVE, lambda e: e.reciprocal(out=out, in_=in_), reads=reads, writes=writes, joint=joint)

    def tiles_of(tb):
        t0, n = tb
        return list(range(t0 // 128, (t0 + n) // 128))

    def phase0(es):
        for t in range(2):
            dma(SP, x_sb[:, t, :], ctx_in[t * 128:(t + 1) * 128, :], [], [xb[t]], xb[t])
        for t in range(16):
            dma(SP, x_sb[:, 2 + t, :], x_in[t * 128:(t + 1) * 128, :], [], [xb[2 + t]], xb[2 + t])
        dma(SP, identF[:], identF_in[:, :], [], [cst], cst)
        dma(SP, matsB[:], matsB_in[:, :, :], [], [cst], cst)
        P.op(DVE, lambda e: e.memset(epsT[:], EPS), writes=[cst], joint=True)
        cfm = sb(es, "cfm", [128, 8, 2], F32)
        silu2 = sb(es, "silu2", [128, 8, 2], F32)
        b_c = Buf("cfm")
        b_s = Buf("silu2")
        dma(SP, cfm[:], cfm_in[:, :, :], [], [b_c], b_c)
        act(silu2[:], cfm[:], AF.Silu, [b_c], [b_s])
        ones2 = sb(es, "ones2", [1, 2], F32)
        b_o2 = Buf("ones2")
        P.op(DVE, lambda e: e.memset(ones2[:], 1.0), writes=[b_o2])
        wr = Ring(es, nc, "adaw", [128, 8, 512], F32, 2)
        br = Ring(es, nc, "adab", [1, 512], F32, 2)
        rows = sb(es, "rows", [2, 6 * D], F32)
        b_rows = Buf("rows")
        pr = Ring(es, nc, "p0ps", [128, 512], F32, 2, psum=True)
        pt = Ring(es, nc, "p0pt", [128, 512], F32, 1, psum=True)
        for L in range(nlayers):
            for cb in range(12):
                w, bw = wr.next()
                bt, bbt = br.next()
                dma(SP, w[:], ada_w[L].rearrange("(c p) n -> p c n", p=128)[:, :, cb * 512:(cb + 1) * 512], [], [bw], bw)
                dma(SP, bt[:], ada_b[L:L + 1, cb * 512:(cb + 1) * 512], [], [bbt], bbt)
                ps, bps = pr.next()
                for kc in range(8):
                    mm(ps[0:2, :], silu2[:, kc, :], w[:, kc, :], kc == 0, False, [b_s, bw], bps)
                mm(ps[0:2, :], ones2[:, :], bt[:, :], False, True, [b_o2, bbt], bps)
                P.op(DVE, lambda e, ps=ps, cb=cb: e.tensor_copy(out=rows[:, cb * 512:(cb + 1) * 512], in_=ps[0:2, :]),
                     reads=[bps], writes=[b_rows], joint=True)
            dma(SP, modrows[L], rows[:, :], [b_rows], [], b_rows)
            tp, btp = pt.next()
            for j in range(48):
                P.op(PE, lambda e, j=j, tp=tp: e.transpose(out=tp[:, 2 * j:2 * j + 2], in_=rows[0:2, j * 128:(j + 1) * 128], identity=identF[0:2, 0:2]),
                     reads=[b_rows, cst], writes=[btp], joint=(j > 0))
            P.op(DVE, lambda e, tp=tp, L=L: e.tensor_copy(out=modF[:, L, :, :].rearrange("p r j -> p j r"), in_=tp[:, 0:96].rearrange("p (j r) -> p j r", r=2)),
                 reads=[btp], writes=[b_modF], joint=True)

    def norm_phase(es, L, which, hT, hb, tbs, moe=None):
        gsrc = (norm_mix if which == 0 else norm_ffn)
        gF = sb(es, "gF", [128, 8], F32)
        AB = sb(es, "AB", [128, 2, 2, 8], F32)
        b_g = Buf("gF")
        b_AB = Buf("AB")
        dma(SP, gF[:], gsrc[L].rearrange("(c p) -> p c", p=128), [], [b_g], b_g, allow_slow_non_contiguous=True)
        ish, isc = (0, 1) if which == 0 else (3, 4)
        for r in range(2):
            stt(AB[:, r, 0, :], modF[:, L, r, isc * 8:(isc + 1) * 8], 1.0, gF[:], ALU.add, ALU.mult, [b_modF, b_g], [b_AB])
            P.op(DVE, lambda e, r=r: e.tensor_copy(out=AB[:, r, 1, :], in_=modF[:, L, r, ish * 8:(ish + 1) * 8]), reads=[b_modF], writes=[b_AB], joint=True)
        junk = sb(es, "junk", [128, D], BF16)
        b_junk = Buf("junk")
        ss = sb(es, "ss", [128, NT], F32)
        rstd = sb(es, "rstd", [128, NT], F32)
        b_ss = [Buf(f"ss{i}") for i in range(5)]
        b_rstd = [Buf(f"rstd{i}") for i in range(5)]
        P.op(DVE, lambda e: e.memset(ss[:], 0.0), writes=b_ss)
        xnr = Ring(es, nc, "xn", [128, 4, D], F32, 1)
        ptr = Ring(es, nc, "nps", [128, 512], F32, 2, psum=True)
        if moe is not None:
            h32r = Ring(es, nc, "h32", [128, 8, 512], F32, 1)
            R32 = sb(es, "R32", [128, 8, 8], F32)
            b_R = Buf("R32")
            dma(SP, R32[:], moe["router"].rearrange("(c p) e -> p c e", p=128), [], [b_R], b_R)
            lgr = Ring(es, nc, "lgps", [128, 8], F32, 2, psum=True)
        for bi, tb in enumerate(TBS):
            if tb not in tbs:
                continue
            t0, n = tb
            tl = tiles_of(tb)
            r = 1 if bi == 0 else 0
            for t in tl:
                act(junk[:], x_sb[:, t, :], AF.Square, [xb[t]], [b_junk, b_ss[bi]], joint=False, accum_out=ss[:, t:t + 1])
            act(rstd[:, tl[0]:tl[-1] + 1], ss[:, tl[0]:tl[-1] + 1], AF.Sqrt, [b_ss[bi], cst], [b_rstd[bi]], joint=False, scale=1.0 / D, bias=epsT[:, 0:1])
            recip(rstd[:, tl[0]:tl[-1] + 1], rstd[:, tl[0]:tl[-1] + 1], [b_rstd[bi]], [b_rstd[bi]], joint=False)
            xn, bxn = xnr.next()
            for j, t in enumerate(tl):
                ts(xn[:, j, :], x_sb[:, t, :], rstd[:, t:t + 1], None, ALU.mult, None, [xb[t], b_rstd[bi]], [bxn])
            if moe is not None:
                h32, bh32 = h32r.next()
            for c in range(8):
                ps, bps = ptr.next()
                for j, t in enumerate(tl):
                    P.op(PE, lambda e, ps=ps, j=j, c=c, xn=xn: e.transpose(out=ps[:, j * 128:(j + 1) * 128], in_=xn[:, j, c * 128:(c + 1) * 128], identity=identF[:]),
                         reads=[bxn, cst], writes=[bps], joint=(j > 0))
                act(hT[:, c, t0:t0 + n], ps[:, 0:n], AF.Identity, [bps, b_AB], [hb[bi]], scale=AB[:, r, 0, c:c + 1], bias=AB[:, r, 1, c:c + 1])
                if moe is not None:
                    ts(h32[:, c, 0:n], ps[:, 0:n], AB[:, r, 0, c:c + 1], AB[:, r, 1, c:c + 1], ALU.mult, ALU.add, [bps, b_AB], [bh32])
            if moe is not None:
                for j, t in enumerate(tl):
                    lp, blp = lgr.next()
                    for c in range(8):
                        mm(lp[:, :], h32[:, c, j * 128:(j + 1) * 128], R32[:, c, :], c == 0, c == 7, [bh32, b_R], blp)
                    P.op(DVE, lambda e, lp=lp, t=t: e.tensor_copy(out=moe["lg"][:, t, :], in_=lp[:, :]), reads=[blp], writes=[moe["blg"]], joint=True)

    def route(es, moe, comb, b_comb):
        lg = moe["lg"]
        blg = moe["blg"]
        m8 = sb(es, "m8", [128, NT, 8], F32)
        tmp = sb(es, "rtmp", [128, NT, 8], F32)
        msk = sb(es, "rmsk", [128, NT, 8], F32)
        den = sb(es, "rden", [128, NT], F32)
        b1, b2, b3, b4 = Buf("m8"), Buf("rtmp"), Buf("rmsk"), Buf("rden")
        for t in range(NT):
            P.op(DVE, lambda e, t=t: e.max(out=m8[:, t, :], in_=lg[:, t, :]), reads=[blg], writes=[b1], joint=True)
        tt(msk[:], lg[:], m8[:, :, 1:2].to_broadcast([128, NT, 8]), ALU.is_ge, [blg, b1], [b3])
        tt(tmp[:], lg[:], m8[:, :, 0:1].to_broadcast([128, NT, 8]), ALU.subtract, [blg, b1], [b2])
        act(tmp[:], tmp[:], AF.Exp, [b2], [b2], joint=False)
        tt(tmp[:], tmp[:], msk[:], ALU.mult, [b2, b3], [b2], joint=False)
        P.op(DVE, lambda e: e.reduce_sum(out=den[:], in_=tmp[:], axis=AX.X), reads=[b2], writes=[b4])
        recip(den[:], den[:], [b4], [b4], joint=False)
        tt(comb[:], tmp[:], den[:].unsqueeze(2).to_broadcast([128, NT, 8]), ALU.mult, [b2, b4], [b_comb], joint=False)

    def load_gate_rows(es, L, idx):
        g = sb(es, "grow", [128, 2, D], F32)
        bg = Buf("grow")
        for r in range(2):
            dma(SP, g[:, r, :], modrows[L, r:r + 1, idx * D:(idx + 1) * D].partition_broadcast(128), [], [bg], bg)
        return g, bg

    def ffn_phase(es, L, hT, hb, tbs, experts, comb, b_comb):
        grow, bgrow = load_gate_rows(es, L, 5)
        w13r = Ring(es, nc, "w13", [128, 8, 1024], BF16, 2)
        w2r = Ring(es, nc, "w2", [128, 4, D], BF16, 2)
        has_ctx = TBS[0] in tbs
        if has_ctx:
            w2cr = Ring(es, nc, "w2c", [128, 4, D], BF16, 2)
        y = sb(es, "y", [128, 4, T], BF16)
        yb = [Buf(f"y{i}") for i in range(5)]
        sgr = Ring(es, nc, "sg", [128, 512], BF16, 3)
        gpr = Ring(es, nc, "gps", [128, 512], F32, 2, psum=True)
        upr = Ring(es, nc, "ups", [128, 512], F32, 2, psum=True)
        opr = Ring(es, nc, "ops", [128, 512], F32, 3, psum=True)
        for ei, (w13, w2) in enumerate(experts):
            w13v = w13.rearrange("(c p) n -> p c n", p=128)
            w2v = w2.rearrange("(c p) n -> p c n", p=128)
            for fb in range(7):
                wa, bwa = w13r.next()
                wb_, bwb = w2r.next()
                dma(POOL, wa[:, :, 0:512], w13v[:, :, fb * 512:(fb + 1) * 512], [], [bwa], bwa)
                dma(POOL, wa[:, :, 512:1024], w13v[:, :, 3584 + fb * 512:3584 + (fb + 1) * 512], [], [bwa], bwa)
                dma(POOL, wb_[:], w2v[:, fb * 4:(fb + 1) * 4, :], [], [bwb], bwb)
                if has_ctx:
                    wc_, bwc = w2cr.next()
                    tt(wc_[:], wb_[:], grow[:, 1, :].unsqueeze(1).to_broadcast([128, 4, D]), ALU.mult, [bwb, bgrow], [bwc], joint=False, eng=POOL)
                tt(wb_[:], wb_[:], grow[:, 0, :].unsqueeze(1).to_broadcast([128, 4, D]), ALU.mult, [bwb, bgrow], [bwb], joint=False, eng=POOL)
                for bi, tb in enumerate(TBS):
                    if tb not in tbs:
                        continue
                    t0, n = tb
                    for fc in range(4):
                        gp, bgp = gpr.next()
                        up, bup = upr.next()
                        for kc in range(8):
                            mm(gp[:, 0:n], wa[:, kc, fc * 128:(fc + 1) * 128], hT[:, kc, t0:t0 + n], kc == 0, kc == 7, [bwa, hb[bi]], bgp)
                        for kc in range(8):
                            mm(up[:, 0:n], wa[:, kc, 512 + fc * 128:512 + (fc + 1) * 128], hT[:, kc, t0:t0 + n], kc == 0, kc == 7, [bwa, hb[bi]], bup)
                        sg, bsg = sgr.next()
                        act(sg[:, 0:n], gp[:, 0:n], AF.Silu, [bgp], [bsg], joint=False)
                        tt(y[:, fc, t0:t0 + n], sg[:, 0:n], up[:, 0:n], ALU.mult, [bsg, bup], [yb[bi]])
                for bi, tb in enumerate(TBS):
                    if tb not in tbs:
                        continue
                    r = 1 if bi == 0 else 0
                    for t in tiles_of(tb):
                        for dh in range(2):
                            op_, bop = opr.next()
                            wsel, bwsel = (wc_, bwc) if r == 1 else (wb_, bwb)
                            for fc in range(4):
                                mm(op_[:, :], y[:, fc, t * 128:(t + 1) * 128], wsel[:, fc, dh * 512:(dh + 1) * 512], fc == 0, fc == 3, [yb[bi], bwsel], bop)
                            xs = x_sb[:, t, dh * 512:(dh + 1) * 512]
                            if comb is None:
                                tt(xs, op_[:, :], xs, ALU.add, [bop, xb[t]], [xb[t]], joint=False)
                            else:
                                stt(xs, op_[:, :], comb[:, t, ei:ei + 1], xs, ALU.mult, ALU.add, [bop, xb[t], b_comb], [xb[t]], joint=False)

    def load_w_chunk(ring, wv, pieces, kc_n):
        w, bw = ring.next()
        off = 0
        for (c0, ncol) in pieces:
            dma(POOL, w[:, 0:kc_n, off:off + ncol], wv[:, :, c0:c0 + ncol], [], [bw], bw)
            off += ncol
        return w, bw

    def gain_tile(es, pieces):
        g = sb(es, "gain", [128, 1], F32)
        bg = Buf("gain")
        off = 0
        for ap in pieces:
            n = ap.shape[0]
            dma(SP, g[off:off + n, :], ap.rearrange("(p o) -> p o", o=1), [], [bg], bg)
            off += n
        return g, bg

    class ProjCtx:
        pass

    def proj_setup(es, need_rope):
        pc = ProjCtx()
        pc.wring = Ring(es, nc, "wch", [128, 8, 128], BF16, 3)
        pc.pps = Ring(es, nc, "pps", [128, 512], F32, 3, psum=True)
        pc.sps = Ring(es, nc, "sps", [128, 512], F32, 1, psum=True)
        pc.rps = Ring(es, nc, "rps", [128, 512], F32, 1, psum=True)
        pc.sq = Ring(es, nc, "sq", [128, 512], BF16, 3)
        pc.qg = Ring(es, nc, "qg", [128, 512], BF16, 3)
        pc.rstd = Ring(es, nc, "prstd", [128, 512], F32, 3)
        pc.t1 = Ring(es, nc, "pt1", [128, 512], F32, 1)
        pc.t2 = Ring(es, nc, "pt2", [128, 512], F32, 1)
        pc.ob = Ring(es, nc, "pob", [128, 512], BF16, 3)
        pc.rope = None
        if need_rope is not None:
            pc.rope = sb(es, "rope", [128, 2, T], F32)
            pc.b_rope = Buf("rope")
            i0 = 0 if need_rope == 128 else 2
            for k in range(2):
                dma(SP, pc.rope[:, k, :], rope_in[i0 + k], [], [pc.b_rope], pc.b_rope)
        return pc

    def proj_group(pc, src, src_bufs, kc_n, chunks, tbs, group, rope, dsts):
        nch = len(chunks)
        for bi, tb in enumerate(TBS):
            if tb not in tbs:
                continue
            t0, n = tb
            pss = []
            for (w, bw, g, bg) in chunks:
                ps, bps = pc.pps.next()
                for kc in range(kc_n):
                    mm(ps[:, 0:n], w[:, kc, :], src[:, kc, t0:t0 + n], kc == 0, kc == kc_n - 1, [bw, src_bufs[bi]], bps)
                pss.append((ps, bps))
            qgs = []
            sp, bsp = pc.sps.next()
            for ci, (ps, bps) in enumerate(pss):
                (w, bw, g, bg) = chunks[ci]
                sq, bsq = pc.sq.next()
                act(sq[:, 0:n], ps[:, 0:n], AF.Square, [bps], [bsq], joint=False)
                qg, bqg = pc.qg.next()
                ts(qg[:, 0:n], ps[:, 0:n], g[:, 0:1], None, ALU.mult, None, [bps, bg], [bqg], joint=False)
                qgs.append((qg, bqg))
                onesm = ones_bd if group == 64 else ones128
                if group == "all":
                    mm(sp[:, 0:n], onesm, sq[:, 0:n], ci == 0, ci == nch - 1, [bsq, cst], bsp)
                else:
                    assert nch == 1
                    mm(sp[:, 0:n], onesm, sq[:, 0:n], True, True, [bsq, cst], bsp)
            cnt = {128: 128, 64: 64, "all": 128 * nch}[group]
            rs0, brs0 = pc.rstd.next()
            act(rs0[:, 0:n], sp[:, 0:n], AF.Ln, [bsp, cst], [brs0], joint=False, scale=1.0 / cnt, bias=epsT[:, 0:1])
            rs, brs = pc.rstd.next()
            act(rs[:, 0:n], rs0[:, 0:n], AF.Exp, [brs0], [brs], joint=False, scale=-0.5)
            for ci, (qg, bqg) in enumerate(qgs):
                ob, bob = pc.ob.next()
                if rope is not None:
                    rp, brp = pc.rps.next()
                    mm(rp[:, 0:n], perm128 if rope == 128 else perm64, qg[:, 0:n], True, True, [bqg, cst], brp)
                    t1, bt1 = pc.t1.next()
                    t2, bt2 = pc.t2.next()
                    tt(t1[:, 0:n], qg[:, 0:n], pc.rope[:, 0, t0:t0 + n], ALU.mult, [bqg, pc.b_rope], [bt1], joint=False)
                    tt(t2[:, 0:n], rp[:, 0:n], pc.rope[:, 1, t0:t0 + n], ALU.mult, [brp, pc.b_rope], [bt2], joint=False)
                    tt(t1[:, 0:n], t1[:, 0:n], t2[:, 0:n], ALU.add, [bt1, bt2], [bt1], joint=False)
                    tt(ob[:, 0:n], t1[:, 0:n], rs[:, 0:n], ALU.mult, [bt1, brs], [bob], joint=False)
                else:
                    tt(ob[:, 0:n], qg[:, 0:n], rs[:, 0:n], ALU.mult, [bqg, brs], [bob], joint=False)
                d = dsts[ci]
                if d[0] == "dram":
                    dma(SP, d[1][:, t0:t0 + n], ob[:, 0:n], [bob], [], bob)
                else:
                    P.op(POOL, lambda e, d=d, ob=ob, t0=t0, n=n: e.tensor_copy(out=d[1][:, t0:t0 + n], in_=ob[:, 0:n]),
                         reads=[bob], writes=[d[2][bi]], joint=True)

    def proj_v(es, pc, src, src_bufs, kc_n, wv, pieces, tbs):
        F = sum(p[1] for p in pieces)
        wt = sb(es, "wv", [128, kc_n, F], BF16)
        bwt = Buf("wv")
        off = 0
        for (c0, ncol) in pieces:
            dma(POOL, wt[:, :, off:off + ncol], wv[:, :, c0:c0 + ncol], [], [bwt], bwt)
            off += ncol
        vps = pc.pps
        vob = Ring(es, nc, "vob", [128, 1024], BF16, 1)
        for bi, tb in enumerate(TBS):
            if tb not in tbs:
                continue
            for t in tiles_of(tb):
                vo, bvo = vob.next()
                for f0 in range(0, F, 512):
                    fn_ = min(512, F - f0)
                    ps, bps = vps.next()
                    for kc in range(kc_n):
                        mm(ps[:, 0:fn_], src[:, kc, t * 128:(t + 1) * 128], wt[:, kc, f0:f0 + fn_], kc == 0, kc == kc_n - 1, [src_bufs[bi], bwt], bps)
                    P.op(DVE, lambda e, vo=vo, ps=ps, f0=f0, fn_=fn_: e.tensor_copy(out=vo[:, f0:f0 + fn_], in_=ps[:, 0:fn_]),
                         reads=[bps], writes=[bvo], joint=True)
                dma(SP, Vs[t * 128:(t + 1) * 128, 0:F], vo[:, 0:F], [bvo], [], bvo)

    def attn_phase(es, AO, aob, units, scale, masks=None):
        kq = {}
        ldr = Ring(es, nc, "akq", [128, T], BF16, 8)
        vr = Ring(es, nc, "av", [128, NT, 128], BF16, 4)
        spr = Ring(es, nc, "asps", [128, 512], F32, 3, psum=True)
        nacc = len(set(s_["acc"] for u_ in units for s_ in u_["subs"]))
        ftm = [Ring(es, nc, f"aft{i}", [128, 512], F32, 2) for i in range(5 if nacc == 2 else 3)]
        opr = [Ring(es, nc, "aops", [128, 512], F32, 2 if nacc == 1 else 1, psum=True) for _ in range(nacc)]
        lpr = [Ring(es, nc, "alps", [128, 512], F32, 2 if nacc == 1 else 1, psum=True) for _ in range(nacc)]
        fpr = Ring(es, nc, "afps", [128, 512], F32, 1, psum=True)
        ptr = Ring(es, nc, "apt", [128, 512], BF16, 5)
        fsq = Ring(es, nc, "afsq", [128, 512], BF16, 2)
        if masks is not None:
            wm = sb(es, "wm", [128, 6, 512], BF16)
            b_wm = Buf("wm")
            dma(SP, wm[:], wmask_in[:, :, :], [], [b_wm], b_wm)
        cache = {}
        R = dict(ftm=ftm, fsq=fsq, fpr=fpr)
        items = []
        for u in units:
            for qi, (q0, qn, ktiles_fn) in enumerate(u["qblocks"]):
                work = []
                for s_ in u["subs"]:
                    for kt in ktiles_fn():
                        work.append((s_, kt))
                lastidx = {}
                firstidx = {}
                for i, (s_, kt) in enumerate(work):
                    lastidx[s_["acc"]] = i
                    firstidx.setdefault(s_["acc"], i)
                for i, (s_, kt) in enumerate(work):
                    a_ = s_["acc"]
                    items.append(dict(u=u, q0=q0, qn=qn, s=s_, kt=kt, first=(i == firstidx[a_]), last=(i == lastidx[a_]),
                                      end=(i == len(work) - 1), start_qb=(i == 0), start_unit=(i == 0 and qi == 0)))

        def do_loads(u):
            nonlocal cache
            tl = {}
            for ld in u["loads"]:
                key, ap = ld[0], ld[1]
                if key in cache:
                    tl[key] = cache[key]
                    continue
                tle, btl = ldr.next()
                if len(ld) > 2:
                    r0_, nr_ = ld[2], ld[3]
                    P.op(DVE, lambda e, tle=tle: e.memset(tle[:], 0.0), writes=[btl])
                    dma(SP, tle[r0_:r0_ + nr_, :], ap[r0_:r0_ + nr_, :], [], [btl], btl, joint=False)
                else:
                    dma(SP, tle[:], ap, [], [btl], btl, joint=False)
                cache = {k: v for k, v in cache.items() if v[0] is not tle}
                cache[key] = (tle, btl)
                tl[key] = (tle, btl)
            for (key, c0, ncol, pad) in u["vloads"]:
                if key in cache:
                    tl[key] = cache[key]
                    continue
                tle, btl = vr.next()
                if ncol < 128:
                    P.op(DVE, lambda e, tle=tle: e.memset(tle[:], 0.0), writes=[btl])
                dma(SP, tle[:, :, pad:pad + ncol], Vs[:, c0:c0 + ncol].rearrange("(t p) f -> p t f", p=128), [], [btl], btl, joint=False)
                cache = {k: v for k, v in cache.items() if v[0] is not tle}
                cache[key] = (tle, btl)
                tl[key] = (tle, btl)
            return tl

        def do_OL(it):
            s_ = it["s"]
            a_ = s_["acc"]
            qn = it["qn"]
            O, Lp = it["O"], it["Lp"]
            vt, bv = it["tl"][s_["v"]]
            pt_, bpt = it["pt"]
            ktile = it["kt"][0]
            mm(O[a_][0][:, 0:qn], vt[:, ktile, :], pt_[:, 0:qn], it["first"], it["last"], [bv, bpt], O[a_][1])
            mm(Lp[a_][0][:, 0:qn], s_["ones"], pt_[:, 0:qn], it["first"], it["last"], [cst, bpt], Lp[a_][1])
            if it["end"]:
                cont = it["u"]["fin"](it["u"], it["q0"], qn, O, Lp, R)
                if cont is not None:
                    deferred.append([12, cont])

        pend = []
        deferred = []
        cur_tl = None
        cur_O = cur_L = None
        for it in items:
            u = it["u"]
            if it["start_unit"]:
                cur_tl = do_loads(u)
            if it["start_qb"]:
                accs = sorted(set(s_["acc"] for s_ in u["subs"]))
                cur_O = {a_: opr[a_].next() for a_ in accs}
                cur_L = {a_: lpr[a_].next() for a_ in accs}
            it["tl"], it["O"], it["Lp"] = cur_tl, cur_O, cur_L
            s_ = it["s"]
            q0, qn = it["q0"], it["qn"]
            ktile = it["kt"][0]
            sp, bsp = spr.next()
            npairs = len(s_["pairs"])
            for pi, (kkey, qkey, r0, nr) in enumerate(s_["pairs"]):
                kt_, bk = cur_tl[kkey]
                qt_, bq = cur_tl[qkey]
                mm(sp[:, 0:qn], kt_[r0:r0 + nr, ktile * 128:(ktile + 1) * 128], qt_[r0:r0 + nr, q0:q0 + qn], pi == 0, pi == npairs - 1, [bk, bq], bsp)
            pt_, bpt = ptr.next()
            act(pt_[:, 0:qn], sp[:, 0:qn], AF.Exp, [bsp], [bpt], joint=False, scale=scale)
            if it["kt"][1] is not None:
                tt(pt_[:, 0:qn], pt_[:, 0:qn], wm[:, it["kt"][1], 0:qn], ALU.mult, [bpt, b_wm], [bpt], joint=False)
            it["pt"] = (pt_, bpt)
            pend.append(it)
            if len(pend) > 2:
                do_OL(pend.pop(0))
            for d_ in list(deferred):
                d_[0] -= 1
                if d_[0] <= 0:
                    deferred.remove(d_)
                    d_[1]()
        while pend:
            do_OL(pend.pop(0))
        for d_ in deferred:
            d_[1]()

    def fin_default(extra=None):
        def fin(u, q0, qn, O, Lp, R):
            r, br = R["ftm"][0].next()
            if extra is not None:
                es_t, bes = extra
                r2, br2 = R["ftm"][1].next()
                ts(r2[:, 0:qn], Lp[0][0][:, 0:qn], es_t[:, u["c"]:u["c"] + 1], None, ALU.add, None, [Lp[0][1], bes], [br2], joint=False)
                frecip(r[:, 0:qn], r2[:, 0:qn], [br2], [br])
            else:
                frecip(r[:, 0:qn], Lp[0][0][:, 0:qn], [Lp[0][1]], [br])
            tt(u["AO"][:, u["c"], q0:q0 + qn], O[0][0][:, 0:qn], r[:, 0:qn], ALU.mult, [O[0][1], br], [u["aob"]], joint=True)
        return fin

    def all_k():
        return [(k, None) for k in range(NT)]

    def ctx_k():
        return [(0, None), (1, None)]

    LAT_QB = [(256 + 512 * i, 512, all_k) for i in range(4)]
    CTX_QB = [(0, 256, ctx_k)]

    def std_layer(L):
        kind = L % 4
        need_ctx = L < 3
        moe = (L % 2 == 1)
        tbs_all = list(TBS)
        tbs_lat = TBS[1:]
        hT = None
        state = {}

        def phA(es):
            hT = sb(es, "hT", [128, 8, T], BF16)
            hb = [Buf(f"hT{i}") for i in range(5)]
            norm_phase(es, L, 0, hT, hb, tbs_all)
            if kind == 0:
                wv = gqa_wqkv[0].rearrange("(c p) n -> p c n", p=128)
                pc = proj_setup(es, 128)
                gq, bgq = gain_tile(es, [gqa_q_gain[0]])
                gk, bgk = gain_tile(es, [gqa_k_gain[0]])
                for h in range(8):
                    w, bw = load_w_chunk(pc.wring, wv, [(h * 128, 128)], 8)
                    proj_group(pc, hT, hb, 8, [(w, bw, gq, bgq)], tbs_all, 128, 128, [("dram", Qs[h])])
                for kvh in range(2):
                    w, bw = load_w_chunk(pc.wring, wv, [(1024 + kvh * 128, 128)], 8)
                    proj_group(pc, hT, hb, 8, [(w, bw, gk, bgk)], tbs_all, 128, 128, [("dram", Ks[kvh])])
                proj_v(es, pc, hT, hb, 8, wv, [(1280, 256)], tbs_all)
            elif kind == 2:
                wv = win_wqkv[0].rearrange("(c p) n -> p c n", p=128)
                pc = proj_setup(es, 64)
                gq, bgq = gain_tile(es, [win_q_gain[0], win_q_gain[0]])
                gk, bgk = gain_tile(es, [win_k_gain[0], win_k_gain[0]])
                for c in range(8):
                    w, bw = load_w_chunk(pc.wring, wv, [(c * 128, 128)], 8)
                    proj_group(pc, hT, hb, 8, [(w, bw, gq, bgq)], tbs_all, 64, 64, [("dram", Qs[c])])
                for kvh in range(2):
                    w, bw = load_w_chunk(pc.wring, wv, [(1024 + kvh * 64, 64), (1024 + kvh * 64, 64)], 8)
                    proj_group(pc, hT, hb, 8, [(w, bw, gk, bgk)], tbs_all, 64, 64, [("dram", Ks[kvh])])
                proj_v(es, pc, hT, hb, 8, wv, [(1152, 128)], tbs_all)
            elif kind == 3:
                wv = diff_wqkv[0].rearrange("(c p) n -> p c n", p=128)
                pc = proj_setup(es, 64)
                gq, bgq = gain_tile(es, [diff_q_gain[0], diff_q_gain[0]])
                gk, bgk = gain_tile(es, [diff_k_gain[0], diff_k_gain[0]])
                for h in range(8):
                    w, bw = load_w_chunk(pc.wring, wv, [(h * 128, 128)], 8)
                    proj_group(pc, hT, hb, 8, [(w, bw, gq, bgq)], tbs_lat, 64, 64, [("dram", Qs[h])])
                for h in range(8):
                    w, bw = load_w_chunk(pc.wring, wv, [(1024 + h * 128, 128)], 8)
                    proj_group(pc, hT, hb, 8, [(w, bw, gk, bgk)], tbs_all, 64, 64, [("dram", Ks[h])])
                proj_v(es, pc, hT, hb, 8, wv, [(2048, 1024)], tbs_all)
            else:
                wd = mla_wdown[0].rearrange("(c p) n -> p c n", p=128)
                pc = proj_setup(es, 64)
                cqn = sb(es, "cqn", [128, 3, T], BF16)
                ckvn = sb(es, "ckvn", [128, 2, T], BF16)
                bcq = [Buf(f"cqn{i}") for i in range(5)]
                bckv = [Buf(f"ckvn{i}") for i in range(5)]
                ch = []
                for c in range(3):
                    w, bw = load_w_chunk(pc.wring, wd, [(c * 128, 128)], 8)
                    g, bg = gain_tile(es, [mla_qa_gain[0, c * 128:(c + 1) * 128]])
                    ch.append((w, bw, g, bg))
                proj_group(pc, hT, hb, 8, ch, tbs_all, "all", None, [("sbuf", cqn[:, c, :], bcq) for c in range(3)])
                ch = []
                for c in range(2):
                    w, bw = load_w_chunk(pc.wring, wd, [(384 + c * 128, 128)], 8)
                    g, bg = gain_tile(es, [mla_kva_gain[0, c * 128:(c + 1) * 128]])
                    ch.append((w, bw, g, bg))
                proj_group(pc, hT, hb, 8, ch, tbs_all, "all", None, [("sbuf", ckvn[:, c, :], bckv) for c in range(2)])
                w, bw = load_w_chunk(pc.wring, wd, [(640, 64), (640, 64)], 8)
                g, bg = gain_tile(es, [mla_k_gain[0, 128:192], mla_k_gain[0, 128:192]])
                proj_group(pc, hT, hb, 8, [(w, bw, g, bg)], tbs_all, 64, 64, [("dram", Ks[8])])
                wq = mla_wuq[0].rearrange("(c p) n -> p c n", p=128)
                wkv = mla_wukv[0].rearrange("(c p) n -> p c n", p=128)
                gqn, bgqn = gain_tile(es, [mla_q_gain[0, 0:128]])
                gqp, bgqp = gain_tile(es, [mla_q_gain[0, 128:192], mla_q_gain[0, 128:192]])
                gkn, bgkn = gain_tile(es, [mla_k_gain[0, 0:128]])
                for h in range(8):
                    w, bw = load_w_chunk(pc.wring, wq, [(h * 192, 128)], 3)
                    proj_group(pc, cqn, bcq, 3, [(w, bw, gqn, bgqn)], tbs_all, 128, None, [("dram", Qs[h])])
                for c in range(4):
                    w, bw = load_w_chunk(pc.wring, wq, [(2 * c * 192 + 128, 64), ((2 * c + 1) * 192 + 128, 64)], 3)
                    proj_group(pc, cqn, bcq, 3, [(w, bw, gqp, bgqp)], tbs_all, 64, 64, [("dram", Qs[8 + c])])
                for h in range(8):
                    w, bw = load_w_chunk(pc.wring, wkv, [(h * 256, 128)], 2)
                    proj_group(pc, ckvn, bckv, 2, [(w, bw, gkn, bgkn)], tbs_all, 128, None, [("dram", Ks[h])])
                proj_v(es, pc, ckvn, bckv, 2, wkv, [(h * 256 + 128, 128) for h in range(8)], tbs_all)

            if dbg and L == nlayers - 1:
                bd = Buf("dbgh")
                dma(SP, dbg_hT[:, :, :], hT[:], hb, [], bd)
                bd2 = Buf("dbgm")
                dma(SP, dbg_modF[:, :, :, :], modF[:], [b_modF], [], bd2)

        run_phase(phA)

        def phBC(es0):
            AO = sb(es0, "AO", [128, 8, T], BF16)
            aob = Buf("AO")

            def phB(es):
                qbs = (CTX_QB if need_ctx else []) + LAT_QB
                units = []
                if kind == 0:
                    scale = 128 ** -0.5
                    for h in range(8):
                        kvh = h // 4
                        units.append(dict(c=h, loads=[(("q", h), Qs[h]), (("k", kvh), Ks[kvh])], vloads=[(("v", kvh), kvh * 128, 128, 0)],
                                          subs=[dict(pairs=[(("k", kvh), ("q", h), 0, 128)], v=("v", kvh), ones=ones128, acc=0)],
                                          qblocks=qbs, fin=fin_default(), AO=AO, aob=aob))
                    attn_phase(es, AO, aob, units, scale)
                elif kind == 1:
                    scale = 192 ** -0.5
                    for h in range(8):
                        r0 = (h % 2) * 64
                        units.append(dict(c=h, loads=[(("q", h), Qs[h]), (("qp", h), Qs[8 + h // 2], r0, 64), (("k", h), Ks[h]), (("kp", 0), Ks[8])],
                                          vloads=[(("v", h), h * 128, 128, 0)],
                                          subs=[dict(pairs=[(("k", h), ("q", h), 0, 128), (("kp", 0), ("qp", h), 0, 128)], v=("v", h), ones=ones128, acc=0)],
                                          qblocks=qbs, fin=fin_default(), AO=AO, aob=aob))
                    attn_phase(es, AO, aob, units, scale)
                elif kind == 2:
                    scale = 64 ** -0.5
                    esk = sb(es, "esk", [128, 8], F32)
                    besk = Buf("esk")
                    sv = win_sink[0].rearrange("(c two) -> two c", two=2)
                    dma(SP, esk[0:64, :], sv[0:1, :].partition_broadcast(64), [], [besk], besk, allow_slow_non_contiguous=True)
                    dma(SP, esk[64:128, :], sv[1:2, :].partition_broadcast(64), [], [besk], besk, allow_slow_non_contiguous=True)
                    act(esk[:], esk[:], AF.Exp, [besk], [besk], joint=False)

                    def mk_kfn(qb):
                        def kfn():
                            i0 = 4 * qb
                            res = [(0, None), (1, None)]
                            for j in range(max(0, i0 - 1), min(15, i0 + 4) + 1):
                                res.append((2 + j, j - i0 + 1))
                            return res
                        return kfn
                    wqbs = (CTX_QB if need_ctx else []) + [(256 + 512 * i, 512, mk_kfn(i)) for i in range(4)]
                    for c in range(8):
                        kvh = c // 4
                        subs = []
                        for half in range(2):
                            subs.append(dict(pairs=[(("k", kvh), ("q", c, half), 0, 128)], v=("v", kvh, half), ones=(ones_lo if half == 0 else ones_hi), acc=0))
                        units.append(dict(c=c, loads=[(("q", c, 0), Qs[c], 0, 64), (("q", c, 1), Qs[c], 64, 64), (("k", kvh), Ks[kvh])],
                                          vloads=[(("v", kvh, 0), kvh * 64, 64, 0), (("v", kvh, 1), kvh * 64, 64, 64)],
                                          subs=subs, qblocks=wqbs, fin=fin_default((esk, besk)), AO=AO, aob=aob))
                    attn_phase(es, AO, aob, units, scale, masks=True)
                else:
                    scale = 64 ** -0.5
                    lam_init = 0.8 - 0.6 * math.exp(-0.3 * L)
                    lp = sb(es, "lp", [128, 4, 64], F32)
                    blp = Buf("lp")
                    dma(SP, lp[:], diff_lambda[0:1].partition_broadcast(128), [], [blp], blp)
                    lpp = sb(es, "lpp", [128, 2, 64], F32)
                    lsum = sb(es, "lsum", [128, 2], F32)
                    nlam = sb(es, "nlam", [128, 1], F32)
                    bl2 = Buf("lpp")
                    bl3 = Buf("lsum")
                    bnl = Buf("nlam")
                    tt(lpp[:, 0, :], lp[:, 0, :], lp[:, 1, :], ALU.mult, [blp], [bl2])
                    tt(lpp[:, 1, :], lp[:, 2, :], lp[:, 3, :], ALU.mult, [blp], [bl2])
                    P.op(DVE, lambda e: e.reduce_sum(out=lsum[:], in_=lpp[:], axis=AX.X), reads=[bl2], writes=[bl3])
                    act(lsum[:], lsum[:], AF.Exp, [bl3], [bl3], joint=False)
                    tt(nlam[:], lsum[:, 1:2], lsum[:, 0:1], ALU.subtract, [bl3], [bnl], joint=False)
                    ts(nlam[:], nlam[:], -lam_init, None, ALU.add, None, [bnl], [bnl], joint=False)
                    sg, bsg = gain_tile(es, [diff_subln[0]])
                    ts(sg[:], sg[:], 1.0 - lam_init, None, ALU.mult, None, [bsg], [bsg], joint=False)

                    def fin_diff(u, q0, qn, O, Lp, R):
                        r0_, br0 = R["ftm"][0].next()
                        r1_, br1 = R["ftm"][1].next()
                        o_, bo = R["ftm"][2].next()
                        o0_, bo0 = R["ftm"][3].next()
                        o1_, bo1 = R["ftm"][4].next()
                        P.op(DVE, lambda e: e.tensor_copy(out=r0_[:, 0:qn], in_=Lp[0][0][:, 0:qn]), reads=[Lp[0][1]], writes=[br0])
                        P.op(DVE, lambda e: e.tensor_copy(out=r1_[:, 0:qn], in_=Lp[1][0][:, 0:qn]), reads=[Lp[1][1]], writes=[br1])
                        P.op(DVE, lambda e: e.tensor_copy(out=o0_[:, 0:qn], in_=O[0][0][:, 0:qn]), reads=[O[0][1]], writes=[bo0])
                        P.op(DVE, lambda e: e.tensor_copy(out=o1_[:, 0:qn], in_=O[1][0][:, 0:qn]), reads=[O[1][1]], writes=[bo1])
                        recip(o_[:, 0:qn], r0_[:, 0:qn], [br0], [bo], joint=False)
                        tt(o0_[:, 0:qn], o0_[:, 0:qn], o_[:, 0:qn], ALU.mult, [bo0, bo], [bo0], joint=False)
                        recip(o_[:, 0:qn], r1_[:, 0:qn], [br1], [bo], joint=False)
                        tt(o1_[:, 0:qn], o1_[:, 0:qn], o_[:, 0:qn], ALU.mult, [bo1, bo], [bo1], joint=False)
                        stt(o_[:, 0:qn], o1_[:, 0:qn], nlam[:, 0:1], o0_[:, 0:qn], ALU.mult, ALU.add, [bo0, bo1, bnl], [bo], joint=False)
                        sq, bsq = R["fsq"].next()
                        tt(sq[:, 0:qn], o_[:, 0:qn], o_[:, 0:qn], ALU.mult, [bo], [bsq], joint=False)

                        def stage_b():
                            fp, bfp = R["fpr"].next()
                            mm(fp[:, 0:qn], ones128, sq[:, 0:qn], True, True, [bsq, cst], bfp)
                            act(r0_[:, 0:qn], fp[:, 0:qn], AF.Ln, [bfp, cst], [br0], joint=False, scale=1.0 / 128, bias=epsT[:, 0:1])
                            act(r1_[:, 0:qn], r0_[:, 0:qn], AF.Exp, [br0], [br1], joint=False, scale=-0.5)
                            stt(u["AO"][:, u["c"], q0:q0 + qn], o_[:, 0:qn], sg[:, 0:1], r1_[:, 0:qn], ALU.mult, ALU.mult, [bo, br1, bsg], [u["aob"]], joint=True)
                        return stage_b

                    for h in range(8):
                        subs = [dict(pairs=[(("k", h), ("q", h, m), 0, 128)], v=("v", h), ones=ones128, acc=m) for m in range(2)]
                        units.append(dict(c=h, loads=[(("q", h, 0), Qs[h], 0, 64), (("q", h, 1), Qs[h], 64, 64), (("k", h), Ks[h])], vloads=[(("v", h), h * 128, 128, 0)],
                                          subs=subs, qblocks=qbs, fin=fin_diff, AO=AO, aob=aob))
                    attn_phase(es, AO, aob, units, scale)

            run_phase(phB)
            if dbg and L == nlayers - 1:
                def phBd(es):
                    bd = Buf("dbga")
                    dma(SP, dbg_AO[:, :, :], AO[:], [aob], [], bd)
                run_phase(phBd)

            def phC(es):
                wo_d = [gqa_wo, mla_wo, win_wo, diff_wo][kind][0].rearrange("(c p) n -> p c n", p=128)
                wo = sb(es, "wo", [128, 8, D], BF16)
                bwo = Buf("wo")
                for hh in range(2):
                    dma(POOL, wo[:, hh * 4:(hh + 1) * 4, :], wo_d[:, hh * 4:(hh + 1) * 4, :], [], [bwo], bwo)
                grow, bgrow = load_gate_rows(es, L, 2)
                opr = Ring(es, nc, "cps", [128, 512], F32, 4, psum=True)
                tmr = Ring(es, nc, "ctmp", [128, 512], F32, 3)
                tl = list(range(NT)) if need_ctx else list(range(2, NT))
                for t in tl:
                    r = 1 if t < 2 else 0
                    for dh in range(2):
                        ps, bps = opr.next()
                        for c in range(8):
                            mm(ps[:, :], AO[:, c, t * 128:(t + 1) * 128], wo[:, c, dh * 512:(dh + 1) * 512], c == 0, c == 7, [aob, bwo], bps)
                        tm, btm = tmr.next()
                        tt(tm[:], ps[:, :], grow[:, r, dh * 512:(dh + 1) * 512], ALU.mult, [bps, bgrow], [btm], joint=False)
                        xs = x_sb[:, t, dh * 512:(dh + 1) * 512]
                        tt(xs, tm[:], xs, ALU.add, [btm, xb[t]], [xb[t]], joint=False)
            run_phase(phC)
            if dbg and L == nlayers - 1:
                def phCd(es):
                    for t in range(NT):
                        dma(SP, dbg_xa[:, t, :], x_sb[:, t, :], [xb[t]], [], xb[t])
                run_phase(phCd)

        with ExitStack() as es0:
            phBC(es0)

        tbs_f = tbs_all if need_ctx else tbs_lat
        with ExitStack() as es0:
            hT = sb(es0, "hTf", [128, 8, T], BF16)
            hb = [Buf(f"hTf{i}") for i in range(5)]
            comb = sb(es0, "comb", [128, NT, 8], F32)
            b_comb = Buf("comb")

            def phD(es):
                if moe:
                    lg = sb(es, "lg", [128, NT, 8], F32)
                    blg = Buf("lg")
                    P.op(DVE, lambda e: e.memset(lg[:], 0.0), writes=[blg])
                    m = dict(router=moe_router[L // 2], lg=lg, blg=blg)
                    norm_phase(es, L, 1, hT, hb, tbs_f, moe=m)
                    route(es, m, comb, b_comb)
                else:
                    norm_phase(es, L, 1, hT, hb, tbs_f)
            run_phase(phD)

            def phE(es):
                if moe:
                    experts = [(moe_w13[L // 2, e], moe_w2[L // 2, e]) for e in range(8)]
                    ffn_phase(es, L, hT, hb, tbs_f, experts, comb, b_comb)
                else:
                    ffn_phase(es, L, hT, hb, tbs_f, [(ffn_w13[L // 2], ffn_w2[L // 2])], None, None)
            run_phase(phE)

    run_phase(phase0)
    for L in range(nlayers):
        std_layer(L)

    def phase_out(es):
        for t in range(16):
            dma(SP, out_d[t * 128:(t + 1) * 128, :], x_sb[:, 2 + t, :], [xb[2 + t]], [], xb[2 + t])
    run_phase(phase_out)
    top.close()
    return nc


_CACHE = {}


def kernel(**inputs):
    nl = int(inputs.pop("_nlayers", 4))
    dbg = bool(inputs.pop("_dbg", False))
    ncores = int(inputs.pop("_ncores", 8))
    trace = bool(inputs.pop("_trace", False))
    if (nl, dbg) not in _CACHE:
        _CACHE[(nl, dbg)] = build(nl, dbg)
    nc = _CACHE[(nl, dbg)]
    consts = _consts()
    shared = {}
    for k in ["ada_w", "ada_b", "norm_mix", "norm_ffn", "gqa_wqkv", "gqa_q_gain", "gqa_k_gain", "gqa_wo", "mla_wdown",
              "mla_qa_gain", "mla_kva_gain", "mla_wuq", "mla_wukv", "mla_q_gain", "mla_k_gain", "mla_wo", "win_wqkv",
              "win_q_gain", "win_k_gain", "win_sink", "win_wo", "diff_wqkv", "diff_q_gain", "diff_k_gain", "diff_lambda",
              "diff_subln", "diff_wo", "ffn_w13", "ffn_w2", "moe_router", "moe_w13", "moe_w2"]:
        shared[k] = np.ascontiguousarray(np.asarray(inputs[k], dtype=np.float32))
        if nl < 2 and k in ("moe_w13", "moe_w2"):
            shared[k] = np.zeros((1, 1, 8, 8), np.float32)
    shared.update(consts)
    x = np.asarray(inputs["x"], dtype=np.float32)
    c = np.asarray(inputs["c"], dtype=np.float32)
    ctx = np.asarray(inputs["ctx"], dtype=np.float32)
    c_ctx = np.asarray(inputs["c_ctx"], dtype=np.float32)
    in_maps = []
    for b in range(ncores):
        m = dict(shared)
        m["x"] = np.ascontiguousarray(x[b])
        m["ctx"] = np.ascontiguousarray(ctx[b])
        cfm = np.stack([c[b].reshape(8, 128).T, c_ctx.reshape(8, 128).T], axis=-1)
        m["cfm"] = np.ascontiguousarray(cfm.astype(np.float32))
        in_maps.append(m)
    if trace:
        res = run_bass_kernel_spmd(nc, in_maps, core_ids=list(range(ncores)), trace=True)
        print("EXEC_NS", res.exec_time_ns, flush=True)
        try:
            sc = res.per_core_scope_times or {}
            for k_, v_ in sc.items():
                print("SCOPE", k_, v_, flush=True)
        except Exception as e_:
            print("scope err", e_)
    else:
        res = run_bass_kernel_spmd(nc, in_maps, core_ids=list(range(ncores)))
    if dbg:
        return res.results
    return np.stack([r["out"] for r in res.results], axis=0).astype(np.float32)
```

```python
import math
from contextlib import ExitStack
import numpy as np
import ml_dtypes
import concourse.bass as bass
import concourse.mybir as mybir
from concourse.bass_utils import run_bass_kernel_spmd

F32 = mybir.dt.float32
BF16 = mybir.dt.bfloat16
AF = mybir.ActivationFunctionType
ALU = mybir.AluOpType
AX = mybir.AxisListType
PE, ACT, DVE, POOL, SP = "tensor", "scalar", "vector", "gpsimd", "sync"
ENGS = (PE, ACT, DVE, POOL, SP)

D = 1024
T = 2304
NT = 18
CTX = 256
EPS = 1e-6
TBS = [(0, 256)] + [(256 + 512 * i, 512) for i in range(4)]
N_DMA_SEMS = 72


class Buf:
    __slots__ = ("name", "writers", "readers", "dsem", "excl")

    def __init__(self, name, excl=False):
        self.name = name
        self.writers = []
        self.readers = []
        self.dsem = None
        self.excl = excl


class Op:
    __slots__ = ("eng", "fn", "deps", "is_dma", "sig", "sigval", "waits", "id", "dsem_idx")


class Prog:
    def __init__(self):
        self.ops = []
        self.n_dsem = 0
        self.free_dsems = []
        self.live = []
        self.phase_dma = []
        self.last_op = {}
        self.emitted = 0
        self.cnt = {e: 0 for e in ENGS}
        self.seen = {e: {} for e in ENGS}

    def _new(self, eng, fn, is_dma):
        o = Op()
        o.id = len(self.ops)
        o.eng = eng
        o.fn = fn
        o.is_dma = is_dma
        o.sig = False
        o.sigval = None
        o.waits = None
        o.dsem_idx = None
        o.deps = set()
        return o

    def op(self, eng, fn, reads=(), writes=(), joint=False, dma_buf=None):
        o = self._new(eng, fn, dma_buf is not None)
        deps = o.deps
        for b in reads:
            deps.update(b.writers)
            if b.excl:
                deps.update(b.readers)
        for b in writes:
            deps.update(b.readers)
            if not (joint and not b.readers):
                deps.update(b.writers)
        for b in reads:
            self._add(b.readers, o)
        for b in writes:
            if b.readers or not joint:
                b.writers = [o.id]
                b.readers = []
            else:
                self._add(b.writers, o)
        if o.is_dma:
            if dma_buf.dsem is None:
                if self.free_dsems:
                    dma_buf.dsem = self.free_dsems.pop()
                else:
                    dma_buf.dsem = [self.n_dsem, 0]
                    self.n_dsem += 1
                    assert self.n_dsem <= N_DMA_SEMS, "out of DMA semaphores"
                self.live.append(dma_buf)
            dma_buf.dsem[1] += 16
            o.sigval = dma_buf.dsem[1]
            o.dsem_idx = dma_buf.dsem[0]
            self.phase_dma.append(o.id)
        else:
            self.last_op[eng] = o.id
        self.ops.append(o)
        return o

    def _add(self, lst, o):
        if not o.is_dma:
            for i, pid in enumerate(lst):
                p = self.ops[pid]
                if (not p.is_dma) and p.eng == o.eng:
                    lst[i] = o.id
                    return
        lst.append(o.id)

    def barrier(self):
        last = dict(self.last_op)
        dmas = list(self.phase_dma)
        for e in ENGS:
            o = self._new(e, None, False)
            o.deps = set(dmas)
            for e2, oid in last.items():
                if e2 != e:
                    o.deps.add(oid)
            self.ops.append(o)
        self.phase_dma = []
        for b in self.live:
            self.free_dsems.append(b.dsem)
            b.dsem = None
        self.live = []

    def emit(self, block, sems):
        ops = self.ops
        s0 = self.emitted
        new = ops[s0:]
        for o in new:
            o.deps = {d for d in o.deps if d >= s0}
            for d in o.deps:
                p = ops[d]
                if p.is_dma:
                    continue
                if p.eng == PE and o.eng == PE and not o.is_dma:
                    continue
                p.sig = True
        for o in new:
            if o.is_dma:
                continue
            if o.sig:
                self.cnt[o.eng] += 1
                o.sigval = self.cnt[o.eng]
        for o in new:
            w = {}
            for d in o.deps:
                p = ops[d]
                if p.is_dma:
                    key = ("dma", p.dsem_idx)
                else:
                    if p.eng == PE and o.eng == PE and not o.is_dma:
                        continue
                    key = ("eng", p.eng)
                if w.get(key, 0) < p.sigval:
                    w[key] = p.sigval
            s = self.seen[o.eng]
            o.waits = []
            for key, v in w.items():
                if s.get(key, 0) >= v:
                    continue
                s[key] = v
                o.waits.append((key, v))
        per = {e: [] for e in ENGS}
        for o in new:
            per[o.eng].append(o)

        def run(name, eng):
            for o in per[name]:
                for key, v in o.waits:
                    sem = sems["dma"][key[1]] if key[0] == "dma" else sems[key[1]]
                    eng.wait_ge(sem, v)
                if o.fn is None:
                    continue
                ins = o.fn(eng)
                if ins is None:
                    continue
                if o.is_dma:
                    ins.then_inc(sems["dma"][o.dsem_idx], 16)
                elif o.sig:
                    ins.then_inc(sems[o.eng], 1)

        block.tensor(lambda e: run(PE, e))
        block.scalar(lambda e: run(ACT, e))
        block.vector(lambda e: run(DVE, e))
        block.gpsimd(lambda e: run(POOL, e))
        block.sync(lambda e: run(SP, e))
        self.emitted = len(ops)
        for o in new:
            o.fn = None


_uid = [0]


def uname(s):
    _uid[0] += 1
    return f"{s}_{_uid[0]}"


class Ring:
    def __init__(self, es, nc, name, shape, dtype, n, psum=False):
        self.items = []
        for i in range(n):
            mk = nc.psum_tensor if psum else nc.sbuf_tensor
            t = es.enter_context(mk(uname(name), list(shape), dtype))
            self.items.append((t, Buf(name + str(i), excl=psum)))
        self.i = 0

    def next(self):
        it = self.items[self.i % len(self.items)]
        self.i += 1
        return it


def _rope_tables(g):
    half = g // 2
    quarter = g // 4
    inv = (10000.0 ** (-np.arange(quarter, dtype=np.float32) / quarter)).astype(np.float32)
    s = np.arange(2048)
    row = (s // 64).astype(np.float32)
    col = (s % 64).astype(np.float32)
    ang = np.concatenate([row[:, None] * inv[None, :], col[:, None] * inv[None, :]], axis=1).astype(np.float32)
    cos = np.cos(ang).astype(np.float32)
    sin = np.sin(ang).astype(np.float32)
    cosF = np.ones((128, T), np.float32)
    sinF = np.zeros((128, T), np.float32)
    for p in range(128):
        i = p % g
        j = i % half
        cosF[p, CTX:] = cos[:, j]
        sinF[p, CTX:] = -sin[:, j] if i < half else sin[:, j]
    return cosF, sinF


def _consts():
    c = {}
    c["identF"] = np.eye(128, dtype=np.float32)
    ones_bd = np.zeros((128, 128), np.float32)
    ones_bd[:64, :64] = 1
    ones_bd[64:, 64:] = 1
    ones_lo = np.zeros((128, 128), np.float32)
    ones_lo[:, :64] = 1
    ones_hi = np.zeros((128, 128), np.float32)
    ones_hi[:, 64:] = 1
    perm128 = np.zeros((128, 128), np.float32)
    perm64 = np.zeros((128, 128), np.float32)
    for m in range(128):
        perm128[(m + 64) % 128, m] = 1
        i = m % 64
        perm64[(m - i) + (i + 32) % 64, m] = 1
    mats = np.stack([np.ones((128, 128), np.float32), ones_bd, ones_lo, ones_hi, perm128, perm64], axis=1)
    c["matsB"] = mats.astype(ml_dtypes.bfloat16)
    c128, s128 = _rope_tables(128)
    c64, s64 = _rope_tables(64)
    c["rope"] = np.stack([c128, s128, c64, s64], axis=0)
    mask = np.zeros((128, 6, 512), np.float32)
    kp = np.arange(128)[:, None]
    q = np.arange(512)[None, :]
    for rel in range(6):
        mask[:, rel, :] = (np.abs(q - (rel - 1) * 128 - kp) <= 128).astype(np.float32)
    c["wmask"] = mask.astype(ml_dtypes.bfloat16)
    return c


def build(nlayers=4, dbg=False):
    nc = bass.Bass("TRN2", target_bir_lowering=False)
    big = nlayers >= 2

    def din(name, shape, dt=F32):
        return nc.dram_tensor(name, list(shape), dt, kind="ExternalInput").ap()

    x_in = din("x", [2048, D])
    ctx_in = din("ctx", [CTX, D])
    cfm_in = din("cfm", [128, 8, 2])
    ada_w = din("ada_w", [4, D, 6 * D])
    ada_b = din("ada_b", [4, 6 * D])
    norm_mix = din("norm_mix", [4, D])
    norm_ffn = din("norm_ffn", [4, D])
    gqa_wqkv = din("gqa_wqkv", [1, D, 1536])
    gqa_q_gain = din("gqa_q_gain", [1, 128])
    gqa_k_gain = din("gqa_k_gain", [1, 128])
    gqa_wo = din("gqa_wo", [1, D, D])
    mla_wdown = din("mla_wdown", [1, D, 704])
    mla_qa_gain = din("mla_qa_gain", [1, 384])
    mla_kva_gain = din("mla_kva_gain", [1, 256])
    mla_wuq = din("mla_wuq", [1, 384, 1536])
    mla_wukv = din("mla_wukv", [1, 256, 2048])
    mla_q_gain = din("mla_q_gain", [1, 192])
    mla_k_gain = din("mla_k_gain", [1, 192])
    mla_wo = din("mla_wo", [1, D, D])
    win_wqkv = din("win_wqkv", [1, D, 1280])
    win_q_gain = din("win_q_gain", [1, 64])
    win_k_gain = din("win_k_gain", [1, 64])
    win_sink = din("win_sink", [1, 16])
    win_wo = din("win_wo", [1, D, D])
    diff_wqkv = din("diff_wqkv", [1, D, 3072])
    diff_q_gain = din("diff_q_gain", [1, 64])
    diff_k_gain = din("diff_k_gain", [1, 64])
    diff_lambda = din("diff_lambda", [1, 4, 64])
    diff_subln = din("diff_subln", [1, 128])
    diff_wo = din("diff_wo", [1, D, D])
    ffn_w13 = din("ffn_w13", [2, D, 7168])
    ffn_w2 = din("ffn_w2", [2, 3584, D])
    moe_router = din("moe_router", [2, D, 8])
    moe_w13 = din("moe_w13", [2, 8, D, 7168] if big else [1, 1, 8, 8])
    moe_w2 = din("moe_w2", [2, 8, 3584, D] if big else [1, 1, 8, 8])
    identF_in = din("identF", [128, 128])
    matsB_in = din("matsB", [128, 6, 128], BF16)
    rope_in = din("rope", [4, 128, T])
    wmask_in = din("wmask", [128, 6, 512], BF16)
    out_d = nc.dram_tensor("out", [2048, D], F32, kind="ExternalOutput").ap()
    skind = "ExternalOutput" if dbg else "Internal"
    modrows = nc.dram_tensor("modrows", [4, 2, 6 * D], F32, kind=skind).ap()
    Qs = nc.dram_tensor("Qs", [12, 128, T], BF16, kind=skind).ap()
    Ks = nc.dram_tensor("Ks", [9, 128, T], BF16, kind=skind).ap()
    Vs = nc.dram_tensor("Vs", [T, 1024], BF16, kind=skind).ap()
    if dbg:
        dbg_hT = nc.dram_tensor("dbg_hT", [128, 8, T], BF16, kind="ExternalOutput").ap()
        dbg_AO = nc.dram_tensor("dbg_AO", [128, 8, T], BF16, kind="ExternalOutput").ap()
        dbg_xa = nc.dram_tensor("dbg_xa", [128, NT, D], F32, kind="ExternalOutput").ap()
        dbg_modF = nc.dram_tensor("dbg_modF", [128, 4, 2, 48], F32, kind="ExternalOutput").ap()

    P = Prog()
    top = ExitStack()
    sems = {e: top.enter_context(nc.semaphore("s_" + e)) for e in ENGS}
    sems["dma"] = [top.enter_context(nc.semaphore(f"sd{i}")) for i in range(N_DMA_SEMS)]

    def sb(es, name, shape, dt):
        return es.enter_context(nc.sbuf_tensor(uname(name), list(shape), dt))

    x_sb = sb(top, "x_sb", [128, NT, D], F32)
    xb = [Buf(f"x{t}") for t in range(NT)]
    identF = sb(top, "identF", [128, 128], F32)
    matsB = sb(top, "matsB", [128, 6, 128], BF16)
    epsT = sb(top, "epsT", [128, 1], F32)
    modF = sb(top, "modF", [128, 4, 2, 48], F32)
    cst = Buf("consts")
    b_modF = Buf("modF")
    ones128 = matsB[:, 0, :]
    ones_bd = matsB[:, 1, :]
    ones_lo = matsB[:, 2, :]
    ones_hi = matsB[:, 3, :]
    perm128 = matsB[:, 4, :]
    perm64 = matsB[:, 5, :]

    def run_phase(fn, name=None):
        with ExitStack() as es:
            fn(es)
            P.barrier()
            with nc.named_scope(uname(name or getattr(fn, "__name__", "ph"))):
                with nc.Block() as block:
                    P.emit(block, sems)

    def dma(eng, out, in_, reads, writes, sbuf, joint=True, **kw):
        return P.op(eng, lambda e: e.dma_start(out=out, in_=in_, **kw), reads=reads, writes=writes, joint=joint, dma_buf=sbuf)

    def mm(out, lhsT, rhs, start, stop, reads, wbuf):
        return P.op(PE, lambda e: e.matmul(out, lhsT=lhsT, rhs=rhs, start=start, stop=stop), reads=reads, writes=[wbuf], joint=not start)

    def act(out, in_, func, reads, writes, joint=True, **kw):
        return P.op(ACT, lambda e: e.activation(out=out, in_=in_, func=func, **kw), reads=reads, writes=writes, joint=joint)

    def tt(out, in0, in1, op, reads, writes, joint=True, eng=DVE):
        return P.op(eng, lambda e: e.tensor_tensor(out=out, in0=in0, in1=in1, op=op), reads=reads, writes=writes, joint=joint)

    def ts(out, in0, s1, s2, op0, op1, reads, writes, joint=True, eng=DVE):
        if s2 is None:
            return P.op(eng, lambda e: e.tensor_scalar(out=out, in0=in0, scalar1=s1, scalar2=None, op0=op0), reads=reads, writes=writes, joint=joint)
        return P.op(eng, lambda e: e.tensor_scalar(out=out, in0=in0, scalar1=s1, scalar2=s2, op0=op0, op1=op1), reads=reads, writes=writes, joint=joint)

    def stt(out, in0, scalar, in1, op0, op1, reads, writes, joint=True, eng=DVE):
        return P.op(eng, lambda e: e.scalar_tensor_tensor(out=out, in0=in0, scalar=scalar, in1=in1, op0=op0, op1=op1), reads=reads, writes=writes, joint=joint)

    def recip(out, in_, reads, writes, joint=True):
        return P.op(DVE, lambda e: e.reciprocal(out=out, in_=in_), reads=reads, writes=writes, joint=joint)

    def frecip(out, in_, reads, writes, joint=False):
        return P.op(DVE, lambda e: e.reciprocal(out=out, in_=in_), reads=reads, writes=writes, joint=joint)

    def tiles_of(tb):
        t0, n = tb
        return list(range(t0 // 128, (t0 + n) // 128))

    def phase0(es):
        for t in range(2):
            dma(SP, x_sb[:, t, :], ctx_in[t * 128:(t + 1) * 128, :], [], [xb[t]], xb[t])
        for t in range(16):
            dma(SP, x_sb[:, 2 + t, :], x_in[t * 128:(t + 1) * 128, :], [], [xb[2 + t]], xb[2 + t])
        dma(SP, identF[:], identF_in[:, :], [], [cst], cst)
        dma(SP, matsB[:], matsB_in[:, :, :], [], [cst], cst)
        P.op(DVE, lambda e: e.memset(epsT[:], EPS), writes=[cst], joint=True)
        cfm = sb(es, "cfm", [128, 8, 2], F32)
        silu2 = sb(es, "silu2", [128, 8, 2], F32)
        b_c = Buf("cfm")
        b_s = Buf("silu2")
        dma(SP, cfm[:], cfm_in[:, :, :], [], [b_c], b_c)
        act(silu2[:], cfm[:], AF.Silu, [b_c], [b_s])
        ones2 = sb(es, "ones2", [1, 2], F32)
        b_o2 = Buf("ones2")
        P.op(DVE, lambda e: e.memset(ones2[:], 1.0), writes=[b_o2])
        wr = Ring(es, nc, "adaw", [128, 8, 512], F32, 2)
        br = Ring(es, nc, "adab", [1, 512], F32, 2)
        rows = sb(es, "rows", [2, 6 * D], F32)
        b_rows = Buf("rows")
        pr = Ring(es, nc, "p0ps", [128, 512], F32, 2, psum=True)
        pt = Ring(es, nc, "p0pt", [128, 512], F32, 1, psum=True)
        for L in range(nlayers):
            for cb in range(12):
                w, bw = wr.next()
                bt, bbt = br.next()
                dma(SP, w[:], ada_w[L].rearrange("(c p) n -> p c n", p=128)[:, :, cb * 512:(cb + 1) * 512], [], [bw], bw)
                dma(SP, bt[:], ada_b[L:L + 1, cb * 512:(cb + 1) * 512], [], [bbt], bbt)
                ps, bps = pr.next()
                for kc in range(8):
                    mm(ps[0:2, :], silu2[:, kc, :], w[:, kc, :], kc == 0, False, [b_s, bw], bps)
                mm(ps[0:2, :], ones2[:, :], bt[:, :], False, True, [b_o2, bbt], bps)
                P.op(DVE, lambda e, ps=ps, cb=cb: e.tensor_copy(out=rows[:, cb * 512:(cb + 1) * 512], in_=ps[0:2, :]),
                     reads=[bps], writes=[b_rows], joint=True)
            dma(SP, modrows[L], rows[:, :], [b_rows], [], b_rows)
            tp, btp = pt.next()
            for j in range(48):
                P.op(PE, lambda e, j=j, tp=tp: e.transpose(out=tp[:, 2 * j:2 * j + 2], in_=rows[0:2, j * 128:(j + 1) * 128], identity=identF[0:2, 0:2]),
                     reads=[b_rows, cst], writes=[btp], joint=(j > 0))
            P.op(DVE, lambda e, tp=tp, L=L: e.tensor_copy(out=modF[:, L, :, :].rearrange("p r j -> p j r"), in_=tp[:, 0:96].rearrange("p (j r) -> p j r", r=2)),
                 reads=[btp], writes=[b_modF], joint=True)

    def norm_phase(es, L, which, hT, hb, tbs, moe=None):
        gsrc = (norm_mix if which == 0 else norm_ffn)
        gF = sb(es, "gF", [128, 8], F32)
        AB = sb(es, "AB", [128, 2, 2, 8], F32)
        b_g = Buf("gF")
        b_AB = Buf("AB")
        dma(SP, gF[:], gsrc[L].rearrange("(c p) -> p c", p=128), [], [b_g], b_g, allow_slow_non_contiguous=True)
        ish, isc = (0, 1) if which == 0 else (3, 4)
        for r in range(2):
            stt(AB[:, r, 0, :], modF[:, L, r, isc * 8:(isc + 1) * 8], 1.0, gF[:], ALU.add, ALU.mult, [b_modF, b_g], [b_AB])
            P.op(DVE, lambda e, r=r: e.tensor_copy(out=AB[:, r, 1, :], in_=modF[:, L, r, ish * 8:(ish + 1) * 8]), reads=[b_modF], writes=[b_AB], joint=True)
        junk = sb(es, "junk", [128, D], BF16)
        b_junk = Buf("junk")
        ss = sb(es, "ss", [128, NT], F32)
        rstd = sb(es, "rstd", [128, NT], F32)
        b_ss = [Buf(f"ss{i}") for i in range(5)]
        b_rstd = [Buf(f"rstd{i}") for i in range(5)]
        P.op(DVE, lambda e: e.memset(ss[:], 0.0), writes=b_ss)
        xnr = Ring(es, nc, "xn", [128, 4, D], F32, 1)
        ptr = Ring(es, nc, "nps", [128, 512], F32, 2, psum=True)
        if moe is not None:
            h32r = Ring(es, nc, "h32", [128, 8, 512], F32, 1)
            R32 = sb(es, "R32", [128, 8, 8], F32)
            b_R = Buf("R32")
            dma(SP, R32[:], moe["router"].rearrange("(c p) e -> p c e", p=128), [], [b_R], b_R)
            lgr = Ring(es, nc, "lgps", [128, 8], F32, 2, psum=True)
        for bi, tb in enumerate(TBS):
            if tb not in tbs:
                continue
            t0, n = tb
            tl = tiles_of(tb)
            r = 1 if bi == 0 else 0
            for t in tl:
                act(junk[:], x_sb[:, t, :], AF.Square, [xb[t]], [b_junk, b_ss[bi]], joint=False, accum_out=ss[:, t:t + 1])
            act(rstd[:, tl[0]:tl[-1] + 1], ss[:, tl[0]:tl[-1] + 1], AF.Sqrt, [b_ss[bi], cst], [b_rstd[bi]], joint=False, scale=1.0 / D, bias=epsT[:, 0:1])
            recip(rstd[:, tl[0]:tl[-1] + 1], rstd[:, tl[0]:tl[-1] + 1], [b_rstd[bi]], [b_rstd[bi]], joint=False)
            xn, bxn = xnr.next()
            for j, t in enumerate(tl):
                ts(xn[:, j, :], x_sb[:, t, :], rstd[:, t:t + 1], None, ALU.mult, None, [xb[t], b_rstd[bi]], [bxn])
            if moe is not None:
                h32, bh32 = h32r.next()
            for c in range(8):
                ps, bps = ptr.next()
                for j, t in enumerate(tl):
                    P.op(PE, lambda e, ps=ps, j=j, c=c, xn=xn: e.transpose(out=ps[:, j * 128:(j + 1) * 128], in_=xn[:, j, c * 128:(c + 1) * 128], identity=identF[:]),
                         reads=[bxn, cst], writes=[bps], joint=(j > 0))
                act(hT[:, c, t0:t0 + n], ps[:, 0:n], AF.Identity, [bps, b_AB], [hb[bi]], scale=AB[:, r, 0, c:c + 1], bias=AB[:, r, 1, c:c + 1])
                if moe is not None:
                    ts(h32[:, c, 0:n], ps[:, 0:n], AB[:, r, 0, c:c + 1], AB[:, r, 1, c:c + 1], ALU.mult, ALU.add, [bps, b_AB], [bh32])
            if moe is not None:
                for j, t in enumerate(tl):
                    lp, blp = lgr.next()
                    for c in range(8):
                        mm(lp[:, :], h32[:, c, j * 128:(j + 1) * 128], R32[:, c, :], c == 0, c == 7, [bh32, b_R], blp)
                    P.op(DVE, lambda e, lp=lp, t=t: e.tensor_copy(out=moe["lg"][:, t, :], in_=lp[:, :]), reads=[blp], writes=[moe["blg"]], joint=True)

    def route(es, moe, comb, b_comb):
        lg = moe["lg"]
        blg = moe["blg"]
        m8 = sb(es, "m8", [128, NT, 8], F32)
        tmp = sb(es, "rtmp", [128, NT, 8], F32)
        msk = sb(es, "rmsk", [128, NT, 8], F32)
        den = sb(es, "rden", [128, NT], F32)
        b1, b2, b3, b4 = Buf("m8"), Buf("rtmp"), Buf("rmsk"), Buf("rden")
        for t in range(NT):
            P.op(DVE, lambda e, t=t: e.max(out=m8[:, t, :], in_=lg[:, t, :]), reads=[blg], writes=[b1], joint=True)
        tt(msk[:], lg[:], m8[:, :, 1:2].to_broadcast([128, NT, 8]), ALU.is_ge, [blg, b1], [b3])
        tt(tmp[:], lg[:], m8[:, :, 0:1].to_broadcast([128, NT, 8]), ALU.subtract, [blg, b1], [b2])
        act(tmp[:], tmp[:], AF.Exp, [b2], [b2], joint=False)
        tt(tmp[:], tmp[:], msk[:], ALU.mult, [b2, b3], [b2], joint=False)
        P.op(DVE, lambda e: e.reduce_sum(out=den[:], in_=tmp[:], axis=AX.X), reads=[b2], writes=[b4])
        recip(den[:], den[:], [b4], [b4], joint=False)
        tt(comb[:], tmp[:], den[:].unsqueeze(2).to_broadcast([128, NT, 8]), ALU.mult, [b2, b4], [b_comb], joint=False)

    def load_gate_rows(es, L, idx):
        g = sb(es, "grow", [128, 2, D], F32)
        bg = Buf("grow")
        for r in range(2):
            dma(SP, g[:, r, :], modrows[L, r:r + 1, idx * D:(idx + 1) * D].partition_broadcast(128), [], [bg], bg)
        return g, bg

    def ffn_phase(es, L, hT, hb, tbs, experts, comb, b_comb):
        grow, bgrow = load_gate_rows(es, L, 5)
        w13r = Ring(es, nc, "w13", [128, 8, 1024], BF16, 2)
        w2r = Ring(es, nc, "w2", [128, 4, D], BF16, 2)
        has_ctx = TBS[0] in tbs
        if has_ctx:
            w2cr = Ring(es, nc, "w2c", [128, 4, D], BF16, 2)
        y = sb(es, "y", [128, 4, T], BF16)
        yb = [Buf(f"y{i}") for i in range(5)]
        sgr = Ring(es, nc, "sg", [128, 512], BF16, 3)
        gpr = Ring(es, nc, "gps", [128, 512], F32, 2, psum=True)
        upr = Ring(es, nc, "ups", [128, 512], F32, 2, psum=True)
        opr = Ring(es, nc, "ops", [128, 512], F32, 3, psum=True)
        for ei, (w13, w2) in enumerate(experts):
            w13v = w13.rearrange("(c p) n -> p c n", p=128)
            w2v = w2.rearrange("(c p) n -> p c n", p=128)
            for fb in range(7):
                wa, bwa = w13r.next()
                wb_, bwb = w2r.next()
                dma(POOL, wa[:, :, 0:512], w13v[:, :, fb * 512:(fb + 1) * 512], [], [bwa], bwa)
                dma(POOL, wa[:, :, 512:1024], w13v[:, :, 3584 + fb * 512:3584 + (fb + 1) * 512], [], [bwa], bwa)
                dma(POOL, wb_[:], w2v[:, fb * 4:(fb + 1) * 4, :], [], [bwb], bwb)
                if has_ctx:
                    wc_, bwc = w2cr.next()
                    tt(wc_[:], wb_[:], grow[:, 1, :].unsqueeze(1).to_broadcast([128, 4, D]), ALU.mult, [bwb, bgrow], [bwc], joint=False, eng=POOL)
                tt(wb_[:], wb_[:], grow[:, 0, :].unsqueeze(1).to_broadcast([128, 4, D]), ALU.mult, [bwb, bgrow], [bwb], joint=False, eng=POOL)
                for bi, tb in enumerate(TBS):
                    if tb not in tbs:
                        continue
                    t0, n = tb
                    for fc in range(4):
                        gp, bgp = gpr.next()
                        up, bup = upr.next()
                        for kc in range(8):
                            mm(gp[:, 0:n], wa[:, kc, fc * 128:(fc + 1) * 128], hT[:, kc, t0:t0 + n], kc == 0, kc == 7, [bwa, hb[bi]], bgp)
                        for kc in range(8):
                            mm(up[:, 0:n], wa[:, kc, 512 + fc * 128:512 + (fc + 1) * 128], hT[:, kc, t0:t0 + n], kc == 0, kc == 7, [bwa, hb[bi]], bup)
                        sg, bsg = sgr.next()
                        act(sg[:, 0:n], gp[:, 0:n], AF.Silu, [bgp], [bsg], joint=False)
                        tt(y[:, fc, t0:t0 + n], sg[:, 0:n], up[:, 0:n], ALU.mult, [bsg, bup], [yb[bi]])
                for bi, tb in enumerate(TBS):
                    if tb not in tbs:
                        continue
                    r = 1 if bi == 0 else 0
                    for t in tiles_of(tb):
                        for dh in range(2):
                            op_, bop = opr.next()
                            wsel, bwsel = (wc_, bwc) if r == 1 else (wb_, bwb)
                            for fc in range(4):
                                mm(op_[:, :], y[:, fc, t * 128:(t + 1) * 128], wsel[:, fc, dh * 512:(dh + 1) * 512], fc == 0, fc == 3, [yb[bi], bwsel], bop)
                            xs = x_sb[:, t, dh * 512:(dh + 1) * 512]
                            if comb is None:
                                tt(xs, op_[:, :], xs, ALU.add, [bop, xb[t]], [xb[t]], joint=False)
                            else:
                                stt(xs, op_[:, :], comb[:, t, ei:ei + 1], xs, ALU.mult, ALU.add, [bop, xb[t], b_comb], [xb[t]], joint=False)

    def load_w_chunk(ring, wv, pieces, kc_n):
        w, bw = ring.next()
        off = 0
        for (c0, ncol) in pieces:
            dma(POOL, w[:, 0:kc_n, off:off + ncol], wv[:, :, c0:c0 + ncol], [], [bw], bw)
            off += ncol
        return w, bw

    def gain_tile(es, pieces):
        g = sb(es, "gain", [128, 1], F32)
        bg = Buf("gain")
        off = 0
        for ap in pieces:
            n = ap.shape[0]
            dma(SP, g[off:off + n, :], ap.rearrange("(p o) -> p o", o=1), [], [bg], bg)
            off += n
        return g, bg

    class ProjCtx:
        pass

    def proj_setup(es, need_rope):
        pc = ProjCtx()
        pc.wring = Ring(es, nc, "wch", [128, 8, 128], BF16, 3)
        pc.pps = Ring(es, nc, "pps", [128, 512], F32, 3, psum=True)
        pc.sps = Ring(es, nc, "sps", [128, 512], F32, 1, psum=True)
        pc.rps = Ring(es, nc, "rps", [128, 512], F32, 1, psum=True)
        pc.sq = Ring(es, nc, "sq", [128, 512], BF16, 3)
        pc.qg = Ring(es, nc, "qg", [128, 512], BF16, 3)
        pc.rstd = Ring(es, nc, "prstd", [128, 512], F32, 3)
        pc.t1 = Ring(es, nc, "pt1", [128, 512], F32, 1)
        pc.t2 = Ring(es, nc, "pt2", [128, 512], F32, 1)
        pc.ob = Ring(es, nc, "pob", [128, 512], BF16, 3)
        pc.rope = None
        if need_rope is not None:
            pc.rope = sb(es, "rope", [128, 2, T], F32)
            pc.b_rope = Buf("rope")
            i0 = 0 if need_rope == 128 else 2
            for k in range(2):
                dma(SP, pc.rope[:, k, :], rope_in[i0 + k], [], [pc.b_rope], pc.b_rope)
        return pc

    def proj_group(pc, src, src_bufs, kc_n, chunks, tbs, group, rope, dsts):
        nch = len(chunks)
        for bi, tb in enumerate(TBS):
            if tb not in tbs:
                continue
            t0, n = tb
            pss = []
            for (w, bw, g, bg) in chunks:
                ps, bps = pc.pps.next()
                for kc in range(kc_n):
                    mm(ps[:, 0:n], w[:, kc, :], src[:, kc, t0:t0 + n], kc == 0, kc == kc_n - 1, [bw, src_bufs[bi]], bps)
                pss.append((ps, bps))
            qgs = []
            sp, bsp = pc.sps.next()
            for ci, (ps, bps) in enumerate(pss):
                (w, bw, g, bg) = chunks[ci]
                sq, bsq = pc.sq.next()
                act(sq[:, 0:n], ps[:, 0:n], AF.Square, [bps], [bsq], joint=False)
                qg, bqg = pc.qg.next()
                ts(qg[:, 0:n], ps[:, 0:n], g[:, 0:1], None, ALU.mult, None, [bps, bg], [bqg], joint=False)
                qgs.append((qg, bqg))
                onesm = ones_bd if group == 64 else ones128
                if group == "all":
                    mm(sp[:, 0:n], onesm, sq[:, 0:n], ci == 0, ci == nch - 1, [bsq, cst], bsp)
                else:
                    assert nch == 1
                    mm(sp[:, 0:n], onesm, sq[:, 0:n], True, True, [bsq, cst], bsp)
            cnt = {128: 128, 64: 64, "all": 128 * nch}[group]
            rs0, brs0 = pc.rstd.next()
            act(rs0[:, 0:n], sp[:, 0:n], AF.Ln, [bsp, cst], [brs0], joint=False, scale=1.0 / cnt, bias=epsT[:, 0:1])
            rs, brs = pc.rstd.next()
            act(rs[:, 0:n], rs0[:, 0:n], AF.Exp, [brs0], [brs], joint=False, scale=-0.5)
            for ci, (qg, bqg) in enumerate(qgs):
                ob, bob = pc.ob.next()
                if rope is not None:
                    rp, brp = pc.rps.next()
                    mm(rp[:, 0:n], perm128 if rope == 128 else perm64, qg[:, 0:n], True, True, [bqg, cst], brp)
                    t1, bt1 = pc.t1.next()
                    t2, bt2 = pc.t2.next()
                    tt(t1[:, 0:n], qg[:, 0:n], pc.rope[:, 0, t0:t0 + n], ALU.mult, [bqg, pc.b_rope], [bt1], joint=False)
                    tt(t2[:, 0:n], rp[:, 0:n], pc.rope[:, 1, t0:t0 + n], ALU.mult, [brp, pc.b_rope], [bt2], joint=False)
                    tt(t1[:, 0:n], t1[:, 0:n], t2[:, 0:n], ALU.add, [bt1, bt2], [bt1], joint=False)
                    tt(ob[:, 0:n], t1[:, 0:n], rs[:, 0:n], ALU.mult, [bt1, brs], [bob], joint=False)
                else:
                    tt(ob[:, 0:n], qg[:, 0:n], rs[:, 0:n], ALU.mult, [bqg, brs], [bob], joint=False)
                d = dsts[ci]
                if d[0] == "dram":
                    dma(SP, d[1][:, t0:t0 + n], ob[:, 0:n], [bob], [], bob)
                else:
                    P.op(POOL, lambda e, d=d, ob=ob, t0=t0, n=n: e.tensor_copy(out=d[1][:, t0:t0 + n], in_=ob[:, 0:n]),
                         reads=[bob], writes=[d[2][bi]], joint=True)

    def proj_v(es, pc, src, src_bufs, kc_n, wv, pieces, tbs):
        F = sum(p[1] for p in pieces)
        wt = sb(es, "wv", [128, kc_n, F], BF16)
        bwt = Buf("wv")
        off = 0
        for (c0, ncol) in pieces:
            dma(POOL, wt[:, :, off:off + ncol], wv[:, :, c0:c0 + ncol], [], [bwt], bwt)
            off += ncol
        vps = pc.pps
        vob = Ring(es, nc, "vob", [128, 1024], BF16, 1)
        for bi, tb in enumerate(TBS):
            if tb not in tbs:
                continue
            for t in tiles_of(tb):
                vo, bvo = vob.next()
                for f0 in range(0, F, 512):
                    fn_ = min(512, F - f0)
                    ps, bps = vps.next()
                    for kc in range(kc_n):
                        mm(ps[:, 0:fn_], src[:, kc, t * 128:(t + 1) * 128], wt[:, kc, f0:f0 + fn_], kc == 0, kc == kc_n - 1, [src_bufs[bi], bwt], bps)
                    P.op(DVE, lambda e, vo=vo, ps=ps, f0=f0, fn_=fn_: e.tensor_copy(out=vo[:, f0:f0 + fn_], in_=ps[:, 0:fn_]),
                         reads=[bps], writes=[bvo], joint=True)
                dma(SP, Vs[t * 128:(t + 1) * 128, 0:F], vo[:, 0:F], [bvo], [], bvo)

    def attn_phase(es, AO, aob, units, scale, masks=None):
        kq = {}
        ldr = Ring(es, nc, "akq", [128, T], BF16, 8)
        vr = Ring(es, nc, "av", [128, NT, 128], BF16, 4)
        depth = 3 if masks is not None else 2
        spr = Ring(es, nc, "asps", [128, 512], F32, depth + 1, psum=True)
        nacc = len(set(s_["acc"] for u_ in units for s_ in u_["subs"]))
        ftm = [Ring(es, nc, f"aft{i}", [128, 512], F32, 2) for i in range(5 if nacc == 2 else 3)]
        opr = [Ring(es, nc, "aops", [128, 512], F32, 2 if nacc == 1 else 1, psum=True) for _ in range(nacc)]
        lpr = [Ring(es, nc, "alps", [128, 512], F32, 2 if nacc == 1 else 1, psum=True) for _ in range(nacc)]
        fpr = Ring(es, nc, "afps", [128, 512], F32, 1, psum=True) if masks is None else None
        ptr = Ring(es, nc, "apt", [128, 512], BF16, 6)
        fsq = Ring(es, nc, "afsq", [128, 512], BF16, 2)
        if masks is not None:
            wm = sb(es, "wm", [128, 6, 512], BF16)
            b_wm = Buf("wm")
            dma(SP, wm[:], wmask_in[:, :, :], [], [b_wm], b_wm)
        cache = {}
        R = dict(ftm=ftm, fsq=fsq, fpr=fpr)
        items = []
        for u in units:
            for qi, (q0, qn, ktiles_fn) in enumerate(u["qblocks"]):
                work = []
                for s_ in u["subs"]:
                    for kt in ktiles_fn():
                        work.append((s_, kt))
                lastidx = {}
                firstidx = {}
                for i, (s_, kt) in enumerate(work):
                    lastidx[s_["acc"]] = i
                    firstidx.setdefault(s_["acc"], i)
                for i, (s_, kt) in enumerate(work):
                    a_ = s_["acc"]
                    items.append(dict(u=u, q0=q0, qn=qn, s=s_, kt=kt, first=(i == firstidx[a_]), last=(i == lastidx[a_]),
                                      end=(i == len(work) - 1), start_qb=(i == 0), start_unit=(i == 0 and qi == 0)))

        def do_loads(u):
            nonlocal cache
            tl = {}
            for ld in u["loads"]:
                key, ap = ld[0], ld[1]
                if key in cache:
                    tl[key] = cache[key]
                    continue
                tle, btl = ldr.next()
                if len(ld) > 2:
                    r0_, nr_ = ld[2], ld[3]
                    P.op(DVE, lambda e, tle=tle: e.memset(tle[:], 0.0), writes=[btl])
                    dma(SP, tle[r0_:r0_ + nr_, :], ap[r0_:r0_ + nr_, :], [], [btl], btl, joint=False)
                else:
                    dma(SP, tle[:], ap, [], [btl], btl, joint=False)
                cache = {k: v for k, v in cache.items() if v[0] is not tle}
                cache[key] = (tle, btl)
                tl[key] = (tle, btl)
            for (key, c0, ncol, pad) in u["vloads"]:
                if key in cache:
                    tl[key] = cache[key]
                    continue
                tle, btl = vr.next()
                if ncol < 128:
                    P.op(DVE, lambda e, tle=tle: e.memset(tle[:], 0.0), writes=[btl])
                dma(SP, tle[:, :, pad:pad + ncol], Vs[:, c0:c0 + ncol].rearrange("(t p) f -> p t f", p=128), [], [btl], btl, joint=False)
                cache = {k: v for k, v in cache.items() if v[0] is not tle}
                cache[key] = (tle, btl)
                tl[key] = (tle, btl)
            return tl

        def do_OL(it):
            s_ = it["s"]
            a_ = s_["acc"]
            qn = it["qn"]
            O, Lp = it["O"], it["Lp"]
            vt, bv = it["tl"][s_["v"]]
            pt_, bpt = it["pt"]
            ktile = it["kt"][0]
            mm(O[a_][0][:, 0:qn], vt[:, ktile, :], pt_[:, 0:qn], it["first"], it["last"], [bv, bpt], O[a_][1])
            mm(Lp[a_][0][:, 0:qn], s_["ones"], pt_[:, 0:qn], it["first"], it["last"], [cst, bpt], Lp[a_][1])
            if it["end"]:
                cont = it["u"]["fin"](it["u"], it["q0"], qn, O, Lp, R)
                if cont is not None:
                    deferred.append([12 if nacc == 2 else 4, cont])

        pend = []
        deferred = []
        cur_tl = None
        cur_O = cur_L = None
        for it in items:
            u = it["u"]
            if it["start_unit"]:
                cur_tl = do_loads(u)
            if it["start_qb"]:
                accs = sorted(set(s_["acc"] for s_ in u["subs"]))
                cur_O = {a_: opr[a_].next() for a_ in accs}
                cur_L = {a_: lpr[a_].next() for a_ in accs}
            it["tl"], it["O"], it["Lp"] = cur_tl, cur_O, cur_L
            s_ = it["s"]
            q0, qn = it["q0"], it["qn"]
            ktile = it["kt"][0]
            sp, bsp = spr.next()
            npairs = len(s_["pairs"])
            for pi, (kkey, qkey, r0, nr) in enumerate(s_["pairs"]):
                kt_, bk = cur_tl[kkey]
                qt_, bq = cur_tl[qkey]
                mm(sp[:, 0:qn], kt_[r0:r0 + nr, ktile * 128:(ktile + 1) * 128], qt_[r0:r0 + nr, q0:q0 + qn], pi == 0, pi == npairs - 1, [bk, bq], bsp)
            pt_, bpt = ptr.next()
            act(pt_[:, 0:qn], sp[:, 0:qn], AF.Exp, [bsp], [bpt], joint=False, scale=scale)
            if it["kt"][1] is not None:
                tt(pt_[:, 0:qn], pt_[:, 0:qn], wm[:, it["kt"][1], 0:qn], ALU.mult, [bpt, b_wm], [bpt], joint=False)
            it["pt"] = (pt_, bpt)
            pend.append(it)
            if len(pend) > depth:
                do_OL(pend.pop(0))
            for d_ in list(deferred):
                d_[0] -= 1
                if d_[0] <= 0:
                    deferred.remove(d_)
                    d_[1]()
        while pend:
            do_OL(pend.pop(0))
        for d_ in deferred:
            d_[1]()

    def fin_default(extra=None):
        def fin(u, q0, qn, O, Lp, R):
            r, br = R["ftm"][0].next()
            if extra is not None:
                es_t, bes = extra
                r2, br2 = R["ftm"][1].next()

                def stage_b():
                    act(r2[:, 0:qn], Lp[0][0][:, 0:qn], AF.Ln, [Lp[0][1], bes], [br2], joint=False, bias=es_t[:, u["c"]:u["c"] + 1])
                    act(r[:, 0:qn], r2[:, 0:qn], AF.Exp, [br2], [br], joint=False, scale=-1.0)
                    tt(u["AO"][:, u["c"], q0:q0 + qn], O[0][0][:, 0:qn], r[:, 0:qn], ALU.mult, [O[0][1], br], [u["aob"]], joint=True)
                return stage_b
            else:
                frecip(r[:, 0:qn], Lp[0][0][:, 0:qn], [Lp[0][1]], [br])
            tt(u["AO"][:, u["c"], q0:q0 + qn], O[0][0][:, 0:qn], r[:, 0:qn], ALU.mult, [O[0][1], br], [u["aob"]], joint=True)
        return fin

    def all_k():
        return [(k, None) for k in range(NT)]

    def ctx_k():
        return [(0, None), (1, None)]

    LAT_QB = [(256 + 512 * i, 512, all_k) for i in range(4)]
    CTX_QB = [(0, 256, ctx_k)]

    def std_layer(L):
        kind = L % 4
        need_ctx = L < 3
        moe = (L % 2 == 1)
        tbs_all = list(TBS)
        tbs_lat = TBS[1:]
        hT = None
        state = {}

        def phA(es):
            hT = sb(es, "hT", [128, 8, T], BF16)
            hb = [Buf(f"hT{i}") for i in range(5)]
            norm_phase(es, L, 0, hT, hb, tbs_all)
            if kind == 0:
                wv = gqa_wqkv[0].rearrange("(c p) n -> p c n", p=128)
                pc = proj_setup(es, 128)
                gq, bgq = gain_tile(es, [gqa_q_gain[0]])
                gk, bgk = gain_tile(es, [gqa_k_gain[0]])
                for h in range(8):
                    w, bw = load_w_chunk(pc.wring, wv, [(h * 128, 128)], 8)
                    proj_group(pc, hT, hb, 8, [(w, bw, gq, bgq)], tbs_all, 128, 128, [("dram", Qs[h])])
                for kvh in range(2):
                    w, bw = load_w_chunk(pc.wring, wv, [(1024 + kvh * 128, 128)], 8)
                    proj_group(pc, hT, hb, 8, [(w, bw, gk, bgk)], tbs_all, 128, 128, [("dram", Ks[kvh])])
                proj_v(es, pc, hT, hb, 8, wv, [(1280, 256)], tbs_all)
            elif kind == 2:
                wv = win_wqkv[0].rearrange("(c p) n -> p c n", p=128)
                pc = proj_setup(es, 64)
                gq, bgq = gain_tile(es, [win_q_gain[0], win_q_gain[0]])
                gk, bgk = gain_tile(es, [win_k_gain[0], win_k_gain[0]])
                for c in range(8):
                    w, bw = load_w_chunk(pc.wring, wv, [(c * 128, 128)], 8)
                    proj_group(pc, hT, hb, 8, [(w, bw, gq, bgq)], tbs_all, 64, 64, [("dram", Qs[c])])
                for kvh in range(2):
                    w, bw = load_w_chunk(pc.wring, wv, [(1024 + kvh * 64, 64), (1024 + kvh * 64, 64)], 8)
                    proj_group(pc, hT, hb, 8, [(w, bw, gk, bgk)], tbs_all, 64, 64, [("dram", Ks[kvh])])
                proj_v(es, pc, hT, hb, 8, wv, [(1152, 128)], tbs_all)
            elif kind == 3:
                wv = diff_wqkv[0].rearrange("(c p) n -> p c n", p=128)
                pc = proj_setup(es, 64)
                gq, bgq = gain_tile(es, [diff_q_gain[0], diff_q_gain[0]])
                gk, bgk = gain_tile(es, [diff_k_gain[0], diff_k_gain[0]])
                for h in range(8):
                    w, bw = load_w_chunk(pc.wring, wv, [(h * 128, 128)], 8)
                    proj_group(pc, hT, hb, 8, [(w, bw, gq, bgq)], tbs_lat, 64, 64, [("dram", Qs[h])])
                for h in range(8):
                    w, bw = load_w_chunk(pc.wring, wv, [(1024 + h * 128, 128)], 8)
                    proj_group(pc, hT, hb, 8, [(w, bw, gk, bgk)], tbs_all, 64, 64, [("dram", Ks[h])])
                proj_v(es, pc, hT, hb, 8, wv, [(2048, 1024)], tbs_all)
            else:
                wd = mla_wdown[0].rearrange("(c p) n -> p c n", p=128)
                pc = proj_setup(es, 64)
                cqn = sb(es, "cqn", [128, 3, T], BF16)
                ckvn = sb(es, "ckvn", [128, 2, T], BF16)
                bcq = [Buf(f"cqn{i}") for i in range(5)]
                bckv = [Buf(f"ckvn{i}") for i in range(5)]
                ch = []
                for c in range(3):
                    w, bw = load_w_chunk(pc.wring, wd, [(c * 128, 128)], 8)
                    g, bg = gain_tile(es, [mla_qa_gain[0, c * 128:(c + 1) * 128]])
                    ch.append((w, bw, g, bg))
                proj_group(pc, hT, hb, 8, ch, tbs_all, "all", None, [("sbuf", cqn[:, c, :], bcq) for c in range(3)])
                ch = []
                for c in range(2):
                    w, bw = load_w_chunk(pc.wring, wd, [(384 + c * 128, 128)], 8)
                    g, bg = gain_tile(es, [mla_kva_gain[0, c * 128:(c + 1) * 128]])
                    ch.append((w, bw, g, bg))
                proj_group(pc, hT, hb, 8, ch, tbs_all, "all", None, [("sbuf", ckvn[:, c, :], bckv) for c in range(2)])
                w, bw = load_w_chunk(pc.wring, wd, [(640, 64), (640, 64)], 8)
                g, bg = gain_tile(es, [mla_k_gain[0, 128:192], mla_k_gain[0, 128:192]])
                proj_group(pc, hT, hb, 8, [(w, bw, g, bg)], tbs_all, 64, 64, [("dram", Ks[8])])
                wq = mla_wuq[0].rearrange("(c p) n -> p c n", p=128)
                wkv = mla_wukv[0].rearrange("(c p) n -> p c n", p=128)
                gqn, bgqn = gain_tile(es, [mla_q_gain[0, 0:128]])
                gqp, bgqp = gain_tile(es, [mla_q_gain[0, 128:192], mla_q_gain[0, 128:192]])
                gkn, bgkn = gain_tile(es, [mla_k_gain[0, 0:128]])
                for h in range(8):
                    w, bw = load_w_chunk(pc.wring, wq, [(h * 192, 128)], 3)
                    proj_group(pc, cqn, bcq, 3, [(w, bw, gqn, bgqn)], tbs_all, 128, None, [("dram", Qs[h])])
                for c in range(4):
                    w, bw = load_w_chunk(pc.wring, wq, [(2 * c * 192 + 128, 64), ((2 * c + 1) * 192 + 128, 64)], 3)
                    proj_group(pc, cqn, bcq, 3, [(w, bw, gqp, bgqp)], tbs_all, 64, 64, [("dram", Qs[8 + c])])
                for h in range(8):
                    w, bw = load_w_chunk(pc.wring, wkv, [(h * 256, 128)], 2)
                    proj_group(pc, ckvn, bckv, 2, [(w, bw, gkn, bgkn)], tbs_all, 128, None, [("dram", Ks[h])])
                proj_v(es, pc, ckvn, bckv, 2, wkv, [(h * 256 + 128, 128) for h in range(8)], tbs_all)

            if dbg and L == nlayers - 1:
                bd = Buf("dbgh")
                dma(SP, dbg_hT[:, :, :], hT[:], hb, [], bd)
                bd2 = Buf("dbgm")
                dma(SP, dbg_modF[:, :, :, :], modF[:], [b_modF], [], bd2)

        run_phase(phA)

        def phBC(es0):
            AO = sb(es0, "AO", [128, 8, T], BF16)
            aob = Buf("AO")

            def phB(es):
                qbs = (CTX_QB if need_ctx else []) + LAT_QB
                units = []
                if kind == 0:
                    scale = 128 ** -0.5
                    for h in range(8):
                        kvh = h // 4
                        units.append(dict(c=h, loads=[(("q", h), Qs[h]), (("k", kvh), Ks[kvh])], vloads=[(("v", kvh), kvh * 128, 128, 0)],
                                          subs=[dict(pairs=[(("k", kvh), ("q", h), 0, 128)], v=("v", kvh), ones=ones128, acc=0)],
                                          qblocks=qbs, fin=fin_default(), AO=AO, aob=aob))
                    attn_phase(es, AO, aob, units, scale)
                elif kind == 1:
                    scale = 192 ** -0.5
                    for h in range(8):
                        r0 = (h % 2) * 64
                        units.append(dict(c=h, loads=[(("q", h), Qs[h]), (("qp", h), Qs[8 + h // 2], r0, 64), (("k", h), Ks[h]), (("kp", 0), Ks[8])],
                                          vloads=[(("v", h), h * 128, 128, 0)],
                                          subs=[dict(pairs=[(("k", h), ("q", h), 0, 128), (("kp", 0), ("qp", h), 0, 128)], v=("v", h), ones=ones128, acc=0)],
                                          qblocks=qbs, fin=fin_default(), AO=AO, aob=aob))
                    attn_phase(es, AO, aob, units, scale)
                elif kind == 2:
                    scale = 64 ** -0.5
                    esk = sb(es, "esk", [128, 8], F32)
                    besk = Buf("esk")
                    sv = win_sink[0].rearrange("(c two) -> two c", two=2)
                    dma(SP, esk[0:64, :], sv[0:1, :].partition_broadcast(64), [], [besk], besk, allow_slow_non_contiguous=True)
                    dma(SP, esk[64:128, :], sv[1:2, :].partition_broadcast(64), [], [besk], besk, allow_slow_non_contiguous=True)
                    act(esk[:], esk[:], AF.Exp, [besk], [besk], joint=False)

                    def mk_kfn(qb):
                        def kfn():
                            i0 = 4 * qb
                            res = [(0, None), (1, None)]
                            for j in range(max(0, i0 - 1), min(15, i0 + 4) + 1):
                                res.append((2 + j, j - i0 + 1))
                            return res
                        return kfn
                    wqbs = (CTX_QB if need_ctx else []) + [(256 + 512 * i, 512, mk_kfn(i)) for i in range(4)]
                    for c in range(8):
                        kvh = c // 4
                        subs = []
                        for half in range(2):
                            subs.append(dict(pairs=[(("k", kvh), ("q", c, half), 0, 128)], v=("v", kvh, half), ones=(ones_lo if half == 0 else ones_hi), acc=0))
                        units.append(dict(c=c, loads=[(("q", c, 0), Qs[c], 0, 64), (("q", c, 1), Qs[c], 64, 64), (("k", kvh), Ks[kvh])],
                                          vloads=[(("v", kvh, 0), kvh * 64, 64, 0), (("v", kvh, 1), kvh * 64, 64, 64)],
                                          subs=subs, qblocks=wqbs, fin=fin_default((esk, besk)), AO=AO, aob=aob))
                    attn_phase(es, AO, aob, units, scale, masks=True)
                else:
                    scale = 64 ** -0.5
                    lam_init = 0.8 - 0.6 * math.exp(-0.3 * L)
                    lp = sb(es, "lp", [128, 4, 64], F32)
                    blp = Buf("lp")
                    dma(SP, lp[:], diff_lambda[0:1].partition_broadcast(128), [], [blp], blp)
                    lpp = sb(es, "lpp", [128, 2, 64], F32)
                    lsum = sb(es, "lsum", [128, 2], F32)
                    nlam = sb(es, "nlam", [128, 1], F32)
                    bl2 = Buf("lpp")
                    bl3 = Buf("lsum")
                    bnl = Buf("nlam")
                    tt(lpp[:, 0, :], lp[:, 0, :], lp[:, 1, :], ALU.mult, [blp], [bl2])
                    tt(lpp[:, 1, :], lp[:, 2, :], lp[:, 3, :], ALU.mult, [blp], [bl2])
                    P.op(DVE, lambda e: e.reduce_sum(out=lsum[:], in_=lpp[:], axis=AX.X), reads=[bl2], writes=[bl3])
                    act(lsum[:], lsum[:], AF.Exp, [bl3], [bl3], joint=False)
                    tt(nlam[:], lsum[:, 1:2], lsum[:, 0:1], ALU.subtract, [bl3], [bnl], joint=False)
                    ts(nlam[:], nlam[:], -lam_init, None, ALU.add, None, [bnl], [bnl], joint=False)
                    sg, bsg = gain_tile(es, [diff_subln[0]])
                    ts(sg[:], sg[:], 1.0 - lam_init, None, ALU.mult, None, [bsg], [bsg], joint=False)

                    def fin_diff(u, q0, qn, O, Lp, R):
                        r0_, br0 = R["ftm"][0].next()
                        r1_, br1 = R["ftm"][1].next()
                        o_, bo = R["ftm"][2].next()
                        o0_, bo0 = R["ftm"][3].next()
                        o1_, bo1 = R["ftm"][4].next()
                        P.op(DVE, lambda e: e.tensor_copy(out=r0_[:, 0:qn], in_=Lp[0][0][:, 0:qn]), reads=[Lp[0][1]], writes=[br0])
                        P.op(DVE, lambda e: e.tensor_copy(out=r1_[:, 0:qn], in_=Lp[1][0][:, 0:qn]), reads=[Lp[1][1]], writes=[br1])
                        P.op(DVE, lambda e: e.tensor_copy(out=o0_[:, 0:qn], in_=O[0][0][:, 0:qn]), reads=[O[0][1]], writes=[bo0])
                        P.op(DVE, lambda e: e.tensor_copy(out=o1_[:, 0:qn], in_=O[1][0][:, 0:qn]), reads=[O[1][1]], writes=[bo1])
                        recip(o_[:, 0:qn], r0_[:, 0:qn], [br0], [bo], joint=False)
                        tt(o0_[:, 0:qn], o0_[:, 0:qn], o_[:, 0:qn], ALU.mult, [bo0, bo], [bo0], joint=False)
                        recip(o_[:, 0:qn], r1_[:, 0:qn], [br1], [bo], joint=False)
                        tt(o1_[:, 0:qn], o1_[:, 0:qn], o_[:, 0:qn], ALU.mult, [bo1, bo], [bo1], joint=False)
                        stt(o_[:, 0:qn], o1_[:, 0:qn], nlam[:, 0:1], o0_[:, 0:qn], ALU.mult, ALU.add, [bo0, bo1, bnl], [bo], joint=False)
                        sq, bsq = R["fsq"].next()
                        tt(sq[:, 0:qn], o_[:, 0:qn], o_[:, 0:qn], ALU.mult, [bo], [bsq], joint=False)

                        def stage_b():
                            fp, bfp = R["fpr"].next()
                            mm(fp[:, 0:qn], ones128, sq[:, 0:qn], True, True, [bsq, cst], bfp)
                            act(r0_[:, 0:qn], fp[:, 0:qn], AF.Ln, [bfp, cst], [br0], joint=False, scale=1.0 / 128, bias=epsT[:, 0:1])
                            act(r1_[:, 0:qn], r0_[:, 0:qn], AF.Exp, [br0], [br1], joint=False, scale=-0.5)
                            stt(u["AO"][:, u["c"], q0:q0 + qn], o_[:, 0:qn], sg[:, 0:1], r1_[:, 0:qn], ALU.mult, ALU.mult, [bo, br1, bsg], [u["aob"]], joint=True)
                        return stage_b

                    for h in range(8):
                        subs = [dict(pairs=[(("k", h), ("q", h, m), 0, 128)], v=("v", h), ones=ones128, acc=m) for m in range(2)]
                        units.append(dict(c=h, loads=[(("q", h, 0), Qs[h], 0, 64), (("q", h, 1), Qs[h], 64, 64), (("k", h), Ks[h])], vloads=[(("v", h), h * 128, 128, 0)],
                                          subs=subs, qblocks=qbs, fin=fin_diff, AO=AO, aob=aob))
                    attn_phase(es, AO, aob, units, scale)

            run_phase(phB)
            if dbg and L == nlayers - 1:
                def phBd(es):
                    bd = Buf("dbga")
                    dma(SP, dbg_AO[:, :, :], AO[:], [aob], [], bd)
                run_phase(phBd)

            def phC(es):
                wo_d = [gqa_wo, mla_wo, win_wo, diff_wo][kind][0].rearrange("(c p) n -> p c n", p=128)
                wo = sb(es, "wo", [128, 8, D], BF16)
                bwo = Buf("wo")
                for hh in range(2):
                    dma(POOL, wo[:, hh * 4:(hh + 1) * 4, :], wo_d[:, hh * 4:(hh + 1) * 4, :], [], [bwo], bwo)
                grow, bgrow = load_gate_rows(es, L, 2)
                opr = Ring(es, nc, "cps", [128, 512], F32, 4, psum=True)
                tmr = Ring(es, nc, "ctmp", [128, 512], F32, 3)
                tl = list(range(NT)) if need_ctx else list(range(2, NT))
                for t in tl:
                    r = 1 if t < 2 else 0
                    for dh in range(2):
                        ps, bps = opr.next()
                        for c in range(8):
                            mm(ps[:, :], AO[:, c, t * 128:(t + 1) * 128], wo[:, c, dh * 512:(dh + 1) * 512], c == 0, c == 7, [aob, bwo], bps)
                        tm, btm = tmr.next()
                        tt(tm[:], ps[:, :], grow[:, r, dh * 512:(dh + 1) * 512], ALU.mult, [bps, bgrow], [btm], joint=False)
                        xs = x_sb[:, t, dh * 512:(dh + 1) * 512]
                        tt(xs, tm[:], xs, ALU.add, [btm, xb[t]], [xb[t]], joint=False)
            run_phase(phC)
            if dbg and L == nlayers - 1:
                def phCd(es):
                    for t in range(NT):
                        dma(SP, dbg_xa[:, t, :], x_sb[:, t, :], [xb[t]], [], xb[t])
                run_phase(phCd)

        with ExitStack() as es0:
            phBC(es0)

        tbs_f = tbs_all if need_ctx else tbs_lat
        with ExitStack() as es0:
            hT = sb(es0, "hTf", [128, 8, T], BF16)
            hb = [Buf(f"hTf{i}") for i in range(5)]
            comb = sb(es0, "comb", [128, NT, 8], F32)
            b_comb = Buf("comb")

            def phD(es):
                if moe:
                    lg = sb(es, "lg", [128, NT, 8], F32)
                    blg = Buf("lg")
                    P.op(DVE, lambda e: e.memset(lg[:], 0.0), writes=[blg])
                    m = dict(router=moe_router[L // 2], lg=lg, blg=blg)
                    norm_phase(es, L, 1, hT, hb, tbs_f, moe=m)
                    route(es, m, comb, b_comb)
                else:
                    norm_phase(es, L, 1, hT, hb, tbs_f)
            run_phase(phD)

            def phE(es):
                if moe:
                    experts = [(moe_w13[L // 2, e], moe_w2[L // 2, e]) for e in range(8)]
                    ffn_phase(es, L, hT, hb, tbs_f, experts, comb, b_comb)
                else:
                    ffn_phase(es, L, hT, hb, tbs_f, [(ffn_w13[L // 2], ffn_w2[L // 2])], None, None)
            run_phase(phE)

    run_phase(phase0)
    for L in range(nlayers):
        std_layer(L)

    def phase_out(es):
        for t in range(16):
            dma(SP, out_d[t * 128:(t + 1) * 128, :], x_sb[:, 2 + t, :], [xb[2 + t]], [], xb[2 + t])
    run_phase(phase_out)
    top.close()
    return nc


_CACHE = {}


def kernel(**inputs):
    nl = int(inputs.pop("_nlayers", 4))
    dbg = bool(inputs.pop("_dbg", False))
    ncores = int(inputs.pop("_ncores", 8))
    trace = bool(inputs.pop("_trace", False))
    if (nl, dbg) not in _CACHE:
        _CACHE[(nl, dbg)] = build(nl, dbg)
    nc = _CACHE[(nl, dbg)]
    consts = _consts()
    shared = {}
    for k in ["ada_w", "ada_b", "norm_mix", "norm_ffn", "gqa_wqkv", "gqa_q_gain", "gqa_k_gain", "gqa_wo", "mla_wdown",
              "mla_qa_gain", "mla_kva_gain", "mla_wuq", "mla_wukv", "mla_q_gain", "mla_k_gain", "mla_wo", "win_wqkv",
              "win_q_gain", "win_k_gain", "win_sink", "win_wo", "diff_wqkv", "diff_q_gain", "diff_k_gain", "diff_lambda",
              "diff_subln", "diff_wo", "ffn_w13", "ffn_w2", "moe_router", "moe_w13", "moe_w2"]:
        shared[k] = np.ascontiguousarray(np.asarray(inputs[k], dtype=np.float32))
        if nl < 2 and k in ("moe_w13", "moe_w2"):
            shared[k] = np.zeros((1, 1, 8, 8), np.float32)
    shared.update(consts)
    x = np.asarray(inputs["x"], dtype=np.float32)
    c = np.asarray(inputs["c"], dtype=np.float32)
    ctx = np.asarray(inputs["ctx"], dtype=np.float32)
    c_ctx = np.asarray(inputs["c_ctx"], dtype=np.float32)
    in_maps = []
    for b in range(ncores):
        m = dict(shared)
        m["x"] = np.ascontiguousarray(x[b])
        m["ctx"] = np.ascontiguousarray(ctx[b])
        cfm = np.stack([c[b].reshape(8, 128).T, c_ctx.reshape(8, 128).T], axis=-1)
        m["cfm"] = np.ascontiguousarray(cfm.astype(np.float32))
        in_maps.append(m)
    if trace:
        res = run_bass_kernel_spmd(nc, in_maps, core_ids=list(range(ncores)), trace=True)
        print("EXEC_NS", res.exec_time_ns, flush=True)
        try:
            sc = res.per_core_scope_times or {}
            for k_, v_ in sc.items():
                print("SCOPE", k_, v_, flush=True)
        except Exception as e_:
            print("scope err", e_)
    else:
        res = run_bass_kernel_spmd(nc, in_maps, core_ids=list(range(ncores)))
    if dbg:
        return res.results
    return np.stack([r["out"] for r in res.results], axis=0).astype(np.float32)
```
